# Optimizing a Trainium2 kernel written in Bass

```python
import math
import jax, jax.numpy as jnp
from jax import lax
import numpy as np

D_MODEL = 1024
BATCH = 8
SEQ = 8192
DEPTH = 1

N_MEM = 256
EPS = 1e-6

SSD_D_INNER = D_MODEL
SSD_HEAD_DIM = 64
SSD_HEADS = SSD_D_INNER // SSD_HEAD_DIM
SSD_GROUPS = 4
SSD_HEADS_PER_GROUP = SSD_HEADS // SSD_GROUPS
SSD_STATE = 128
SSD_CONV = 4
SSD_CHUNK = 128
SSD_CONV_DIM = SSD_D_INNER + 2 * SSD_GROUPS * SSD_STATE

DIL_PAIRS = ((128, 1), (512, 4), (2048, 16))
DIL_HEADS_PER_GROUP = 4
DIL_HEAD_DIM = 64
DIL_HEADS = len(DIL_PAIRS) * DIL_HEADS_PER_GROUP
DIL_WIDTH = DIL_HEADS * DIL_HEAD_DIM
DIL_OUT_WIDTH = DIL_HEADS_PER_GROUP * DIL_HEAD_DIM

MEM_HEADS = 4
MEM_HEAD_DIM = 192
MEM_WIDTH = MEM_HEADS * MEM_HEAD_DIM

ROPE_THETA = 500000.0
ROPE_FRACTION = 4

PEER_HEADS = 8
PEER_N_KEYS = 128
PEER_EXPERTS = PEER_N_KEYS * PEER_N_KEYS
PEER_QUERY_DIM = 256
PEER_TOPK = 16
PEER_BLOCK = 64

N_BRANCHES = 3

OFF_Z = 0
OFF_XBC = OFF_Z + SSD_D_INNER
OFF_DT = OFF_XBC + SSD_CONV_DIM
OFF_DQ = OFF_DT + SSD_HEADS
OFF_DK = OFF_DQ + DIL_WIDTH
OFF_DV = OFF_DK + DIL_WIDTH
OFF_MQ = OFF_DV + DIL_WIDTH
OFF_GATE = OFF_MQ + MEM_WIDTH
IN_PROJ_WIDTH = OFF_GATE + N_BRANCHES * D_MODEL

kernel_name = 'hybrid_ssd_dilated_memory_peer_block'

F32 = jnp.float32


def _rms_norm(t, w):
    tf = t.astype(F32)
    tf = tf * lax.rsqrt(jnp.mean(tf * tf, axis=-1, keepdims=True) + EPS)
    return (tf * w.astype(F32)).astype(t.dtype)


def _partial_rope(t, positions):
    hd = t.shape[-1]
    rot = hd // ROPE_FRACTION
    half = rot // 2
    inv = jnp.exp(-math.log(ROPE_THETA) * (2.0 / rot) * jnp.arange(half, dtype=F32))
    ang = positions.astype(F32)[..., None] * inv
    cos = jnp.cos(ang)[:, :, None, :]
    sin = jnp.sin(ang)[:, :, None, :]
    tf = t.astype(F32)
    t1 = tf[..., :half]
    t2 = tf[..., half:rot]
    out = jnp.concatenate([t1 * cos - t2 * sin, t2 * cos + t1 * sin, tf[..., rot:]], axis=-1)
    return out.astype(t.dtype)


def _causal_depthwise_conv(t, w, bias):
    c = t.shape[-1]
    y = lax.conv_general_dilated(
        t, w[:, None, :].astype(t.dtype), window_strides=(1,),
        padding=[(w.shape[0] - 1, 0)],
        dimension_numbers=('NWC', 'WIO', 'NWC'),
        feature_group_count=c)
    return y + bias.astype(t.dtype)


def _ssd_chunked(xh, dt, a, bmat, cmat):
    b, s, g, r, p = xh.shape
    n = bmat.shape[-1]
    nc = s // SSD_CHUNK
    x_dt = xh.astype(F32) * dt[..., None]
    da = dt * a

    def chunks(t):
        return jnp.moveaxis(t.reshape(b, nc, SSD_CHUNK, *t.shape[2:]), 1, 0)

    tril = jnp.tril(jnp.ones((SSD_CHUNK, SSD_CHUNK), dtype=bool))
    strict = jnp.tril(jnp.ones((SSD_CHUNK, SSD_CHUNK), dtype=bool), k=-1)

    def step(state, inp):
        xc, dac, bc, cc = inp
        dat = jnp.moveaxis(dac, 1, -1)
        acum = jnp.cumsum(dat, axis=-1)
        seg = jnp.cumsum(jnp.where(strict, dat[..., :, None], 0.0), axis=-2)
        lmat = jnp.exp(jnp.where(tril, seg, -jnp.inf))
        cb = jnp.einsum('bsgn,btgn->bgst', cc, bc)
        y_diag = jnp.einsum('bgst,bgrst,btgrp->bsgrp', cb, lmat, xc)
        y_off = jnp.einsum('bsgn,bgrpn,bgrs->bsgrp', cc, state, jnp.exp(acum))
        decay = jnp.exp(acum[..., -1:] - acum)
        new_state = (state * jnp.exp(acum[..., -1])[..., None, None]
                     + jnp.einsum('btgn,bgrt,btgrp->bgrpn', bc, decay, xc))
        return new_state, y_diag + y_off

    state0 = jnp.zeros((b, g, r, p, n), F32)
    _, ys = lax.scan(step, state0, (chunks(x_dt), chunks(da),
                                    chunks(bmat.astype(F32)), chunks(cmat.astype(F32))))
    return jnp.moveaxis(ys, 0, 1).reshape(b, s, g, r, p)


def _dilated_group_attention(q, k, v, dilation, n_back):
    b, s, h, hd = q.shape
    m = s // dilation
    blk = n_back
    nb = -(-m // blk)
    mp = nb * blk

    def to_sub(t):
        t = t.reshape(b, m, dilation, h, hd)
        t = jnp.pad(t, ((0, 0), (0, mp - m), (0, 0), (0, 0), (0, 0)))
        return t.reshape(b, nb, blk, dilation, h, hd)

    def with_prev(t):
        prev = jnp.pad(t[:, :-1], ((0, 0), (1, 0), (0, 0), (0, 0), (0, 0), (0, 0)))
        return jnp.concatenate([prev, t], axis=2)

    qs = to_sub(q)
    kk = with_prev(to_sub(k))
    vv = with_prev(to_sub(v))
    scores = jnp.einsum('bnqrhd,bnkrhd->bnrhqk', qs, kk).astype(F32) * (hd ** -0.5)
    qi = jnp.arange(blk)[:, None]
    ki = jnp.arange(2 * blk)[None, :]
    dist = qi + blk - ki
    band = (dist >= 0) & (dist <= n_back)
    valid = band[None] & ((jnp.arange(nb)[:, None, None] > 0) | (ki >= blk)[None])
    scores = jnp.where(valid[None, :, None, None], scores, -jnp.inf)
    lse = jax.nn.logsumexp(scores, axis=-1)
    probs = jnp.exp(scores - lse[..., None]).astype(v.dtype)
    out = jnp.einsum('bnrhqk,bnkrhd->bnqrhd', probs, vv)
    out = out.reshape(b, mp, dilation, h, hd)[:, :m].reshape(b, s, h, hd)
    lse = lse.transpose(0, 1, 4, 2, 3).reshape(b, mp, dilation, h)[:, :m].reshape(b, s, h)
    return out, lse


def _peer(hn, w_query, sub_keys, expert_down, expert_up):
    b, s, d = hn.shape
    q = (hn @ w_query).reshape(b, s, PEER_HEADS, 2, PEER_QUERY_DIM // 2)
    sc = jnp.einsum('bshcd,hckd->bshck', q, sub_keys).astype(F32)
    v1, i1 = lax.top_k(sc[..., 0, :], PEER_TOPK)
    v2, i2 = lax.top_k(sc[..., 1, :], PEER_TOPK)
    cand = (v1[..., :, None] + v2[..., None, :]).reshape(b, s, PEER_HEADS, PEER_TOPK * PEER_TOPK)
    cidx = (i1[..., :, None] * PEER_N_KEYS + i2[..., None, :]).reshape(b, s, PEER_HEADS, PEER_TOPK * PEER_TOPK)
    top, pos = lax.top_k(cand, PEER_TOPK)
    idx = jnp.take_along_axis(cidx, pos, axis=-1)
    gate = jax.nn.softmax(top, axis=-1)
    nblk = s // PEER_BLOCK

    def blockify(t):
        return jnp.moveaxis(t.reshape(b, nblk, PEER_BLOCK, *t.shape[2:]), 1, 0)

    def one_block(args):
        hb, ib, gb = args
        u = expert_down[ib]
        act = jax.nn.gelu(jnp.einsum('bld,blhkd->blhk', hb, u), approximate=False)
        coef = (gb * act.astype(F32)).astype(hb.dtype)
        vv = expert_up[ib]
        return jnp.einsum('blhk,blhkd->bld', coef, vv)

    out = lax.map(one_block, (blockify(hn), blockify(idx), blockify(gate)))
    return jnp.moveaxis(out, 0, 1).reshape(b, s, d)


def setup_inputs(seed: int = 0) -> dict:
    key = jax.random.key(seed)
    ks = jax.random.split(key, 32)
    L = DEPTH

    def nrm(k, shape, scale):
        return jax.random.normal(k, shape, F32) * scale

    def gain(k, shape):
        return 1.0 + 0.05 * jax.random.normal(k, shape, F32)

    x = nrm(ks[0], (BATCH, SEQ, D_MODEL), 1.0)
    mem = nrm(ks[1], (BATCH, N_MEM, D_MODEL), 1.0)
    offset = jax.random.randint(ks[2], (BATCH, 1), 0, 4096, dtype=jnp.int32)
    positions = jnp.arange(SEQ, dtype=jnp.int32)[None, :] + offset
    norm_mix_w = gain(ks[3], (L, D_MODEL))
    w_in = nrm(ks[4], (L, D_MODEL, IN_PROJ_WIDTH), D_MODEL ** -0.5)
    ssd_conv_w = nrm(ks[5], (L, SSD_CONV, SSD_CONV_DIM), SSD_CONV ** -0.5)
    ssd_conv_b = nrm(ks[6], (L, SSD_CONV_DIM), 0.02)
    dt0 = jnp.exp(jax.random.uniform(ks[7], (L, SSD_HEADS), F32, math.log(1e-3), math.log(1e-1)))
    ssd_dt_bias = dt0 + jnp.log(-jnp.expm1(-dt0))
    ssd_a_log = jnp.log(jax.random.uniform(ks[8], (L, SSD_HEADS), F32, 1.0, 16.0))
    ssd_d = gain(ks[9], (L, SSD_HEADS))
    ssd_norm_w = gain(ks[10], (L, SSD_D_INNER))
    dil_q_norm_w = gain(ks[11], (L, DIL_HEAD_DIM))
    dil_k_norm_w = gain(ks[12], (L, DIL_HEAD_DIM))
    mem_norm_w = gain(ks[13], (L, D_MODEL))
    w_mem_kv = nrm(ks[14], (L, D_MODEL, 2 * MEM_WIDTH), D_MODEL ** -0.5)
    mem_q_norm_w = gain(ks[15], (L, MEM_HEAD_DIM))
    mem_k_norm_w = gain(ks[16], (L, MEM_HEAD_DIM))
    w_ssd_br = nrm(ks[17], (L, SSD_D_INNER, D_MODEL), SSD_D_INNER ** -0.5)
    w_dil_br = nrm(ks[18], (L, DIL_OUT_WIDTH, D_MODEL), DIL_OUT_WIDTH ** -0.5)
    w_mem_br = nrm(ks[19], (L, MEM_WIDTH, D_MODEL), MEM_WIDTH ** -0.5)
    w_out = nrm(ks[20], (L, D_MODEL, D_MODEL), D_MODEL ** -0.5)
    norm_ffn_w = gain(ks[21], (L, D_MODEL))
    peer_w_query = nrm(ks[22], (L, D_MODEL, PEER_HEADS * PEER_QUERY_DIM), D_MODEL ** -0.5)
    peer_sub_keys = nrm(ks[23], (L, PEER_HEADS, 2, PEER_N_KEYS, PEER_QUERY_DIM // 2),
                        (PEER_QUERY_DIM // 2) ** -0.5)
    peer_down = nrm(ks[24], (L, PEER_EXPERTS, D_MODEL), D_MODEL ** -0.5)
    peer_up = nrm(ks[25], (L, PEER_EXPERTS, D_MODEL), PEER_HEADS ** -0.5)
    return {
        'x': x, 'mem': mem, 'positions': positions,
        'norm_mix_w': norm_mix_w, 'w_in': w_in,
        'ssd_conv_w': ssd_conv_w, 'ssd_conv_b': ssd_conv_b, 'ssd_dt_bias': ssd_dt_bias,
        'ssd_a_log': ssd_a_log, 'ssd_d': ssd_d, 'ssd_norm_w': ssd_norm_w,
        'dil_q_norm_w': dil_q_norm_w, 'dil_k_norm_w': dil_k_norm_w,
        'mem_norm_w': mem_norm_w, 'w_mem_kv': w_mem_kv,
        'mem_q_norm_w': mem_q_norm_w, 'mem_k_norm_w': mem_k_norm_w,
        'w_ssd_br': w_ssd_br, 'w_dil_br': w_dil_br, 'w_mem_br': w_mem_br, 'w_out': w_out,
        'norm_ffn_w': norm_ffn_w, 'peer_w_query': peer_w_query, 'peer_sub_keys': peer_sub_keys,
        'peer_down': peer_down, 'peer_up': peer_up,
    }


def reference(x, mem, positions, norm_mix_w, w_in, ssd_conv_w, ssd_conv_b, ssd_dt_bias,
              ssd_a_log, ssd_d, ssd_norm_w, dil_q_norm_w, dil_k_norm_w, mem_norm_w, w_mem_kv,
              mem_q_norm_w, mem_k_norm_w, w_ssd_br, w_dil_br, w_mem_br, w_out, norm_ffn_w,
              peer_w_query, peer_sub_keys, peer_down, peer_up):
    b, s, _ = x.shape
    n_mem = mem.shape[1]
    h = x
    for layer in range(DEPTH):
        u = _rms_norm(h, norm_mix_w[layer])
        proj = u @ w_in[layer]
        z = proj[..., OFF_Z:OFF_XBC]
        xbc = proj[..., OFF_XBC:OFF_DT]
        dt_raw = proj[..., OFF_DT:OFF_DQ]
        dq = proj[..., OFF_DQ:OFF_DK].reshape(b, s, DIL_HEADS, DIL_HEAD_DIM)
        dk = proj[..., OFF_DK:OFF_DV].reshape(b, s, DIL_HEADS, DIL_HEAD_DIM)
        dv = proj[..., OFF_DV:OFF_MQ].reshape(b, s, DIL_HEADS, DIL_HEAD_DIM)
        mq = proj[..., OFF_MQ:OFF_GATE].reshape(b, s, MEM_HEADS, MEM_HEAD_DIM)
        gates = jax.nn.sigmoid(proj[..., OFF_GATE:].astype(F32)).reshape(b, s, N_BRANCHES, D_MODEL)

        xbc = jax.nn.silu(_causal_depthwise_conv(xbc, ssd_conv_w[layer], ssd_conv_b[layer]))
        gn = SSD_GROUPS * SSD_STATE
        xs = xbc[..., :SSD_D_INNER].reshape(b, s, SSD_GROUPS, SSD_HEADS_PER_GROUP, SSD_HEAD_DIM)
        bm = xbc[..., SSD_D_INNER:SSD_D_INNER + gn].reshape(b, s, SSD_GROUPS, SSD_STATE)
        cm = xbc[..., SSD_D_INNER + gn:].reshape(b, s, SSD_GROUPS, SSD_STATE)
        dt = jax.nn.softplus(dt_raw.astype(F32) + ssd_dt_bias[layer].astype(F32))
        dt = dt.reshape(b, s, SSD_GROUPS, SSD_HEADS_PER_GROUP)
        a = -jnp.exp(ssd_a_log[layer].astype(F32)).reshape(SSD_GROUPS, SSD_HEADS_PER_GROUP)
        y = _ssd_chunked(xs, dt, a, bm, cm)
        y = y + ssd_d[layer].astype(F32).reshape(SSD_GROUPS, SSD_HEADS_PER_GROUP)[..., None] * xs.astype(F32)
        y = y.reshape(b, s, SSD_D_INNER) * jax.nn.silu(z.astype(F32))
        yg = y.reshape(b, s, SSD_GROUPS, SSD_D_INNER // SSD_GROUPS)
        yg = yg * lax.rsqrt(jnp.mean(yg * yg, axis=-1, keepdims=True) + EPS)
        y_ssd = (yg.reshape(b, s, SSD_D_INNER) * ssd_norm_w[layer].astype(F32)).astype(h.dtype)

        dq = _partial_rope(_rms_norm(dq, dil_q_norm_w[layer]), positions)
        dk = _partial_rope(_rms_norm(dk, dil_k_norm_w[layer]), positions)
        outs = []
        lses = []
        for gi, (window, dilation) in enumerate(DIL_PAIRS):
            sl = slice(gi * DIL_HEADS_PER_GROUP, (gi + 1) * DIL_HEADS_PER_GROUP)
            o, l = _dilated_group_attention(dq[:, :, sl], dk[:, :, sl], dv[:, :, sl],
                                            dilation, window // dilation)
            outs.append(o.astype(F32))
            lses.append(l)
        wts = jax.nn.softmax(jnp.stack(lses, axis=0), axis=0)
        y_dil = jnp.sum(wts[..., None] * jnp.stack(outs, axis=0), axis=0)
        y_dil = y_dil.reshape(b, s, DIL_OUT_WIDTH).astype(h.dtype)

        mkv = _rms_norm(mem, mem_norm_w[layer]) @ w_mem_kv[layer]
        mk = mkv[..., :MEM_WIDTH].reshape(b, n_mem, MEM_HEADS, MEM_HEAD_DIM)
        mv = mkv[..., MEM_WIDTH:].reshape(b, n_mem, MEM_HEADS, MEM_HEAD_DIM)
        mqn = _rms_norm(mq, mem_q_norm_w[layer])
        mk = _rms_norm(mk, mem_k_norm_w[layer])
        msc = jnp.einsum('bshd,bmhd->bhsm', mqn, mk).astype(F32) * (MEM_HEAD_DIM ** -0.5)
        mp = jax.nn.softmax(msc, axis=-1).astype(mv.dtype)
        y_mem = jnp.einsum('bhsm,bmhd->bshd', mp, mv).reshape(b, s, MEM_WIDTH)

        merged = (gates[:, :, 0] * (y_ssd @ w_ssd_br[layer]).astype(F32)
                  + gates[:, :, 1] * (y_dil @ w_dil_br[layer]).astype(F32)
                  + gates[:, :, 2] * (y_mem @ w_mem_br[layer]).astype(F32))
        h = h + merged.astype(h.dtype) @ w_out[layer]

        hn = _rms_norm(h, norm_ffn_w[layer])
        h = h + _peer(hn, peer_w_query[layer], peer_sub_keys[layer], peer_down[layer], peer_up[layer])
    return h
```

```python
import math
from contextlib import ExitStack

import numpy as np
import concourse.bass as bass
import concourse.mybir as mybir
from concourse.bass_utils import run_bass_kernel_spmd

F32 = mybir.dt.float32
BF16 = mybir.dt.bfloat16
U32 = mybir.dt.uint32
I32 = mybir.dt.int32
AF = mybir.ActivationFunctionType
ALU = mybir.AluOpType
AX = mybir.AxisListType

D = 1024
EPS = 1e-6
NPROJ = 9232
OFF_Z, OFF_XBC, OFF_DT, OFF_DQ, OFF_DK, OFF_DV, OFF_MQ, OFF_G = 0, 1024, 3072, 3088, 3856, 4624, 5392, 6160
DILS = (1, 4, 16)
MAGIC = 12582912.0


class Sched:
    ENGS = ("pe", "act", "dve", "pool", "sp")

    def __init__(self, nc):
        self.nc = nc
        self.ops = []
        self.last_w = {}
        self.readers = {}
        self.dma_count = {}

    def add(self, eng, fn, reads=(), writes=(), dma_key=None, wait_all_dma=False):
        idx = len(self.ops)
        deps = {}
        is_dma = dma_key is not None

        def dep(j, kind):
            o = self.ops[j]
            if o["dma"] is None and o["eng"] == eng and not is_dma:
                if eng == "pe" or kind != "raw":
                    return
            deps[j] = True

        for o in reads:
            if o in self.last_w:
                dep(self.last_w[o], "raw")
        for o in writes:
            if o in self.last_w:
                dep(self.last_w[o], "waw")
            for r in self.readers.get(o, {}).values():
                dep(r, "war")
        for o in writes:
            self.last_w[o] = idx
            self.readers[o] = {}
        for o in reads:
            rk = ("dma", idx) if is_dma else eng
            self.readers.setdefault(o, {})[rk] = idx
        dma_waits = {}
        comp_deps = []
        for j in deps:
            o = self.ops[j]
            if o["dma"] is not None:
                dma_waits[o["dma"]] = self.dma_count[o["dma"]]
            else:
                comp_deps.append(j)
        if wait_all_dma:
            for k, c in self.dma_count.items():
                dma_waits[k] = c
        if is_dma:
            self.dma_count[dma_key] = self.dma_count.get(dma_key, 0) + 1
        self.ops.append(dict(eng=eng, fn=fn, dma=dma_key, comp_deps=comp_deps,
                             dma_waits=dma_waits, signal=False, seq=None))
        return idx

    def barrier(self, tag):
        bt = self.bar_tile
        fns = {"pe": lambda eng: eng.nop(), "sp": lambda eng: eng.nop(),
               "act": lambda eng: eng.copy(bt[:, 0:1], bt[:, 4:5]),
               "dve": lambda eng: eng.tensor_copy(bt[:, 1:2], bt[:, 4:5]),
               "pool": lambda eng: eng.memset(bt[:, 2:3], 0.0)}
        for e in self.ENGS:
            self.add(e, fns[e], writes=[("barA", tag, e)], wait_all_dma=True)
        for e in self.ENGS:
            self.add(e, lambda eng: eng.nop(), reads=[("barA", tag, x) for x in self.ENGS],
                     writes=[("barB", tag, e)])
        self.last_w = {}
        self.readers = {}

    def emit(self, stack):
        nc = self.nc
        ops = self.ops
        for o in ops:
            for j in o["comp_deps"]:
                ops[j]["signal"] = True
        cnt = {e: 0 for e in self.ENGS}
        for o in ops:
            if o["signal"]:
                cnt[o["eng"]] += 1
                o["seq"] = cnt[o["eng"]]
        self.stats = dict(cnt=dict(cnt), n_ops=len(ops), max_dma=max([16 * v for v in self.dma_count.values()] + [0]),
                          n_sems=5 + len(self.dma_count))
        sems = {e: stack.enter_context(nc.semaphore("s_" + e)) for e in self.ENGS}
        dsems = {k: stack.enter_context(nc.semaphore("d_%d" % i))
                 for i, k in enumerate(self.dma_count)}
        block = stack.enter_context(nc.Block())
        per = {e: [] for e in self.ENGS}
        for o in ops:
            per[o["eng"]].append(o)

        def run(eng_name, eng):
            seen = {e: 0 for e in self.ENGS}
            dseen = {}
            for o in per[eng_name]:
                need = {}
                for j in o["comp_deps"]:
                    d = ops[j]
                    if d["seq"] > seen[d["eng"]]:
                        need[d["eng"]] = max(need.get(d["eng"], 0), d["seq"])
                for e, v in need.items():
                    eng.wait_ge(sems[e], v)
                    seen[e] = v
                for k, c in o["dma_waits"].items():
                    if dseen.get(k, 0) < c:
                        eng.wait_ge(dsems[k], 16 * c)
                        dseen[k] = c
                ins = o["fn"](eng)
                if o["dma"] is not None:
                    ins.then_inc(dsems[o["dma"]], 16)
                elif o["signal"]:
                    ins.then_inc(sems[eng_name], 1)
            for k in self.dma_count:
                if dseen.get(k, 0) < self.dma_count[k]:
                    eng.wait_ge(dsems[k], 16 * self.dma_count[k])

        @block.tensor
        def _(e):
            run("pe", e)

        @block.scalar
        def _(e):
            run("act", e)

        @block.vector
        def _(e):
            run("dve", e)

        @block.gpsimd
        def _(e):
            run("pool", e)

        @block.sync
        def _(e):
            run("sp", e)


class Arena:
    def __init__(self, ap, nwords):
        self.ap = ap
        self.n = nwords
        self.off = 0
        self.cnt = 0

    def reset(self):
        self.off = 0

    def get(self, shape, dt):
        esz = 4 if dt in (F32, U32, I32) else 2
        nel = 1
        for s in shape[1:]:
            nel *= s
        words = (nel * esz + 3) // 4
        words = (words + 7) // 8 * 8
        assert self.off + words <= self.n, ("arena overflow", self.off, words, self.n)
        v = self.ap[:, self.off:self.off + words]
        self.off += words
        if dt != F32:
            v = v.bitcast(dt)
        v = v[:, 0:nel]
        if len(shape) == 3:
            v = v.rearrange("p (a b) -> p a b", a=shape[1])
        elif len(shape) == 4:
            v = v.rearrange("p (a b c) -> p a b c", a=shape[1], b=shape[2])
        self.cnt += 1
        return v, ("ar", self.cnt)


def bc(ap, shape, axis):
    return ap.unsqueeze(axis).to_broadcast(list(shape))


def build_program(S, debug=False):
    nc = bass.Bass("TRN2", target_bir_lowering=False)
    NT = S // 128
    NB = S // 512
    skind = "ExternalOutput" if debug else "Internal"

    def din(name, shape, dt=F32):
        return nc.dram_tensor(name, list(shape), dt, kind="ExternalInput").ap()

    def dscr(name, shape, dt):
        return nc.dram_tensor(name, list(shape), dt, kind=skind).ap()

    x = din("x", [S, D])
    mem = din("mem", [256, D])
    pos_t = din("pos_t", [128, NT], I32)
    nmw = din("nmw", [128, D])
    w_in_r = din("w_in_r", [128, 8, NPROJ])
    cw = din("cw", [128, 16, 4])
    cb = din("cb", [128, 16])
    dtb = din("dtb", [128, 16])
    alog = din("alog", [128, 16])
    sdd = din("sdd", [128, 16])
    snw = din("snw", [128, D])
    qnw = din("qnw", [128, 64])
    knw = din("knw", [128, 64])
    mnw = din("mnw", [128, D])
    wkv = din("wkv", [128, 8, 1536])
    mqnw = din("mqnw", [128, 192])
    mknw = din("mknw", [128, 192])
    wsb = din("wsb", [128, 8, D])
    wdb = din("wdb", [128, 2, D])
    wmb = din("wmb", [128, 8, D])
    wo = din("wo", [128, 8, D])
    nfw = din("nfw", [128, D])
    wq = din("wq", [128, 8, 2048])
    skT = din("skT", [128, 16, 128])
    dnT_r = din("dnT_r", [128, 128, D])
    up_r = din("up_r", [128, 128, D])
    out = nc.dram_tensor("out", [S, D], F32, kind="ExternalOutput").ap()

    w_in_b = dscr("w_in_b", [128, 8, NPROJ], BF16)
    dnT_b = dscr("dnT_b", [128, 128, D], BF16)
    up_b = dscr("up_b", [128, 128, D], BF16)
    z_s = dscr("z_s", [S, D], BF16)
    xbc_s = dscr("xbc_s", [2048, S], F32)
    dt_s = dscr("dt_s", [S, 16], F32)
    q_s = dscr("q_s", [S, 768], BF16)
    k_s = dscr("k_s", [S, 768], BF16)
    v_s = dscr("v_s", [S, 768], BF16)
    mq_s = dscr("mq_s", [S, 768], BF16)
    g_s = dscr("g_s", [3072, S], BF16)
    yssd_s = dscr("yssd_s", [S, D], BF16)
    od_s = dscr("od_s", [3, S, 260], F32)
    ymem_s = dscr("ymem_s", [S, 768], BF16)
    h_s = dscr("h_s", [S, D], F32)
    hnT_s = dscr("hnT_s", [NT, 128, 8, 128], BF16)
    r_s = dscr("r_s", [NT, 128, 3, 128], F32)

    st = ExitStack()
    with st:
        S_ = Sched(nc)
        add = S_.add
        NA = 44 * 1024
        arena_t = st.enter_context(nc.sbuf_tensor("arena", [128, NA], F32))
        AR = Arena(arena_t, NA)
        NC_ = 3 * 1024
        const_t = st.enter_context(nc.sbuf_tensor("consts", [128, NC_], F32))
        CA = Arena(const_t, NC_)
        psum = st.enter_context(nc.psum_tensor("psum", [128, 8 * 512], F32))

        def bank(i, n=1):
            return psum[:, i * 512:(i + n) * 512]

        def bank_bf(i):
            return psum[:, i * 512:(i + 1) * 512].bitcast(BF16)

        PK = [("ps", i) for i in range(8)]

        ident_f, k_idf = CA.get([128, 128], F32)
        ident_b, k_idb = CA.get([128, 128], BF16)
        iota_f, k_iota = CA.get([128, 128], F32)
        rowi, k_rowi = CA.get([128, 128], F32)
        mge_f, k_mgef = CA.get([128, 128], F32)
        mge_b, k_mgeb = CA.get([128, 128], BF16)
        mle_b, k_mleb = CA.get([128, 128], BF16)
        negm, k_negm = CA.get([128, 128], F32)
        ones_f, k_onesf = CA.get([128, 128], F32)
        bar_t, _kb = CA.get([128, 8], F32)
        S_.bar_tile = bar_t
        add("pool", lambda e: e.memset(bar_t, 0.0), writes=[_kb])
        cs_t, k_cs = CA.get([128, NT, 8], F32)
        sn_t, k_sn = CA.get([128, NT, 8], F32)
        aneg, k_aneg = CA.get([128, 16], F32)
        sd_t, k_sd = CA.get([128, 16], F32)
        dtb_t, k_dtb = CA.get([128, 16], F32)
        cw_t, k_cw = CA.get([128, 16, 4], F32)
        cb_t, k_cb = CA.get([128, 16], F32)

        add("pool", lambda e: e.iota(iota_f, pattern=[[1, 128]], base=0, channel_multiplier=0,
                                     allow_small_or_imprecise_dtypes=True), writes=[k_iota])
        add("pool", lambda e: e.iota(rowi, pattern=[[1, 128]], base=0, channel_multiplier=-1,
                                     allow_small_or_imprecise_dtypes=True), writes=[k_rowi])
        add("dve", lambda e: e.tensor_single_scalar(ident_f, rowi, 0.0, ALU.is_equal), reads=[k_rowi], writes=[k_idf])
        add("dve", lambda e: e.tensor_copy(ident_b, ident_f), reads=[k_idf], writes=[k_idb])
        add("dve", lambda e: e.tensor_single_scalar(mge_f, rowi, 0.0, ALU.is_ge), reads=[k_rowi], writes=[k_mgef])
        add("dve", lambda e: e.tensor_copy(mge_b, mge_f), reads=[k_mgef], writes=[k_mgeb])
        add("dve", lambda e: e.tensor_single_scalar(mle_b, rowi, 0.0, ALU.is_le), reads=[k_rowi], writes=[k_mleb])
        add("dve", lambda e: e.tensor_scalar(negm, mge_f, -1.0, 30000.0, ALU.add, ALU.mult), reads=[k_mgef], writes=[k_negm])
        add("pool", lambda e: e.memset(ones_f, 1.0), writes=[k_onesf])
        add("sp", lambda e: e.dma_start(out=aneg, in_=alog), writes=[k_aneg], dma_key="c_aneg")
        add("act", lambda e: e.activation(out=aneg, in_=aneg, func=AF.Exp), reads=[k_aneg], writes=[k_aneg])
        add("dve", lambda e: e.tensor_single_scalar(aneg, aneg, -1.0, ALU.mult), reads=[k_aneg], writes=[k_aneg])
        add("sp", lambda e: e.dma_start(out=sd_t, in_=sdd), writes=[k_sd], dma_key="c_sd")
        add("sp", lambda e: e.dma_start(out=dtb_t, in_=dtb), writes=[k_dtb], dma_key="c_dtb")
        add("sp", lambda e: e.dma_start(out=cw_t, in_=cw), writes=[k_cw], dma_key="c_cw")
        add("sp", lambda e: e.dma_start(out=cb_t, in_=cb), writes=[k_cb], dma_key="c_cb")

        AR.reset()
        pos_i, k_posi = AR.get([128, NT], I32)
        pos_f, k_posf = AR.get([128, NT], F32)
        ang, k_ang = AR.get([128, NT, 8], F32)
        kk, k_kk = AR.get([128, NT, 8], F32)
        rr, k_rr = AR.get([128, NT, 8], F32)
        add("sp", lambda e: e.dma_start(out=pos_i, in_=pos_t), writes=[k_posi], dma_key="c_pos")
        add("dve", lambda e: e.tensor_copy(pos_f, pos_i), reads=[k_posi], writes=[k_posf])
        inv = np.exp(np.float32(-math.log(500000.0) * (2.0 / 16)) * np.arange(8, dtype=np.float32)).astype(np.float32)
        for j in range(8):
            add("dve", lambda e, j=j: e.tensor_single_scalar(ang[:, :, j], pos_f, float(inv[j]), ALU.mult),
                reads=[k_posf], writes=[k_ang])
        C1 = 6.28125
        rem = 2.0 * math.pi - C1
        C2 = float(np.float32(rem).view(np.uint32) & np.uint32(0xFFFFF000))
        C2 = float(np.array([np.float32(rem).view(np.uint32) & np.uint32(0xFFFFF000)], dtype=np.uint32).view(np.float32)[0])
        C3 = float(np.float32(rem - C2))
        add("dve", lambda e: e.tensor_scalar(kk, ang, 1.0 / (2.0 * math.pi), MAGIC, ALU.mult, ALU.add), reads=[k_ang], writes=[k_kk])
        add("dve", lambda e: e.tensor_single_scalar(kk, kk, -MAGIC, ALU.add), reads=[k_kk], writes=[k_kk])
        add("dve", lambda e: e.scalar_tensor_tensor(out=rr, in0=kk, scalar=-C1, in1=ang, op0=ALU.mult, op1=ALU.add), reads=[k_kk, k_ang], writes=[k_rr])
        add("dve", lambda e: e.scalar_tensor_tensor(out=rr, in0=kk, scalar=-C2, in1=rr, op0=ALU.mult, op1=ALU.add), reads=[k_kk, k_rr], writes=[k_rr])
        add("dve", lambda e: e.scalar_tensor_tensor(out=rr, in0=kk, scalar=-C3, in1=rr, op0=ALU.mult, op1=ALU.add), reads=[k_kk, k_rr], writes=[k_rr])
        add("dve", lambda e: e.tensor_scalar(rr, rr, 3.14159, -3.14159, ALU.min, ALU.max), reads=[k_rr], writes=[k_rr])
        add("act", lambda e: e.activation(out=sn_t, in_=rr, func=AF.Sin), reads=[k_rr], writes=[k_sn])
        add("dve", lambda e: e.tensor_single_scalar(kk, rr, -1.0, ALU.mult), reads=[k_rr], writes=[k_kk])
        add("dve", lambda e: e.tensor_tensor(kk, kk, rr, ALU.max), reads=[k_rr, k_kk], writes=[k_kk])
        add("dve", lambda e: e.tensor_scalar(kk, kk, -1.0, math.pi / 2.0, ALU.mult, ALU.add), reads=[k_kk], writes=[k_kk])
        add("act", lambda e: e.activation(out=cs_t, in_=kk, func=AF.Sin), reads=[k_kk], writes=[k_cs])
        S_.barrier("c0")

        AR.reset()
        wtmp = [AR.get([128, NPROJ], BF16) for _ in range(2)]
        for dc in range(8):
            t, kt = wtmp[dc % 2]
            add("pool", lambda e, t=t, dc=dc: e.dma_start(out=t, in_=w_in_r[:, dc, :]), writes=[kt], dma_key=("wt", dc % 2))
            add("sp", lambda e, t=t, dc=dc: e.dma_start(out=w_in_b[:, dc, :], in_=t), reads=[kt], dma_key="w_in_b")
        S_.barrier("w0")
        AR.reset()
        etmp = [AR.get([128, 8, D], BF16) for _ in range(4)]
        ei = 0
        for (src, dst, nm) in ((dnT_r, dnT_b, "dnT_b"), (up_r, up_b, "up_b")):
            for jg in range(16):
                t, kt = etmp[ei % 4]
                add("pool", lambda e, t=t, src=src, jg=jg: e.dma_start(
                    out=t, in_=src[jg * 8:(jg + 1) * 8].rearrange("j p f -> p j f")), writes=[kt], dma_key=("et", ei % 4))
                add("sp", lambda e, t=t, dst=dst, jg=jg: e.dma_start(
                    out=dst[jg * 8:(jg + 1) * 8].rearrange("j p f -> p j f"), in_=t), reads=[kt], dma_key=nm)
                ei += 1
        S_.barrier("w1")

        def rms_rstd(src, src_keys, n, scratch, k_scr, ssq, k_ssq, rstd, k_rstd, nh=1, hd=None):
            hd = hd or n
            if nh == 1:
                add("dve", lambda e: e.memset(ssq[:, 0:1], 0.0), writes=[k_ssq])
                add("act", lambda e: e.activation(out=scratch[:, 0:n], in_=src, func=AF.Square, accum_out=ssq[:, 0:1]),
                    reads=src_keys, writes=[k_scr, k_ssq])
            else:
                add("act", lambda e: e.activation(out=scratch[:, 0:n], in_=src, func=AF.Square),
                    reads=src_keys, writes=[k_scr])
                add("dve", lambda e: e.tensor_reduce(out=ssq[:, 0:nh], in_=scratch[:, 0:n].rearrange("p (h d) -> p h d", h=nh),
                                                     axis=AX.X, op=ALU.add), reads=[k_scr], writes=[k_ssq])
            add("dve", lambda e: e.tensor_scalar(ssq[:, 0:nh], ssq[:, 0:nh], 1.0 / hd, EPS, ALU.mult, ALU.add),
                reads=[k_ssq], writes=[k_ssq])
            add("act", lambda e: e.activation(out=ssq[:, 0:nh], in_=ssq[:, 0:nh], func=AF.Ln), reads=[k_ssq], writes=[k_ssq])
            add("act", lambda e: e.activation(out=rstd[:, 0:nh], in_=ssq[:, 0:nh], func=AF.Exp, scale=-0.5),
                reads=[k_ssq], writes=[k_rstd])

        AR.reset()
        nmw_t, k_nmw = AR.get([128, D], F32)
        qnw_t, k_qnw = AR.get([128, 64], F32)
        knw_t, k_knw = AR.get([128, 64], F32)
        mqnw_t, k_mqnw = AR.get([128, 192], F32)
        add("sp", lambda e: e.dma_start(out=nmw_t, in_=nmw), writes=[k_nmw], dma_key="a_c0")
        add("sp", lambda e: e.dma_start(out=qnw_t, in_=qnw), writes=[k_qnw], dma_key="a_c1")
        add("sp", lambda e: e.dma_start(out=knw_t, in_=knw), writes=[k_knw], dma_key="a_c2")
        add("sp", lambda e: e.dma_start(out=mqnw_t, in_=mqnw), writes=[k_mqnw], dma_key="a_c3")
        xt_r = [AR.get([128, D], F32) for _ in range(2)]
        sq_t, k_sq = AR.get([128, D], F32)
        ssq_t, k_ssq = AR.get([128, 16], F32)
        rstd_t, k_rstd = AR.get([128, 16], F32)
        ub_t, k_ub = AR.get([128, D], BF16)
        uT_r = [AR.get([128, 8, 512], BF16) for _ in range(2)]
        wseg_r = [AR.get([128, 8, 512], BF16) for _ in range(3)]
        ob_r = [AR.get([128, 512], BF16) for _ in range(4)]
        of_r = [AR.get([128, 512], F32) for _ in range(3)]
        qn_t, k_qn = AR.get([128, 512], F32)
        r1_t, k_r1 = AR.get([128, 8, 8], F32)
        r2_t, k_r2 = AR.get([128, 8, 8], F32)
        dt1_t, k_dt1 = AR.get([128, 16], F32)
        dtv_r = [AR.get([128, 16], F32) for _ in range(2)]

        segs = []
        for c0 in (0, 512):
            segs.append((OFF_Z + c0, 512, "z", c0))
        for c0 in range(0, 2048, 512):
            segs.append((OFF_XBC + c0, 512, "xbc", c0))
        segs.append((OFF_DT, 16, "dt", 0))
        for nm, off in (("q", OFF_DQ), ("k", OFF_DK), ("v", OFF_DV)):
            segs.append((off, 512, nm, 0))
            segs.append((off + 512, 256, nm, 512))
        segs.append((OFF_MQ, 384, "mq", 0))
        segs.append((OFF_MQ + 384, 384, "mq", 384))
        for c0 in range(0, 3072, 512):
            segs.append((OFF_G + c0, 512, "g", c0))

        cnt = dict(ob=0, of=0, ws=0, ps=0, dtv=0)

        def nxt(name, ring):
            i = cnt[name]
            cnt[name] += 1
            return ring[i % len(ring)] + (i % len(ring),)

        for blk in range(NB):
            uT, k_uT = uT_r[blk % 2]
            for ti in range(4):
                tg = blk * 4 + ti
                xt, k_xt = xt_r[tg % 2]
                add("sp", lambda e, xt=xt, tg=tg: e.dma_start(out=xt, in_=x[tg * 128:(tg + 1) * 128, :]),
                    writes=[k_xt], dma_key=("a_x", tg % 2))
                rms_rstd(xt, [k_xt], D, sq_t, k_sq, ssq_t, k_ssq, rstd_t, k_rstd)
                add("dve", lambda e, xt=xt: e.scalar_tensor_tensor(out=ub_t, in0=xt, scalar=rstd_t[:, 0:1], in1=nmw_t,
                                                                   op0=ALU.mult, op1=ALU.mult),
                    reads=[k_xt, k_rstd, k_nmw], writes=[k_ub])
                pb = 6 + (tg % 2)
                for dc in range(8):
                    add("pe", lambda e, dc=dc, pb=pb: e.transpose(bank_bf(pb)[:, dc * 128:(dc + 1) * 128],
                                                                  ub_t[:, dc * 128:(dc + 1) * 128], ident_b),
                        reads=[k_ub, k_idb], writes=[PK[pb]])
                add("act", lambda e, uT=uT, ti=ti, pb=pb: e.copy(
                    uT[:, :, ti * 128:(ti + 1) * 128], bank_bf(pb).rearrange("p (a b) -> p a b", a=8)),
                    reads=[PK[pb]], writes=[k_uT])
            for (c0, width, mode, rel) in segs:
                ws, k_ws, wi = nxt("ws", wseg_r)
                add("sp", lambda e, ws=ws, c0=c0, width=width: e.dma_start(out=ws[:, :, 0:width], in_=w_in_b[:, :, c0:c0 + width]),
                    writes=[k_ws], dma_key=("a_ws", wi))
                if mode in ("xbc", "g"):
                    for c4 in range(4):
                        pbk = cnt["ps"] % 6
                        cnt["ps"] += 1
                        for dc in range(8):
                            add("pe", lambda e, ws=ws, dc=dc, c4=c4, pbk=pbk, uT=uT: e.matmul(
                                bank(pbk), ws[:, dc, c4 * 128:(c4 + 1) * 128], uT[:, dc, :], start=(dc == 0), stop=(dc == 7)),
                                reads=[k_ws, k_uT], writes=[PK[pbk]])
                        frow = rel + c4 * 128
                        if mode == "xbc":
                            of, k_of, oi = nxt("of", of_r)
                            add("act", lambda e, of=of, pbk=pbk: e.copy(of, bank(pbk)), reads=[PK[pbk]], writes=[k_of])
                            add("pool", lambda e, of=of, frow=frow, blk=blk: e.dma_start(
                                out=xbc_s[frow:frow + 128, blk * 512:(blk + 1) * 512], in_=of), reads=[k_of], dma_key=("a_of", oi))
                        else:
                            ob, k_ob, oi = nxt("ob", ob_r)
                            add("act", lambda e, ob=ob, pbk=pbk: e.activation(out=ob, in_=bank(pbk), func=AF.Sigmoid),
                                reads=[PK[pbk]], writes=[k_ob])
                            add("pool", lambda e, ob=ob, frow=frow, blk=blk: e.dma_start(
                                out=g_s[frow:frow + 128, blk * 512:(blk + 1) * 512], in_=ob), reads=[k_ob], dma_key=("a_ob", oi))
                    continue
                for ti in range(4):
                    tg = blk * 4 + ti
                    t0 = tg * 128
                    pbk = cnt["ps"] % 6
                    cnt["ps"] += 1
                    ps = bank(pbk)[:, 0:width]
                    for dc in range(8):
                        add("pe", lambda e, ws=ws, dc=dc, ti=ti, ps=ps, uT=uT, width=width: e.matmul(
                            ps, uT[:, dc, ti * 128:(ti + 1) * 128], ws[:, dc, 0:width], start=(dc == 0), stop=(dc == 7)),
                            reads=[k_ws, k_uT], writes=[PK[pbk]])
                    if mode == "z":
                        ob, k_ob, oi = nxt("ob", ob_r)
                        add("act", lambda e, ob=ob, ps=ps: e.activation(out=ob, in_=ps, func=AF.Silu), reads=[PK[pbk]], writes=[k_ob])
                        add("pool", lambda e, ob=ob, t0=t0, rel=rel: e.dma_start(out=z_s[t0:t0 + 128, rel:rel + 512], in_=ob),
                            reads=[k_ob], dma_key=("a_ob", oi))
                    elif mode == "v":
                        ob, k_ob, oi = nxt("ob", ob_r)
                        add("act", lambda e, ob=ob, ps=ps, width=width: e.copy(ob[:, 0:width], ps), reads=[PK[pbk]], writes=[k_ob])
                        add("pool", lambda e, ob=ob, t0=t0, rel=rel, width=width: e.dma_start(
                            out=v_s[t0:t0 + 128, rel:rel + width], in_=ob[:, 0:width]), reads=[k_ob], dma_key=("a_ob", oi))
                    elif mode == "dt":
                        dtv, k_dtv, di = nxt("dtv", dtv_r)
                        add("dve", lambda e, ps=ps: e.tensor_tensor(dt1_t, ps, dtb_t, ALU.add), reads=[PK[pbk], k_dtb], writes=[k_dt1])
                        add("act", lambda e: e.activation(out=dt1_t, in_=dt1_t, func=AF.Exp), reads=[k_dt1], writes=[k_dt1])
                        add("dve", lambda e: e.tensor_single_scalar(dt1_t, dt1_t, 1.0, ALU.add), reads=[k_dt1], writes=[k_dt1])
                        add("act", lambda e, dtv=dtv: e.activation(out=dtv, in_=dt1_t, func=AF.Ln), reads=[k_dt1], writes=[k_dtv])
                        add("pool", lambda e, dtv=dtv, t0=t0: e.dma_start(out=dt_s[t0:t0 + 128, :], in_=dtv), reads=[k_dtv], dma_key=("a_dtv", di))
                    elif mode in ("q", "k"):
                        nh = width // 64
                        nwt, k_nw = (qnw_t, k_qnw) if mode == "q" else (knw_t, k_knw)
                        dst = q_s if mode == "q" else k_s
                        rms_rstd(ps, [PK[pbk]], width, sq_t, k_sq, ssq_t, k_ssq, rstd_t, k_rstd, nh=nh, hd=64)
                        qv = qn_t[:, 0:width].rearrange("p (h d) -> p h d", h=nh)
                        add("dve", lambda e, ps=ps, qv=qv, nh=nh: e.tensor_tensor(
                            qv, ps.rearrange("p (h d) -> p h d", h=nh), bc(rstd_t[:, 0:nh], [128, nh, 64], 2), ALU.mult),
                            reads=[PK[pbk], k_rstd], writes=[k_qn])
                        add("dve", lambda e, qv=qv, nh=nh, nwt=nwt: e.tensor_tensor(qv, qv, bc(nwt, [128, nh, 64], 1), ALU.mult),
                            reads=[k_qn, k_nw], writes=[k_qn])
                        ob, k_ob, oi = nxt("ob", ob_r)
                        obv = ob[:, 0:width].rearrange("p (h d) -> p h d", h=nh)
                        add("act", lambda e, ob=ob, width=width: e.copy(ob[:, 0:width], qn_t[:, 0:width]), reads=[k_qn], writes=[k_ob])
                        cosb = bc(cs_t[:, tg, :], [128, nh, 8], 1)
                        sinb = bc(sn_t[:, tg, :], [128, nh, 8], 1)
                        a1 = r1_t[:, 0:nh, :]
                        a2 = r2_t[:, 0:nh, :]
                        add("dve", lambda e, qv=qv, a1=a1, cosb=cosb: e.tensor_tensor(a1, qv[:, :, 0:8], cosb, ALU.mult), reads=[k_qn, k_cs], writes=[k_r1])
                        add("dve", lambda e, qv=qv, a2=a2, sinb=sinb: e.tensor_tensor(a2, qv[:, :, 8:16], sinb, ALU.mult), reads=[k_qn, k_sn], writes=[k_r2])
                        add("dve", lambda e, obv=obv, a1=a1, a2=a2: e.tensor_tensor(obv[:, :, 0:8], a1, a2, ALU.subtract),
                            reads=[k_r1, k_r2, k_ob], writes=[k_ob])
                        add("dve", lambda e, qv=qv, a1=a1, cosb=cosb: e.tensor_tensor(a1, qv[:, :, 8:16], cosb, ALU.mult), reads=[k_qn, k_cs, k_ob], writes=[k_r1])
                        add("dve", lambda e, qv=qv, a2=a2, sinb=sinb: e.tensor_tensor(a2, qv[:, :, 0:8], sinb, ALU.mult), reads=[k_qn, k_sn, k_ob], writes=[k_r2])
                        add("dve", lambda e, obv=obv, a1=a1, a2=a2: e.tensor_tensor(obv[:, :, 8:16], a1, a2, ALU.add),
                            reads=[k_r1, k_r2, k_ob], writes=[k_ob])
                        add("pool", lambda e, ob=ob, t0=t0, rel=rel, width=width, dst=dst: e.dma_start(
                            out=dst[t0:t0 + 128, rel:rel + width], in_=ob[:, 0:width]), reads=[k_ob], dma_key=("a_ob", oi))
                    elif mode == "mq":
                        rms_rstd(ps, [PK[pbk]], 384, sq_t, k_sq, ssq_t, k_ssq, rstd_t, k_rstd, nh=2, hd=192)
                        qv = qn_t[:, 0:384].rearrange("p (h d) -> p h d", h=2)
                        add("dve", lambda e, ps=ps, qv=qv: e.tensor_tensor(
                            qv, ps.rearrange("p (h d) -> p h d", h=2), bc(rstd_t[:, 0:2], [128, 2, 192], 2), ALU.mult),
                            reads=[PK[pbk], k_rstd], writes=[k_qn])
                        ob, k_ob, oi = nxt("ob", ob_r)
                        add("dve", lambda e, ob=ob, qv=qv: e.tensor_tensor(
                            ob[:, 0:384].rearrange("p (h d) -> p h d", h=2), qv, bc(mqnw_t, [128, 2, 192], 1), ALU.mult),
                            reads=[k_qn, k_mqnw], writes=[k_ob])
                        add("pool", lambda e, ob=ob, t0=t0, rel=rel: e.dma_start(out=mq_s[t0:t0 + 128, rel:rel + 384], in_=ob[:, 0:384]),
                            reads=[k_ob], dma_key=("a_ob", oi))
        S_.barrier("a")

        AR.reset()
        stT, k_stT = AR.get([128, 16, 64], F32)
        stB, k_stB = AR.get([128, 16, 64], BF16)
        snw_t, k_snw = AR.get([128, D], F32)
        add("sp", lambda e: e.dma_start(out=snw_t, in_=snw), writes=[k_snw], dma_key="b_c0")
        add("dve", lambda e: e.memset(stT, 0.0), writes=[k_stT])
        add("pool", lambda e: e.memset(stB, 0.0), writes=[k_stB])
        raw_r = [AR.get([128, 16, 131], F32) for _ in range(2)]
        dtl_r = [AR.get([128, 16], F32) for _ in range(2)]
        zl_r = [AR.get([128, D], BF16) for _ in range(2)]
        cv_t, k_cv = AR.get([128, 16, 128], F32)
        xbT, k_xbT = AR.get([128, 16, 128], BF16)
        xs_t, k_xs = AR.get([128, 16, 64], BF16)
        Bt_t, k_Bt = AR.get([128, 4, 128], BF16)
        da_t, k_da = AR.get([128, 16], F32)
        acol, k_acol = AR.get([128, 16], F32)
        X_t, k_X = AR.get([128, 16, 128], F32)
        arow, k_arow = AR.get([128, 16, 128], F32)
        E_t, k_E = AR.get([128, 16, 128], F32)
        eA_t, k_eA = AR.get([128, 16, 128], F32)
        W_t, k_W = AR.get([128, 16, 128], BF16)
        CTp, k_CTp = AR.get([128, 16, 128], BF16)
        xdt, k_xdt = AR.get([128, 16, 64], BF16)
        xdd, k_xdd = AR.get([128, 16, 64], BF16)
        dec, k_dec = AR.get([128, 16], F32)
        dtd, k_dtd = AR.get([128, 16], F32)
        y_t, k_y = AR.get([128, D], F32)
        ysq, k_ysq = AR.get([128, D], F32)
        gss, k_gss = AR.get([128, 16], F32)
        grs, k_grs = AR.get([128, 16], F32)
        yb_r = [AR.get([128, D], BF16) for _ in range(2)]
        xbc_v = xbc_s.rearrange("(c p) t -> p c t", p=128)
        for c in range(NT):
            t0 = c * 128
            raw, k_raw = raw_r[c % 2]
            dtl, k_dtl = dtl_r[c % 2]
            zl, k_zl = zl_r[c % 2]
            if c == 0:
                add("pool", lambda e, raw=raw: e.memset(raw[:, :, 0:3], 0.0), writes=[k_raw])
                add("sp", lambda e, raw=raw: e.dma_start(out=raw[:, :, 3:131], in_=xbc_v[:, :, 0:128]), writes=[k_raw], dma_key=("b_raw", c % 2))
            else:
                add("sp", lambda e, raw=raw, t0=t0: e.dma_start(out=raw, in_=xbc_v[:, :, t0 - 3:t0 + 128]), writes=[k_raw], dma_key=("b_raw", c % 2))
            add("sp", lambda e, dtl=dtl, t0=t0: e.dma_start(out=dtl, in_=dt_s[t0:t0 + 128, :]), writes=[k_dtl], dma_key=("b_dt", c % 2))
            add("sp", lambda e, zl=zl, t0=t0: e.dma_start(out=zl, in_=z_s[t0:t0 + 128, :]), writes=[k_zl], dma_key=("b_z", c % 2))
            for cc in range(16):
                en = "dve"
                kc = ("cv", cc)
                add(en, lambda e, raw=raw, cc=cc: e.tensor_scalar(cv_t[:, cc, :], raw[:, cc, 3:131], cw_t[:, cc, 3:4], cb_t[:, cc:cc + 1],
                                                                  ALU.mult, ALU.add), reads=[k_raw, k_cw, k_cb], writes=[kc])
                for kq in (2, 1, 0):
                    add(en, lambda e, raw=raw, cc=cc, kq=kq: e.scalar_tensor_tensor(
                        out=cv_t[:, cc, :], in0=raw[:, cc, kq:kq + 128], scalar=cw_t[:, cc, kq:kq + 1], in1=cv_t[:, cc, :],
                        op0=ALU.mult, op1=ALU.add), reads=[k_raw, k_cw, kc], writes=[kc])
            add("act", lambda e: e.activation(out=xbT, in_=cv_t, func=AF.Silu), reads=[("cv", cc) for cc in range(16)], writes=[k_xbT])
            for cc in range(8):
                add("pe", lambda e, cc=cc: e.transpose(bank_bf(0)[:, cc * 128:(cc + 1) * 128], xbT[:, cc, :], ident_b),
                    reads=[k_xbT, k_idb], writes=[PK[0]])
            for g in range(4):
                add("pe", lambda e, g=g: e.transpose(bank_bf(1)[:, g * 128:(g + 1) * 128], xbT[:, 8 + g, :], ident_b),
                    reads=[k_xbT, k_idb], writes=[PK[1]])
            add("act", lambda e: e.copy(xs_t.rearrange("p a b -> p (a b)"), bank_bf(0)), reads=[PK[0]], writes=[k_xs])
            add("act", lambda e: e.copy(Bt_t.rearrange("p a b -> p (a b)"), bank_bf(1)[:, 0:512]), reads=[PK[1]], writes=[k_Bt])
            add("dve", lambda e, dtl=dtl: e.tensor_tensor(da_t, dtl, aneg, ALU.mult), reads=[k_dtl, k_aneg], writes=[k_da])
            add("pe", lambda e: e.matmul(bank(2)[:, 0:16], mge_f, da_t, start=True, stop=True), reads=[k_mgef, k_da], writes=[PK[2]])
            add("act", lambda e: e.copy(acol, bank(2)[:, 0:16]), reads=[PK[2]], writes=[k_acol])
            add("dve", lambda e: e.tensor_tensor(X_t, bc(da_t, [128, 16, 128], 2), bc(mge_f, [128, 16, 128], 1), ALU.mult),
                reads=[k_da, k_mgef], writes=[k_X])
            for q4 in range(4):
                pbk = 3 + (q4 % 2)
                add("pe", lambda e, q4=q4, pbk=pbk: e.matmul(bank(pbk), ones_f, X_t[:, q4 * 4:(q4 + 1) * 4, :].rearrange("p a b -> p (a b)"),
                                                             start=True, stop=True), reads=[k_onesf, k_X], writes=[PK[pbk]])
                add("act", lambda e, q4=q4, pbk=pbk: e.copy(arow[:, q4 * 4:(q4 + 1) * 4, :].rearrange("p a b -> p (a b)"), bank(pbk)),
                    reads=[PK[pbk]], writes=[k_arow])
            add("dve", lambda e: e.tensor_tensor(E_t, arow, bc(negm, [128, 16, 128], 1), ALU.add), reads=[k_arow, k_negm], writes=[k_E])
            add("dve", lambda e: e.tensor_tensor(E_t, E_t, bc(acol, [128, 16, 128], 2), ALU.subtract), reads=[k_E, k_acol], writes=[k_E])
            add("act", lambda e: e.activation(out=E_t, in_=E_t, func=AF.Exp), reads=[k_E], writes=[k_E])
            add("act", lambda e: e.activation(out=eA_t, in_=arow, func=AF.Exp), reads=[k_arow], writes=[k_eA])
            for g in range(4):
                add("pe", lambda e, g=g: e.matmul(bank(5)[:, g * 128:(g + 1) * 128], xbT[:, 8 + g, :], xbT[:, 12 + g, :], start=True, stop=True),
                    reads=[k_xbT], writes=[PK[5]])
            for g in range(4):
                add("dve", lambda e, g=g: e.tensor_tensor(W_t[:, 4 * g:4 * g + 4, :], E_t[:, 4 * g:4 * g + 4, :],
                                                          bc(bank(5)[:, g * 128:(g + 1) * 128], [128, 4, 128], 1), ALU.mult),
                    reads=[k_E, PK[5]], writes=[k_W])
                add("pool", lambda e, g=g: e.tensor_tensor(CTp[:, 4 * g:4 * g + 4, :], eA_t[:, 4 * g:4 * g + 4, :],
                                                           bc(xbT[:, 12 + g, :], [128, 4, 128], 1), ALU.mult),
                    reads=[k_eA, k_xbT], writes=[k_CTp])
            add("dve", lambda e, dtl=dtl: e.tensor_tensor(xdt, xs_t, bc(dtl, [128, 16, 64], 2), ALU.mult), reads=[k_xs, k_dtl], writes=[k_xdt])
            for hd in range(16):
                pbk = 6 + hd // 8
                o = bank(pbk)[:, (hd % 8) * 64:(hd % 8 + 1) * 64]
                add("pe", lambda e, hd=hd, o=o: e.matmul(o, W_t[:, hd, :], xdt[:, hd, :], start=True, stop=False),
                    reads=[k_W, k_xdt], writes=[PK[pbk]])
                add("pe", lambda e, hd=hd, o=o: e.matmul(o, CTp[:, hd, :], stB[:, hd, :], start=False, stop=True),
                    reads=[k_CTp, k_stB], writes=[PK[pbk]])
            add("dve", lambda e: e.tensor_tensor(dec, arow[:, :, 127], acol, ALU.subtract), reads=[k_arow, k_acol], writes=[k_dec])
            add("act", lambda e: e.activation(out=dec, in_=dec, func=AF.Exp), reads=[k_dec], writes=[k_dec])
            add("dve", lambda e, dtl=dtl: e.tensor_tensor(dtd, dtl, dec, ALU.mult), reads=[k_dtl, k_dec], writes=[k_dtd])
            add("dve", lambda e: e.tensor_tensor(xdd, xs_t, bc(dtd, [128, 16, 64], 2), ALU.mult), reads=[k_xs, k_dtd], writes=[k_xdd])
            for hd in range(16):
                pbk = 3 + hd // 8
                o = bank(pbk)[:, (hd % 8) * 64:(hd % 8 + 1) * 64]
                add("pe", lambda e, hd=hd, o=o: e.matmul(o, Bt_t[:, hd // 4, :], xdd[:, hd, :], start=True, stop=True),
                    reads=[k_Bt, k_xdd], writes=[PK[pbk]])
            add("dve", lambda e: e.tensor_tensor(y_t.rearrange("p (a b) -> p a b", a=16), xs_t, bc(sd_t, [128, 16, 64], 2), ALU.mult),
                reads=[k_xs, k_sd], writes=[k_y])
            add("dve", lambda e: e.tensor_tensor(y_t[:, 0:512], y_t[:, 0:512], bank(6), ALU.add), reads=[k_y, PK[6]], writes=[k_y])
            add("dve", lambda e: e.tensor_tensor(y_t[:, 512:1024], y_t[:, 512:1024], bank(7), ALU.add), reads=[k_y, PK[7]], writes=[k_y])
            add("dve", lambda e, zl=zl: e.tensor_tensor(y_t, y_t, zl, ALU.mult), reads=[k_y, k_zl], writes=[k_y])
            rms_rstd(y_t, [k_y], D, ysq, k_ysq, gss, k_gss, grs, k_grs, nh=4, hd=256)
            add("dve", lambda e: e.tensor_tensor(y_t.rearrange("p (a b) -> p a b", a=4), y_t.rearrange("p (a b) -> p a b", a=4),
                                                 bc(grs[:, 0:4], [128, 4, 256], 2), ALU.mult), reads=[k_y, k_grs], writes=[k_y])
            yb, k_yb = yb_r[c % 2]
            add("dve", lambda e, yb=yb: e.tensor_tensor(yb, y_t, snw_t, ALU.mult), reads=[k_y, k_snw], writes=[k_yb])
            add("pool", lambda e, yb=yb, t0=t0: e.dma_start(out=yssd_s[t0:t0 + 128, :], in_=yb), reads=[k_yb], dma_key=("b_yb", c % 2))
            add("dve", lambda e: e.tensor_tensor(stT, stT, bc(eA_t[:, :, 127], [128, 16, 64], 2), ALU.mult), reads=[k_stT, k_eA], writes=[k_stT])
            add("dve", lambda e: e.tensor_tensor(stT[:, 0:8, :].rearrange("p a b -> p (a b)"), stT[:, 0:8, :].rearrange("p a b -> p (a b)"),
                                                 bank(3), ALU.add), reads=[k_stT, PK[3]], writes=[k_stT])
            add("dve", lambda e: e.tensor_tensor(stT[:, 8:16, :].rearrange("p a b -> p (a b)"), stT[:, 8:16, :].rearrange("p a b -> p (a b)"),
                                                 bank(4), ALU.add), reads=[k_stT, PK[4]], writes=[k_stT])
            add("act", lambda e: e.copy(stB, stT), reads=[k_stT], writes=[k_stB])
        S_.barrier("b")

        AR.reset()
        Qb_r = [AR.get([128, 256], BF16) for _ in range(2)]
        Kb_r = [AR.get([128, 256], BF16) for _ in range(2)]
        Vb_r = [AR.get([128, 256], BF16) for _ in range(2)]
        QT_r = [AR.get([128, 2, 128], BF16) for _ in range(2)]
        KT_r = [AR.get([128, 2, 128], BF16) for _ in range(2)]
        Va_r = [AR.get([128, 4, 65], BF16) for _ in range(2)]
        P_r = [AR.get([128, 128], BF16) for _ in range(4)]
        od_r = [AR.get([128, 260], F32) for _ in range(2)]
        for i in range(2):
            add("pool", lambda e, i=i: e.memset(Va_r[i][0][:, :, 64:65], 1.0), writes=[("va1", i)])
        bi = 0
        pi = 0
        for gi, dil in enumerate(DILS):
            nb = S // dil // 128
            for r in range(dil):
                for n in range(nb):
                    rows = slice(r + n * 128 * dil, r + n * 128 * dil + 127 * dil + 1, dil)
                    cols = slice(gi * 256, (gi + 1) * 256)
                    Qb, k_Qb = Qb_r[bi % 2]
                    Kb, k_Kb = Kb_r[bi % 2]
                    Vb, k_Vb = Vb_r[bi % 2]
                    QT, k_QT = QT_r[bi % 2]
                    KT, k_KT = KT_r[bi % 2]
                    Va, k_Va = Va_r[bi % 2]
                    KTp, k_KTp = KT_r[(bi + 1) % 2]
                    Vap, k_Vap = Va_r[(bi + 1) % 2]
                    add("sp", lambda e, Qb=Qb, rows=rows, cols=cols: e.dma_start(out=Qb, in_=q_s[rows, cols]), writes=[k_Qb], dma_key=("c_q", bi % 2))
                    add("sp", lambda e, Kb=Kb, rows=rows, cols=cols: e.dma_start(out=Kb, in_=k_s[rows, cols]), writes=[k_Kb], dma_key=("c_k", bi % 2))
                    add("sp", lambda e, Vb=Vb, rows=rows, cols=cols: e.dma_start(out=Vb, in_=v_s[rows, cols]), writes=[k_Vb], dma_key=("c_v", bi % 2))
                    for hp in range(2):
                        add("pe", lambda e, hp=hp, Qb=Qb: e.transpose(bank_bf(0)[:, hp * 128:(hp + 1) * 128], Qb[:, hp * 128:(hp + 1) * 128], ident_b),
                            reads=[k_Qb, k_idb], writes=[PK[0]])
                        add("pe", lambda e, hp=hp, Kb=Kb: e.transpose(bank_bf(0)[:, 256 + hp * 128:256 + (hp + 1) * 128], Kb[:, hp * 128:(hp + 1) * 128], ident_b),
                            reads=[k_Kb, k_idb], writes=[PK[0]])
                    add("act", lambda e, QT=QT: e.copy(QT.rearrange("p a b -> p (a b)"), bank_bf(0)[:, 0:256]), reads=[PK[0]], writes=[k_QT])
                    add("act", lambda e, KT=KT: e.copy(KT.rearrange("p a b -> p (a b)"), bank_bf(0)[:, 256:512]), reads=[PK[0]], writes=[k_KT])
                    add("dve", lambda e, Va=Va, Vb=Vb: e.tensor_copy(Va[:, :, 0:64], Vb.rearrange("p (h d) -> p h d", h=4)),
                        reads=[k_Vb, ("va1", bi % 2)], writes=[k_Va])
                    kts = ([("prev", KTp, k_KTp, Vap, k_Vap)] if n > 0 else []) + [("cur", KT, k_KT, Va, k_Va)]
                    opb = 3 + (bi % 2)
                    for h in range(4):
                        hp, hh = h // 2, h % 2
                        for ki, (which, kt_, k_kt, va_, k_va) in enumerate(kts):
                            spb = 1 + (pi % 2)
                            sslot = bank(spb)[:, ((pi // 2) % 4) * 128:((pi // 2) % 4 + 1) * 128]
                            P, k_P = P_r[pi % 4]
                            msk, k_msk = (mge_b, k_mgeb) if which == "cur" else (mle_b, k_mleb)
                            add("pe", lambda e, sslot=sslot, kt_=kt_, QT=QT, hp=hp, hh=hh: e.matmul(
                                sslot, kt_[hh * 64:(hh + 1) * 64, hp, :], QT[hh * 64:(hh + 1) * 64, hp, :], start=True, stop=True),
                                reads=[k_kt, k_QT], writes=[("pss", spb, (pi // 2) % 4)])
                            add("act", lambda e, P=P, sslot=sslot: e.activation(out=P, in_=sslot, func=AF.Exp, scale=0.125),
                                reads=[("pss", spb, (pi // 2) % 4)], writes=[k_P])
                            add("dve", lambda e, P=P, msk=msk: e.tensor_tensor(P, P, msk, ALU.mult), reads=[k_P, k_msk], writes=[k_P])
                            add("pe", lambda e, P=P, va_=va_, h=h, opb=opb, ki=ki, nk=len(kts): e.matmul(
                                bank(opb)[:, h * 65:(h + 1) * 65], P, va_[:, h, :], start=(ki == 0), stop=(ki == nk - 1)),
                                reads=[k_P, k_va, ("va1", 0), ("va1", 1)], writes=[PK[opb]])
                            pi += 1
                    od, k_od = od_r[bi % 2]
                    add("act", lambda e, od=od, opb=opb: e.copy(od, bank(opb)[:, 0:260]), reads=[PK[opb]], writes=[k_od])
                    add("pool", lambda e, od=od, rows=rows, gi=gi: e.dma_start(out=od_s[gi, rows, :], in_=od), reads=[k_od], dma_key=("c_od", bi % 2))
                    bi += 1
        S_.barrier("c")

        AR.reset()
        mnw_t, k_mnw = AR.get([128, D], F32)
        mknw_t, k_mknw = AR.get([128, 192], F32)
        wkv_t, k_wkv = AR.get([128, 8, 1536], BF16)
        add("sp", lambda e: e.dma_start(out=mnw_t, in_=mnw), writes=[k_mnw], dma_key="m_c0")
        add("sp", lambda e: e.dma_start(out=mknw_t, in_=mknw), writes=[k_mknw], dma_key="m_c1")
        add("pool", lambda e: e.dma_start(out=wkv_t, in_=wkv), writes=[k_wkv], dma_key="m_c2")
        mt_t, k_mt = AR.get([128, D], F32)
        msq, k_msq = AR.get([128, D], F32)
        mss, k_mss = AR.get([128, 16], F32)
        mrs, k_mrs = AR.get([128, 16], F32)
        mub, k_mub = AR.get([128, D], BF16)
        memT, k_memT = AR.get([128, 8, 256], BF16)
        mkv, k_mkv = AR.get([128, 2, 1536], F32)
        mkn, k_mkn = AR.get([128, 2, 768], BF16)
        KmA, k_KmA = AR.get([128, 4, 256], BF16)
        KmB, k_KmB = AR.get([128, 4, 256], BF16)
        VmA, k_VmA = AR.get([128, 2, 4, 193], BF16)
        for mt in range(2):
            add("sp", lambda e, mt=mt: e.dma_start(out=mt_t, in_=mem[mt * 128:(mt + 1) * 128, :]), writes=[k_mt], dma_key="m_mem")
            rms_rstd(mt_t, [k_mt], D, msq, k_msq, mss, k_mss, mrs, k_mrs)
            add("dve", lambda e: e.scalar_tensor_tensor(out=mub, in0=mt_t, scalar=mrs[:, 0:1], in1=mnw_t, op0=ALU.mult, op1=ALU.mult),
                reads=[k_mt, k_mrs, k_mnw], writes=[k_mub])
            for dc in range(8):
                add("pe", lambda e, dc=dc: e.transpose(bank_bf(0)[:, dc * 128:(dc + 1) * 128], mub[:, dc * 128:(dc + 1) * 128], ident_b),
                    reads=[k_mub, k_idb], writes=[PK[0]])
            add("act", lambda e, mt=mt: e.copy(memT[:, :, mt * 128:(mt + 1) * 128], bank_bf(0).rearrange("p (a b) -> p a b", a=8)),
                reads=[PK[0]], writes=[k_memT])
        for mt in range(2):
            for cs3 in range(3):
                pbk = 1 + (mt * 3 + cs3) % 2
                for dc in range(8):
                    add("pe", lambda e, mt=mt, cs3=cs3, dc=dc, pbk=pbk: e.matmul(
                        bank(pbk), memT[:, dc, mt * 128:(mt + 1) * 128], wkv_t[:, dc, cs3 * 512:(cs3 + 1) * 512], start=(dc == 0), stop=(dc == 7)),
                        reads=[k_memT, k_wkv], writes=[PK[pbk]])
                add("act", lambda e, mt=mt, cs3=cs3, pbk=pbk: e.copy(mkv[:, mt, cs3 * 512:(cs3 + 1) * 512], bank(pbk)), reads=[PK[pbk]], writes=[k_mkv])
        for mt in range(2):
            src = mkv[:, mt, 0:768]
            rms_rstd(src, [k_mkv], 768, msq, k_msq, mss, k_mss, mrs, k_mrs, nh=4, hd=192)
            add("dve", lambda e, src=src: e.tensor_tensor(msq[:, 0:768].rearrange("p (h d) -> p h d", h=4), src.rearrange("p (h d) -> p h d", h=4),
                                                          bc(mrs[:, 0:4], [128, 4, 192], 2), ALU.mult), reads=[k_mkv, k_mrs], writes=[k_msq])
            add("dve", lambda e, mt=mt: e.tensor_tensor(mkn[:, mt, :].rearrange("p (h d) -> p h d", h=4), msq[:, 0:768].rearrange("p (h d) -> p h d", h=4),
                                                        bc(mknw_t, [128, 4, 192], 1), ALU.mult), reads=[k_msq, k_mknw], writes=[k_mkn])
            for h in range(4):
                add("pe", lambda e, mt=mt, h=h: e.transpose(bank_bf(3)[:, h * 128:(h + 1) * 128], mkn[:, mt, h * 192:h * 192 + 128], ident_b),
                    reads=[k_mkn, k_idb], writes=[PK[3]])
                add("pe", lambda e, mt=mt, h=h: e.transpose(bank_bf(4)[0:64, h * 128:(h + 1) * 128], mkn[:, mt, h * 192 + 128:(h + 1) * 192], ident_b),
                    reads=[k_mkn, k_idb], writes=[PK[4]])
            add("act", lambda e, mt=mt: e.copy(KmA[:, :, mt * 128:(mt + 1) * 128], bank_bf(3)[:, 0:512].rearrange("p (a b) -> p a b", a=4)),
                reads=[PK[3]], writes=[k_KmA])
            add("act", lambda e, mt=mt: e.copy(KmB[0:64, :, mt * 128:(mt + 1) * 128], bank_bf(4)[0:64, 0:512].rearrange("p (a b) -> p a b", a=4)),
                reads=[PK[4]], writes=[k_KmB])
            add("dve", lambda e, mt=mt: e.tensor_copy(VmA[:, mt, :, 0:192], mkv[:, mt, 768:1536].rearrange("p (h d) -> p h d", h=4)),
                reads=[k_mkv], writes=[k_VmA])
            add("pool", lambda e, mt=mt: e.memset(VmA[:, mt, :, 192:193], 1.0), writes=[("vm1", mt)])
        mq_r = [AR.get([128, 768], BF16) for _ in range(2)]
        mqA, k_mqA = AR.get([128, 4, 128], BF16)
        mqB, k_mqB = AR.get([128, 4, 128], BF16)
        Pm_r = [AR.get([128, 128], BF16) for _ in range(4)]
        rdn, k_rdn = AR.get([128, 4], F32)
        ym_r = [AR.get([128, 768], BF16) for _ in range(2)]
        pi = 0
        for tg in range(NT):
            t0 = tg * 128
            mqt, k_mqt = mq_r[tg % 2]
            add("sp", lambda e, mqt=mqt, t0=t0: e.dma_start(out=mqt, in_=mq_s[t0:t0 + 128, :]), writes=[k_mqt], dma_key=("m_mq", tg % 2))
            for h in range(4):
                add("pe", lambda e, mqt=mqt, h=h: e.transpose(bank_bf(3)[:, h * 128:(h + 1) * 128], mqt[:, h * 192:h * 192 + 128], ident_b),
                    reads=[k_mqt, k_idb], writes=[PK[3]])
                add("pe", lambda e, mqt=mqt, h=h: e.transpose(bank_bf(4)[0:64, h * 128:(h + 1) * 128], mqt[:, h * 192 + 128:(h + 1) * 192], ident_b),
                    reads=[k_mqt, k_idb], writes=[PK[4]])
            add("act", lambda e: e.copy(mqA.rearrange("p a b -> p (a b)"), bank_bf(3)[:, 0:512]), reads=[PK[3]], writes=[k_mqA])
            add("act", lambda e: e.copy(mqB[0:64].rearrange("p a b -> p (a b)"), bank_bf(4)[0:64, 0:512]), reads=[PK[4]], writes=[k_mqB])
            ob0 = 6
            for h in range(4):
                for mt in range(2):
                    spb = 1 + (pi % 2)
                    sslot = bank(spb)[:, ((pi // 2) % 4) * 128:((pi // 2) % 4 + 1) * 128]
                    ksl = ("pss", spb, (pi // 2) % 4)
                    P, k_P = Pm_r[pi % 4]
                    add("pe", lambda e, sslot=sslot, h=h, mt=mt: e.matmul(sslot, KmA[:, h, mt * 128:(mt + 1) * 128], mqA[:, h, :], start=True, stop=False),
                        reads=[k_KmA, k_mqA], writes=[ksl])
                    add("pe", lambda e, sslot=sslot, h=h, mt=mt: e.matmul(sslot, KmB[0:64, h, mt * 128:(mt + 1) * 128], mqB[0:64, h, :], start=False, stop=True),
                        reads=[k_KmB, k_mqB], writes=[ksl])
                    add("act", lambda e, P=P, sslot=sslot: e.activation(out=P, in_=sslot, func=AF.Exp, scale=192.0 ** -0.5), reads=[ksl], writes=[k_P])
                    pbk = ob0 + h // 2
                    add("pe", lambda e, P=P, h=h, mt=mt, pbk=pbk: e.matmul(bank(pbk)[:, (h % 2) * 256:(h % 2) * 256 + 193], P, VmA[:, mt, h, :],
                                                                           start=(mt == 0), stop=(mt == 1)),
                        reads=[k_P, k_VmA, ("vm1", 0), ("vm1", 1)], writes=[PK[pbk]])
                    pi += 1
            ym, k_ym = ym_r[tg % 2]
            pv = bank(6, 2).rearrange("p (h c) -> p h c", h=4)
            add("dve", lambda e, pv=pv: e.tensor_copy(rdn, pv[:, :, 192]), reads=[PK[6], PK[7]], writes=[k_rdn])
            add("dve", lambda e: e.reciprocal(rdn, rdn), reads=[k_rdn], writes=[k_rdn])
            add("dve", lambda e, pv=pv, ym=ym: e.tensor_tensor(ym.rearrange("p (h d) -> p h d", h=4), pv[:, :, 0:192], bc(rdn, [128, 4, 192], 2), ALU.mult),
                reads=[PK[6], PK[7], k_rdn], writes=[k_ym])
            add("pool", lambda e, ym=ym, t0=t0: e.dma_start(out=ymem_s[t0:t0 + 128, :], in_=ym), reads=[k_ym], dma_key=("m_ym", tg % 2))
        S_.barrier("m")

        AR.reset()
        wsb_t, k_wsb = AR.get([128, 8, D], BF16)
        wdb_t, k_wdb = AR.get([128, 2, D], BF16)
        wmb_t, k_wmb = AR.get([128, 8, D], BF16)
        wo_t, k_wo = AR.get([128, 8, D], BF16)
        nfw_t, k_nfw = AR.get([128, D], F32)
        add("pool", lambda e: e.dma_start(out=wsb_t, in_=wsb), writes=[k_wsb], dma_key="g_c0")
        add("pool", lambda e: e.dma_start(out=wdb_t, in_=wdb), writes=[k_wdb], dma_key="g_c1")
        add("pool", lambda e: e.dma_start(out=wmb_t, in_=wmb), writes=[k_wmb], dma_key="g_c2")
        add("pool", lambda e: e.dma_start(out=wo_t, in_=wo), writes=[k_wo], dma_key="g_c3")
        add("sp", lambda e: e.dma_start(out=nfw_t, in_=nfw), writes=[k_nfw], dma_key="g_c4")
        ys_r = [AR.get([128, D], BF16) for _ in range(2)]
        odl_r = [AR.get([128, 3, 260], F32) for _ in range(2)]
        yml_r = [AR.get([128, 768], BF16) for _ in range(2)]
        gl_r = [AR.get([128, 24, 128], BF16) for _ in range(2)]
        xl_r = [AR.get([128, D], F32) for _ in range(2)]
        oacc, k_oacc = AR.get([128, 260], F32)
        rdd, k_rdd = AR.get([128, 4], F32)
        ydl, k_ydl = AR.get([128, 256], BF16)
        yT, k_yT = AR.get([128, 18, 128], BF16)
        gm, k_gm = AR.get([128, 3, 128], F32)
        mT, k_mT = AR.get([128, 8, 128], BF16)
        hf_r = [AR.get([128, D], F32) for _ in range(2)]
        hsq, k_hsq = AR.get([128, D], F32)
        hss, k_hss = AR.get([128, 16], F32)
        hrs, k_hrs = AR.get([128, 16], F32)
        hnb, k_hnb = AR.get([128, D], BF16)
        hT_r = [AR.get([128, 8, 128], BF16) for _ in range(2)]
        g_v = g_s.rearrange("(c p) t -> p c t", p=128)
        for tg in range(NT):
            t0 = tg * 128
            ys, k_ys = ys_r[tg % 2]
            odl, k_odl = odl_r[tg % 2]
            yml, k_yml = yml_r[tg % 2]
            gl, k_gl = gl_r[tg % 2]
            xl, k_xl = xl_r[tg % 2]
            add("sp", lambda e, ys=ys, t0=t0: e.dma_start(out=ys, in_=yssd_s[t0:t0 + 128, :]), writes=[k_ys], dma_key=("g_ys", tg % 2))
            add("sp", lambda e, odl=odl, t0=t0: e.dma_start(out=odl, in_=od_s[:, t0:t0 + 128, :].rearrange("g t c -> t g c")), writes=[k_odl], dma_key=("g_od", tg % 2))
            add("sp", lambda e, yml=yml, t0=t0: e.dma_start(out=yml, in_=ymem_s[t0:t0 + 128, :]), writes=[k_yml], dma_key=("g_ym", tg % 2))
            add("sp", lambda e, gl=gl, t0=t0: e.dma_start(out=gl, in_=g_v[:, :, t0:t0 + 128]), writes=[k_gl], dma_key=("g_gl", tg % 2))
            add("sp", lambda e, xl=xl, t0=t0: e.dma_start(out=xl, in_=x[t0:t0 + 128, :]), writes=[k_xl], dma_key=("g_xl", tg % 2))
            add("dve", lambda e, odl=odl: e.tensor_tensor(oacc, odl[:, 0, :], odl[:, 1, :], ALU.add), reads=[k_odl], writes=[k_oacc])
            add("dve", lambda e, odl=odl: e.tensor_tensor(oacc, oacc, odl[:, 2, :], ALU.add), reads=[k_odl, k_oacc], writes=[k_oacc])
            ov = oacc.rearrange("p (h c) -> p h c", h=4)
            add("dve", lambda e, ov=ov: e.tensor_copy(rdd, ov[:, :, 64]), reads=[k_oacc], writes=[k_rdd])
            add("dve", lambda e: e.reciprocal(rdd, rdd), reads=[k_rdd], writes=[k_rdd])
            add("dve", lambda e, ov=ov: e.tensor_tensor(ydl.rearrange("p (h d) -> p h d", h=4), ov[:, :, 0:64], bc(rdd, [128, 4, 64], 2), ALU.mult),
                reads=[k_oacc, k_rdd], writes=[k_ydl])
            for kc in range(8):
                add("pe", lambda e, kc=kc, ys=ys: e.transpose(bank_bf(0)[:, kc * 128:(kc + 1) * 128], ys[:, kc * 128:(kc + 1) * 128], ident_b),
                    reads=[k_ys, k_idb], writes=[PK[0]])
            add("act", lambda e: e.copy(yT[:, 0:8, :].rearrange("p a b -> p (a b)"), bank_bf(0)), reads=[PK[0]], writes=[k_yT])
            for kc in range(2):
                add("pe", lambda e, kc=kc: e.transpose(bank_bf(1)[:, kc * 128:(kc + 1) * 128], ydl[:, kc * 128:(kc + 1) * 128], ident_b),
                    reads=[k_ydl, k_idb], writes=[PK[1]])
            for h in range(4):
                add("pe", lambda e, h=h, yml=yml: e.transpose(bank_bf(1)[:, (2 + h) * 128:(3 + h) * 128], yml[:, h * 192:h * 192 + 128], ident_b),
                    reads=[k_yml, k_idb], writes=[PK[1]])
                add("pe", lambda e, h=h, yml=yml: e.transpose(bank_bf(2)[0:64, h * 128:(h + 1) * 128], yml[:, h * 192 + 128:(h + 1) * 192], ident_b),
                    reads=[k_yml, k_idb], writes=[PK[2]])
            add("act", lambda e: e.copy(yT[:, 8:10, :].rearrange("p a b -> p (a b)"), bank_bf(1)[:, 0:256]), reads=[PK[1]], writes=[k_yT])
            for h in range(4):
                add("act", lambda e, h=h: e.copy(yT[:, 10 + 2 * h, :], bank_bf(1)[:, (2 + h) * 128:(3 + h) * 128]), reads=[PK[1]], writes=[k_yT])
                add("act", lambda e, h=h: e.copy(yT[0:64, 11 + 2 * h, :], bank_bf(2)[0:64, h * 128:(h + 1) * 128]), reads=[PK[2]], writes=[k_yT])
            for dmc in range(8):
                pbk = 3 + dmc % 2
                dsl = slice(dmc * 128, (dmc + 1) * 128)
                for kc in range(8):
                    add("pe", lambda e, kc=kc, dsl=dsl, pbk=pbk: e.matmul(bank(pbk)[:, 0:128], wsb_t[:, kc, dsl], yT[:, kc, :], start=(kc == 0), stop=(kc == 7)),
                        reads=[k_wsb, k_yT], writes=[PK[pbk]])
                for kc in range(2):
                    add("pe", lambda e, kc=kc, dsl=dsl, pbk=pbk: e.matmul(bank(pbk)[:, 128:256], wdb_t[:, kc, dsl], yT[:, 8 + kc, :], start=(kc == 0), stop=(kc == 1)),
                        reads=[k_wdb, k_yT], writes=[PK[pbk]])
                for kc in range(8):
                    if kc % 2 == 0:
                        add("pe", lambda e, kc=kc, dsl=dsl, pbk=pbk: e.matmul(bank(pbk)[:, 256:384], wmb_t[:, kc, dsl], yT[:, 10 + kc, :], start=(kc == 0), stop=False),
                            reads=[k_wmb, k_yT], writes=[PK[pbk]])
                    else:
                        add("pe", lambda e, kc=kc, dsl=dsl, pbk=pbk: e.matmul(bank(pbk)[:, 256:384], wmb_t[0:64, kc, dsl], yT[0:64, 10 + kc, :], start=False, stop=(kc == 7)),
                            reads=[k_wmb, k_yT], writes=[PK[pbk]])
                add("dve", lambda e, dmc=dmc, pbk=pbk, gl=gl: e.tensor_tensor(gm, bank(pbk)[:, 0:384].rearrange("p (a b) -> p a b", a=3), gl[:, dmc:24:8, :], ALU.mult),
                    reads=[PK[pbk], k_gl], writes=[k_gm])
                add("dve", lambda e: e.tensor_tensor(gm[:, 0, :], gm[:, 0, :], gm[:, 1, :], ALU.add), reads=[k_gm], writes=[k_gm])
                add("dve", lambda e, dmc=dmc: e.tensor_tensor(mT[:, dmc, :], gm[:, 0, :], gm[:, 2, :], ALU.add), reads=[k_gm], writes=[k_mT])
            for half in range(2):
                for kc in range(8):
                    add("pe", lambda e, kc=kc, half=half: e.matmul(bank(5 + half), mT[:, kc, :], wo_t[:, kc, half * 512:(half + 1) * 512], start=(kc == 0), stop=(kc == 7)),
                        reads=[k_mT, k_wo], writes=[PK[5 + half]])
            hf, k_hf = hf_r[tg % 2]
            add("dve", lambda e, hf=hf, xl=xl: e.tensor_tensor(hf, xl, bank(5, 2), ALU.add), reads=[k_xl, PK[5], PK[6]], writes=[k_hf])
            add("pool", lambda e, hf=hf, t0=t0: e.dma_start(out=h_s[t0:t0 + 128, :], in_=hf), reads=[k_hf], dma_key=("g_hf", tg % 2))
            rms_rstd(hf, [k_hf], D, hsq, k_hsq, hss, k_hss, hrs, k_hrs)
            add("dve", lambda e, hf=hf: e.scalar_tensor_tensor(out=hnb, in0=hf, scalar=hrs[:, 0:1], in1=nfw_t, op0=ALU.mult, op1=ALU.mult),
                reads=[k_hf, k_hrs, k_nfw], writes=[k_hnb])
            for dc in range(8):
                add("pe", lambda e, dc=dc: e.transpose(bank_bf(7)[:, dc * 128:(dc + 1) * 128], hnb[:, dc * 128:(dc + 1) * 128], ident_b),
                    reads=[k_hnb, k_idb], writes=[PK[7]])
            hT, k_hT = hT_r[tg % 2]
            add("act", lambda e, hT=hT: e.copy(hT.rearrange("p a b -> p (a b)"), bank_bf(7)), reads=[PK[7]], writes=[k_hT])
            add("pool", lambda e, hT=hT, tg=tg: e.dma_start(out=hnT_s[tg], in_=hT), reads=[k_hT], dma_key=("g_hT", tg % 2))
        S_.barrier("g")

        AR.reset()
        wq_t, k_wq = AR.get([128, 8, 2048], BF16)
        sk_t, k_sk = AR.get([128, 16, 128], BF16)
        iota16, k_i16 = AR.get([128, 16], F32)
        add("pool", lambda e: e.dma_start(out=wq_t, in_=wq), writes=[k_wq], dma_key="r_c0")
        add("pool", lambda e: e.dma_start(out=sk_t, in_=skT), writes=[k_sk], dma_key="r_c1")
        add("dve", lambda e: e.tensor_copy(iota16, iota_f[:, 0:16]), reads=[k_iota], writes=[k_i16])
        hl_r = [AR.get([128, 8, 128], BF16) for _ in range(2)]
        qT, k_qT = AR.get([128, 16, 128], BF16)
        sc, k_sc = AR.get([128, 16, 128], F32)
        wk, k_wk = AR.get([128, 16, 128], F32)
        mx, k_mx = AR.get([128, 16, 16], F32)
        mi, k_mi = AR.get([128, 16, 16], U32)
        mif, k_mif = AR.get([128, 16, 16], F32)
        cand, k_cand = AR.get([128, 8, 256], F32)
        wk2, k_wk2 = AR.get([128, 8, 256], F32)
        top, k_top = AR.get([128, 8, 16], F32)
        pos, k_pos = AR.get([128, 8, 16], U32)
        posf, k_pf = AR.get([128, 8, 16], F32)
        pa, k_pa = AR.get([128, 8, 16], F32)
        pbb, k_pb = AR.get([128, 8, 16], F32)
        oh, k_oh = AR.get([128, 8, 16, 16], F32)
        gex, k_gex = AR.get([128, 8, 16], F32)
        gz, k_gz = AR.get([128, 8], F32)
        rt, k_rt = AR.get([128, 3, 128], F32)
        rT_r = [AR.get([128, 3, 128], F32) for _ in range(2)]
        for tg in range(NT):
            hl, k_hl = hl_r[tg % 2]
            add("sp", lambda e, hl=hl, tg=tg: e.dma_start(out=hl, in_=hnT_s[tg]), writes=[k_hl], dma_key=("r_hl", tg % 2))
            for f4 in range(4):
                pbk = f4 % 2
                for fi in range(4):
                    fc = f4 * 4 + fi
                    for dc in range(8):
                        add("pe", lambda e, fc=fc, fi=fi, dc=dc, pbk=pbk, hl=hl: e.matmul(
                            bank(pbk)[:, fi * 128:(fi + 1) * 128], wq_t[:, dc, fc * 128:(fc + 1) * 128], hl[:, dc, :], start=(dc == 0), stop=(dc == 7)),
                            reads=[k_wq, k_hl], writes=[PK[pbk]])
                add("act", lambda e, f4=f4, pbk=pbk: e.copy(qT[:, f4 * 4:(f4 + 1) * 4, :].rearrange("p a b -> p (a b)"), bank(pbk)),
                    reads=[PK[pbk]], writes=[("qT", f4)])
            for f4 in range(4):
                pbk = 2 + f4 % 2
                for fi in range(4):
                    fc = f4 * 4 + fi
                    add("pe", lambda e, fc=fc, fi=fi, pbk=pbk: e.matmul(bank(pbk)[:, fi * 128:(fi + 1) * 128], qT[:, fc, :], sk_t[:, fc, :], start=True, stop=True),
                        reads=[("qT", f4), k_sk], writes=[PK[pbk]])
                add("act", lambda e, f4=f4, pbk=pbk: e.copy(sc[:, f4 * 4:(f4 + 1) * 4, :].rearrange("p a b -> p (a b)"), bank(pbk)),
                    reads=[PK[pbk]], writes=[("sc", f4)])
            for fc in range(16):
                ks = ("sc", fc // 4)
                km = ("mx", fc)
                add("dve", lambda e, fc=fc: e.max(out=mx[:, fc, 0:8], in_=sc[:, fc, :]), reads=[ks], writes=[km])
                add("dve", lambda e, fc=fc: e.max_index(mi[:, fc, 0:8], mx[:, fc, 0:8], sc[:, fc, :]), reads=[ks, km], writes=[("mi", fc)])
                add("dve", lambda e, fc=fc: e.match_replace(out=wk[:, fc, :], in_to_replace=mx[:, fc, 0:8], in_values=sc[:, fc, :], imm_value=-1e30),
                    reads=[ks, km], writes=[("wk", fc)])
                add("dve", lambda e, fc=fc: e.max(out=mx[:, fc, 8:16], in_=wk[:, fc, :]), reads=[("wk", fc)], writes=[("mx2", fc)])
                add("dve", lambda e, fc=fc: e.max_index(mi[:, fc, 8:16], mx[:, fc, 8:16], wk[:, fc, :]), reads=[("wk", fc), ("mx2", fc)], writes=[("mi2", fc)])
            allmx = [("mx", f) for f in range(16)] + [("mx2", f) for f in range(16)]
            allmi = [("mi", f) for f in range(16)] + [("mi2", f) for f in range(16)]
            add("dve", lambda e: e.tensor_copy(mif, mi), reads=allmi, writes=[k_mif])
            mxv = mx.rearrange("p (h c) k -> p h c k", c=2)
            mfv = mif.rearrange("p (h c) k -> p h c k", c=2)
            add("dve", lambda e, mxv=mxv: e.tensor_tensor(cand.rearrange("p h (a b) -> p h a b", a=16), bc(mxv[:, :, 0, :], [128, 8, 16, 16], 3),
                                                          bc(mxv[:, :, 1, :], [128, 8, 16, 16], 2), ALU.add), reads=allmx, writes=[k_cand])
            for h in range(8):
                kt = ("top", h)
                add("dve", lambda e, h=h: e.max(out=top[:, h, 0:8], in_=cand[:, h, :]), reads=[k_cand], writes=[kt])
                add("dve", lambda e, h=h: e.max_index(pos[:, h, 0:8], top[:, h, 0:8], cand[:, h, :]), reads=[k_cand, kt], writes=[("pos", h)])
                add("dve", lambda e, h=h: e.match_replace(out=wk2[:, h, :], in_to_replace=top[:, h, 0:8], in_values=cand[:, h, :], imm_value=-1e30),
                    reads=[k_cand, kt], writes=[("wk2", h)])
                add("dve", lambda e, h=h: e.max(out=top[:, h, 8:16], in_=wk2[:, h, :]), reads=[("wk2", h)], writes=[("top2", h)])
                add("dve", lambda e, h=h: e.max_index(pos[:, h, 8:16], top[:, h, 8:16], wk2[:, h, :]), reads=[("wk2", h), ("top2", h)], writes=[("pos2", h)])
            alltop = [("top", h) for h in range(8)] + [("top2", h) for h in range(8)]
            allpos = [("pos", h) for h in range(8)] + [("pos2", h) for h in range(8)]
            add("dve", lambda e: e.tensor_tensor(gex, top, bc(top[:, :, 0], [128, 8, 16], 2), ALU.subtract), reads=alltop, writes=[k_gex])
            add("act", lambda e: e.activation(out=gex, in_=gex, func=AF.Exp), reads=[k_gex], writes=[k_gex])
            add("dve", lambda e: e.tensor_reduce(out=gz, in_=gex, axis=AX.X, op=ALU.add), reads=[k_gex], writes=[k_gz])
            add("dve", lambda e: e.reciprocal(gz, gz), reads=[k_gz], writes=[k_gz])
            add("dve", lambda e: e.tensor_tensor(rt[:, 2, :].rearrange("p (h k) -> p h k", h=8), gex, bc(gz, [128, 8, 16], 2), ALU.mult),
                reads=[k_gex, k_gz], writes=[("rt", 2)])
            add("dve", lambda e: e.tensor_copy(posf, pos), reads=allpos, writes=[k_pf])
            add("dve", lambda e: e.tensor_scalar(pa, posf, -7.5, 0.0625, ALU.add, ALU.mult), reads=[k_pf], writes=[k_pa])
            add("dve", lambda e: e.tensor_single_scalar(pa, pa, MAGIC, ALU.add), reads=[k_pa], writes=[k_pa])
            add("dve", lambda e: e.tensor_single_scalar(pa, pa, -MAGIC, ALU.add), reads=[k_pa], writes=[k_pa])
            add("dve", lambda e: e.scalar_tensor_tensor(out=pbb, in0=pa, scalar=-16.0, in1=posf, op0=ALU.mult, op1=ALU.add), reads=[k_pa, k_pf], writes=[k_pb])
            for which, src, k_src, cidx in ((0, pa, k_pa, 0), (1, pbb, k_pb, 1)):
                add("dve", lambda e, src=src: e.tensor_tensor(oh, bc(src, [128, 8, 16, 16], 3),
                                                              iota16.unsqueeze(1).unsqueeze(1).to_broadcast([128, 8, 16, 16]), ALU.is_equal),
                    reads=[k_src, k_i16], writes=[k_oh])
                add("dve", lambda e, cidx=cidx, mfv=mfv: e.tensor_tensor(oh, oh, bc(mfv[:, :, cidx, :], [128, 8, 16, 16], 2), ALU.mult),
                    reads=[k_oh, k_mif], writes=[k_oh])
                add("dve", lambda e, which=which: e.tensor_reduce(out=rt[:, which, :].rearrange("p (h k) -> p h k", h=8), in_=oh, axis=AX.X, op=ALU.add),
                    reads=[k_oh], writes=[("rt", which)])
            rT, k_rT = rT_r[tg % 2]
            for w3 in range(3):
                add("pe", lambda e, w3=w3: e.transpose(bank(4)[:, w3 * 128:(w3 + 1) * 128], rt[:, w3, :], ident_f),
                    reads=[("rt", w3), k_idf], writes=[PK[4]])
            add("act", lambda e, rT=rT: e.copy(rT.rearrange("p a b -> p (a b)"), bank(4)[:, 0:384]), reads=[PK[4]], writes=[k_rT])
            add("pool", lambda e, rT=rT, tg=tg: e.dma_start(out=r_s[tg], in_=rT), reads=[k_rT], dma_key=("r_rT", tg % 2))
        S_.barrier("r")

        AR.reset()
        NB2 = S // 256
        hT2_r = [AR.get([128, 8, 256], BF16) for _ in range(2)]
        rl_r = [AR.get([128, 2, 3, 128], F32) for _ in range(2)]
        A_r = [AR.get([128, 64, 128], BF16) for _ in range(1)]
        B_r = [AR.get([128, 64, 128], BF16) for _ in range(1)]
        M_t, k_M = AR.get([128, 256, 128], BF16)
        dW_r = [AR.get([128, 4, D], BF16) for _ in range(2)]
        uW_r = [AR.get([128, 4, D], BF16) for _ in range(2)]
        ge_r = [AR.get([128, 256], BF16) for _ in range(2)]
        co_r = [AR.get([128, 256], BF16) for _ in range(2)]
        hl2_r = [AR.get([128, D], F32) for _ in range(1)]
        of2_r = [AR.get([128, D], F32) for _ in range(2)]
        wi = 0
        ji = 0
        oi2 = 0
        sbi = 0
        for b2 in range(NB2):
            hT2, k_hT2 = hT2_r[b2 % 2]
            rl, k_rl = rl_r[b2 % 2]
            for tt in range(2):
                tg = b2 * 2 + tt
                add("sp", lambda e, hT2=hT2, tt=tt, tg=tg: e.dma_start(out=hT2[:, :, tt * 128:(tt + 1) * 128], in_=hnT_s[tg]),
                    writes=[k_hT2], dma_key=("p_h", b2 % 2))
                add("sp", lambda e, rl=rl, tt=tt, tg=tg: e.dma_start(out=rl[:, tt, :, :], in_=r_s[tg]), writes=[k_rl], dma_key=("p_r", b2 % 2))
            for sb in range(4):
                tt, to = sb // 2, (sb % 2) * 64
                A_, k_A = A_r[0]
                B_, k_B = B_r[0]
                sbi += 1
                i1 = rl[:, tt, 0, to:to + 64]
                i2 = rl[:, tt, 1, to:to + 64]
                gg = rl[:, tt, 2, to:to + 64]
                add("dve", lambda e, A_=A_, i1=i1: e.tensor_tensor(A_, bc(iota_f, [128, 64, 128], 1), bc(i1, [128, 64, 128], 2), ALU.is_equal),
                    reads=[k_rl, k_iota], writes=[k_A])
                add("dve", lambda e, B_=B_, i2=i2: e.tensor_tensor(B_, bc(iota_f, [128, 64, 128], 1), bc(i2, [128, 64, 128], 2), ALU.is_equal),
                    reads=[k_rl, k_iota], writes=[k_B])
                add("dve", lambda e, B_=B_, gg=gg: e.tensor_tensor(B_, B_, bc(gg, [128, 64, 128], 2), ALU.mult), reads=[k_rl, k_B], writes=[k_B])
                for q in range(16):
                    pbk = 4 + (q % 2)
                    for t4 in range(4):
                        tk = q * 4 + t4
                        add("pe", lambda e, A_=A_, B_=B_, tk=tk, t4=t4, pbk=pbk: e.matmul(
                            bank(pbk)[:, t4 * 128:(t4 + 1) * 128], A_[:, tk, :], B_[:, tk, :], start=True, stop=True),
                            reads=[k_A, k_B], writes=[PK[pbk]])
                    tb = sb * 64 + q * 4
                    add("act", lambda e, tb=tb, pbk=pbk: e.copy(M_t[:, tb:tb + 4, :].rearrange("p a b -> p (a b)"), bank(pbk)),
                        reads=[PK[pbk]], writes=[("M", sb)])
            allM = [("M", sb) for sb in range(4)]
            for jq in range(32):
                dW, k_dW = dW_r[wi % 2]
                uW, k_uW = uW_r[wi % 2]
                add("sp", lambda e, dW=dW, jq=jq: e.dma_start(out=dW, in_=dnT_b[jq * 4:(jq + 1) * 4].rearrange("j p f -> p j f")),
                    writes=[k_dW], dma_key=("p_dw", wi % 2))
                add("sp", lambda e, uW=uW, jq=jq: e.dma_start(out=uW, in_=up_b[jq * 4:(jq + 1) * 4].rearrange("j p f -> p j f")),
                    writes=[k_uW], dma_key=("p_uw", wi % 2))
                wi += 1
                for jj in range(4):
                    j = jq * 4 + jj
                    pbk = 4 + (ji % 2)
                    ge, k_ge = ge_r[ji % 2]
                    co, k_co = co_r[ji % 2]
                    ji += 1
                    for dc in range(8):
                        add("pe", lambda e, dW=dW, jj=jj, dc=dc, pbk=pbk, hT2=hT2: e.matmul(
                            bank(pbk)[:, 0:256], dW[:, jj, dc * 128:(dc + 1) * 128], hT2[:, dc, :], start=(dc == 0), stop=(dc == 7)),
                            reads=[k_dW, k_hT2], writes=[PK[pbk]])
                    add("act", lambda e, ge=ge, pbk=pbk: e.activation(out=ge, in_=bank(pbk)[:, 0:256], func=AF.Gelu), reads=[PK[pbk]], writes=[k_ge])
                    add("dve", lambda e, ge=ge, co=co, j=j: e.tensor_tensor(co, ge, M_t[:, :, j], ALU.mult), reads=[k_ge] + allM, writes=[k_co])
                    for tt in range(2):
                        for half in range(2):
                            add("pe", lambda e, co=co, uW=uW, jj=jj, tt=tt, half=half, j=j: e.matmul(
                                bank(tt * 2 + half), co[:, tt * 128:(tt + 1) * 128], uW[:, jj, half * 512:(half + 1) * 512],
                                start=(j == 0), stop=(j == 127)), reads=[k_co, k_uW], writes=[PK[tt * 2 + half]])
            for tt in range(2):
                tg = b2 * 2 + tt
                t0 = tg * 128
                hl2, k_hl2 = hl2_r[0]
                of2, k_of2 = of2_r[oi2 % 2]
                add("sp", lambda e, hl2=hl2, t0=t0: e.dma_start(out=hl2, in_=h_s[t0:t0 + 128, :]), writes=[k_hl2], dma_key="p_hl")
                add("dve", lambda e, hl2=hl2, of2=of2, tt=tt: e.tensor_tensor(of2, hl2, bank(tt * 2, 2), ALU.add),
                    reads=[k_hl2, PK[tt * 2], PK[tt * 2 + 1]], writes=[k_of2])
                add("pool", lambda e, of2=of2, t0=t0: e.dma_start(out=out[t0:t0 + 128, :], in_=of2), reads=[k_of2], dma_key=("p_out", oi2 % 2))
                oi2 += 1
        S_.emit(st)
        build_program.stats = S_.stats
    return nc


def _rep(v, n=128):
    return np.ascontiguousarray(np.broadcast_to(np.asarray(v, dtype=np.float32).reshape(1, -1), (n, np.asarray(v).size)))


def _kmaj(w, nchunk):
    w = np.asarray(w, dtype=np.float32)
    return np.ascontiguousarray(w.reshape(nchunk, 128, w.shape[1]).transpose(1, 0, 2))


def shared_inputs(inp):
    L = 0
    sh = {}
    sh["nmw"] = _rep(inp["norm_mix_w"][L])
    sh["w_in_r"] = _kmaj(inp["w_in"][L], 8)
    cwv = np.asarray(inp["ssd_conv_w"][L], dtype=np.float32)
    sh["cw"] = np.ascontiguousarray(cwv.reshape(4, 16, 128).transpose(2, 1, 0))
    sh["cb"] = np.ascontiguousarray(np.asarray(inp["ssd_conv_b"][L], dtype=np.float32).reshape(16, 128).T)
    sh["dtb"] = _rep(inp["ssd_dt_bias"][L])
    sh["alog"] = _rep(inp["ssd_a_log"][L])
    sh["sdd"] = _rep(inp["ssd_d"][L])
    sh["snw"] = _rep(inp["ssd_norm_w"][L])
    sh["qnw"] = _rep(inp["dil_q_norm_w"][L])
    sh["knw"] = _rep(inp["dil_k_norm_w"][L])
    sh["mnw"] = _rep(inp["mem_norm_w"][L])
    sh["wkv"] = _kmaj(inp["w_mem_kv"][L], 8)
    sh["mqnw"] = _rep(inp["mem_q_norm_w"][L])
    sh["mknw"] = _rep(inp["mem_k_norm_w"][L])
    sh["wsb"] = _kmaj(inp["w_ssd_br"][L], 8)
    sh["wdb"] = _kmaj(inp["w_dil_br"][L], 2)
    wm = np.asarray(inp["w_mem_br"][L], dtype=np.float32)
    wmp = np.zeros((128, 8, D), dtype=np.float32)
    for h in range(4):
        wmp[:, 2 * h, :] = wm[h * 192:h * 192 + 128]
        wmp[0:64, 2 * h + 1, :] = wm[h * 192 + 128:(h + 1) * 192]
    sh["wmb"] = wmp
    sh["wo"] = _kmaj(inp["w_out"][L], 8)
    sh["nfw"] = _rep(inp["norm_ffn_w"][L])
    sh["wq"] = _kmaj(inp["peer_w_query"][L], 8)
    sk = np.asarray(inp["peer_sub_keys"][L], dtype=np.float32)
    sh["skT"] = np.ascontiguousarray(sk.reshape(16, 128, 128).transpose(2, 0, 1))
    dn = np.asarray(inp["peer_down"][L], dtype=np.float32).reshape(128, 128, 8, 128)
    sh["dnT_r"] = np.ascontiguousarray(dn.transpose(1, 3, 2, 0)).reshape(128, 128, D)
    up = np.asarray(inp["peer_up"][L], dtype=np.float32).reshape(128, 128, D)
    sh["up_r"] = np.ascontiguousarray(up.transpose(1, 0, 2))
    return sh


def core_inputs(inp, b, S):
    NT = S // 128
    posb = np.asarray(inp["positions"][b][:S], dtype=np.int32)
    return {
        "x": np.ascontiguousarray(np.asarray(inp["x"][b][:S], dtype=np.float32)),
        "mem": np.ascontiguousarray(np.asarray(inp["mem"][b], dtype=np.float32)),
        "pos_t": np.ascontiguousarray(posb.reshape(NT, 128).T),
    }


def kernel(**inputs):
    B, S = inputs["x"].shape[0], inputs["x"].shape[1]
    nc = build_program(S)
    sh = shared_inputs(inputs)
    in_maps = []
    for b in range(B):
        m = dict(sh)
        m.update(core_inputs(inputs, b, S))
        in_maps.append(m)
    res = run_bass_kernel_spmd(nc, in_maps, core_ids=list(range(B)))
    return np.stack([np.asarray(r["out"], dtype=np.float32) for r in res.results], axis=0)
```

```python
import math
from contextlib import ExitStack

import numpy as np
import concourse.bass as bass
import concourse.mybir as mybir
from concourse.bass_utils import run_bass_kernel_spmd

F32 = mybir.dt.float32
BF16 = mybir.dt.bfloat16
U32 = mybir.dt.uint32
I32 = mybir.dt.int32
AF = mybir.ActivationFunctionType
ALU = mybir.AluOpType
AX = mybir.AxisListType

D = 1024
EPS = 1e-6
NPROJ = 9232
OFF_Z, OFF_XBC, OFF_DT, OFF_DQ, OFF_DK, OFF_DV, OFF_MQ, OFF_G = 0, 1024, 3072, 3088, 3856, 4624, 5392, 6160
DILS = (1, 4, 16)
MAGIC = 12582912.0


class Sched:
    ENGS = ("pe", "act", "dve", "pool", "sp")

    def __init__(self, nc):
        self.nc = nc
        self.ops = []
        self.last_w = {}
        self.readers = {}
        self.dma_count = {}
        self.slots = {}
        self.dead = False
        self.stop_after = None

    def add(self, eng, fn, reads=(), writes=(), dma_key=None, wait_all_dma=False):
        if self.dead:
            return -1
        idx = len(self.ops)
        deps = {}
        is_dma = dma_key is not None
        if is_dma:
            dma_key = self.slots.setdefault((eng, dma_key), (eng, sum(1 for k in self.slots if k[0] == eng)))

        def dep(j, kind):
            o = self.ops[j]
            if o["dma"] is None and o["eng"] == eng and not is_dma:
                if eng == "pe":
                    return
            deps[j] = True

        for o in reads:
            if o in self.last_w:
                dep(self.last_w[o], "raw")
        for o in writes:
            if o in self.last_w:
                dep(self.last_w[o], "waw")
            for r in self.readers.get(o, {}).values():
                dep(r, "war")
        for o in writes:
            self.last_w[o] = idx
            self.readers[o] = {}
        for o in reads:
            rk = ("dma", idx) if is_dma else eng
            self.readers.setdefault(o, {})[rk] = idx
        dma_waits = {}
        comp_deps = []
        for j in deps:
            o = self.ops[j]
            if o["dma"] is not None:
                dma_waits[o["dma"]] = self.dma_count[o["dma"]]
            else:
                comp_deps.append(j)
        if wait_all_dma:
            for k, c in self.dma_count.items():
                dma_waits[k] = c
        if is_dma:
            self.dma_count[dma_key] = self.dma_count.get(dma_key, 0) + 1
        self.ops.append(dict(eng=eng, fn=fn, dma=dma_key, comp_deps=comp_deps,
                             dma_waits=dma_waits, signal=False, seq=None))
        return idx

    def barrier(self, tag):
        bt = self.bar_tile
        fns = {"pe": lambda eng: eng.nop(), "sp": lambda eng: eng.nop(),
               "act": lambda eng: eng.copy(bt[:, 0:1], bt[:, 4:5]),
               "dve": lambda eng: eng.tensor_copy(bt[:, 1:2], bt[:, 4:5]),
               "pool": lambda eng: eng.memset(bt[:, 2:3], 0.0)}
        for e in self.ENGS:
            self.add(e, fns[e], writes=[("barA", tag, e)], wait_all_dma=True)
        for e in self.ENGS:
            self.add(e, lambda eng: eng.nop(), reads=[("barA", tag, x) for x in self.ENGS],
                     writes=[("barB", tag, e)])
        if tag == self.stop_after:
            self.dead = True
        self.slots = {}
        self.last_w = {}
        self.readers = {}

    def emit(self, stack):
        nc = self.nc
        ops = self.ops
        for o in ops:
            for j in o["comp_deps"]:
                ops[j]["signal"] = True
        cnt = {e: 0 for e in self.ENGS}
        for o in ops:
            if o["signal"]:
                cnt[o["eng"]] += 1
                o["seq"] = cnt[o["eng"]]
        self.stats = dict(cnt=dict(cnt), n_ops=len(ops), max_dma=max([16 * v for v in self.dma_count.values()] + [0]),
                          n_sems=5 + len(self.dma_count))
        sems = {e: stack.enter_context(nc.semaphore("s_" + e)) for e in self.ENGS}
        dsems = {k: stack.enter_context(nc.semaphore("d_%d" % i))
                 for i, k in enumerate(self.dma_count)}
        block = stack.enter_context(nc.Block())
        per = {e: [] for e in self.ENGS}
        for o in ops:
            per[o["eng"]].append(o)

        def run(eng_name, eng):
            seen = {e: 0 for e in self.ENGS}
            dseen = {}
            for o in per[eng_name]:
                need = {}
                for j in o["comp_deps"]:
                    d = ops[j]
                    if d["seq"] > seen[d["eng"]]:
                        need[d["eng"]] = max(need.get(d["eng"], 0), d["seq"])
                for e, v in need.items():
                    eng.wait_ge(sems[e], v)
                    seen[e] = v
                for k, c in o["dma_waits"].items():
                    if dseen.get(k, 0) < c:
                        eng.wait_ge(dsems[k], 16 * c)
                        dseen[k] = c
                ins = o["fn"](eng)
                if o["dma"] is not None:
                    ins.then_inc(dsems[o["dma"]], 16)
                elif o["signal"]:
                    ins.then_inc(sems[eng_name], 1)
            for k in self.dma_count:
                if dseen.get(k, 0) < self.dma_count[k]:
                    eng.wait_ge(dsems[k], 16 * self.dma_count[k])

        @block.tensor
        def _(e):
            run("pe", e)

        @block.scalar
        def _(e):
            run("act", e)

        @block.vector
        def _(e):
            run("dve", e)

        @block.gpsimd
        def _(e):
            run("pool", e)

        @block.sync
        def _(e):
            run("sp", e)


class Arena:
    def __init__(self, ap, nwords):
        self.ap = ap
        self.n = nwords
        self.off = 0
        self.cnt = 0

    def reset(self):
        self.off = 0

    def get(self, shape, dt):
        esz = 4 if dt in (F32, U32, I32) else 2
        nel = 1
        for s in shape[1:]:
            nel *= s
        words = (nel * esz + 3) // 4
        words = (words + 7) // 8 * 8
        assert self.off + words <= self.n, ("arena overflow", self.off, words, self.n)
        v = self.ap[:, self.off:self.off + words]
        self.off += words
        if dt != F32:
            v = v.bitcast(dt)
        v = v[:, 0:nel]
        if len(shape) == 3:
            v = v.rearrange("p (a b) -> p a b", a=shape[1])
        elif len(shape) == 4:
            v = v.rearrange("p (a b c) -> p a b c", a=shape[1], b=shape[2])
        self.cnt += 1
        return v, ("ar", self.cnt)


def bc(ap, shape, axis):
    return ap.unsqueeze(axis).to_broadcast(list(shape))


def build_program(S, debug=False, stop_after=None):
    nc = bass.Bass("TRN2", target_bir_lowering=False)
    NT = S // 128
    NB = S // 512
    skind = "ExternalOutput" if debug else "Internal"

    def din(name, shape, dt=F32):
        return nc.dram_tensor(name, list(shape), dt, kind="ExternalInput").ap()

    def dscr(name, shape, dt):
        return nc.dram_tensor(name, list(shape), dt, kind=skind).ap()

    x = din("x", [S, D])
    mem = din("mem", [256, D])
    pos_t = din("pos_t", [128, NT], I32)
    nmw = din("nmw", [128, D])
    w_in_r = din("w_in_r", [128, 8, NPROJ])
    cw = din("cw", [128, 16, 4])
    cb = din("cb", [128, 16])
    dtb = din("dtb", [128, 16])
    alog = din("alog", [128, 16])
    sdd = din("sdd", [128, 16])
    snw = din("snw", [128, D])
    qnw = din("qnw", [128, 64])
    knw = din("knw", [128, 64])
    mnw = din("mnw", [128, D])
    wkv = din("wkv", [128, 8, 1536])
    mqnw = din("mqnw", [128, 192])
    mknw = din("mknw", [128, 192])
    wsb = din("wsb", [128, 8, D])
    wdb = din("wdb", [128, 2, D])
    wmb = din("wmb", [128, 8, D])
    wo = din("wo", [128, 8, D])
    nfw = din("nfw", [128, D])
    wq = din("wq", [128, 8, 2048])
    skT = din("skT", [128, 16, 128])
    dnT_r = din("dnT_r", [128, 128, D])
    up_r = din("up_r", [128, 128, D])
    out = nc.dram_tensor("out", [S, D], F32, kind="ExternalOutput").ap()

    w_in_b = dscr("w_in_b", [128, 8, NPROJ], BF16)
    dnT_b = dscr("dnT_b", [128, 128, D], BF16)
    up_b = dscr("up_b", [128, 128, D], BF16)
    z_s = dscr("z_s", [S, D], BF16)
    xbc_s = dscr("xbc_s", [2048, S], F32)
    dt_s = dscr("dt_s", [S, 16], F32)
    q_s = dscr("q_s", [S, 768], BF16)
    k_s = dscr("k_s", [S, 768], BF16)
    v_s = dscr("v_s", [S, 768], BF16)
    mq_s = dscr("mq_s", [S, 768], BF16)
    g_s = dscr("g_s", [3072, S], BF16)
    yssd_s = dscr("yssd_s", [S, D], BF16)
    od_s = dscr("od_s", [3, S, 260], F32)
    ymem_s = dscr("ymem_s", [S, 768], BF16)
    h_s = dscr("h_s", [S, D], F32)
    hnT_s = dscr("hnT_s", [NT, 128, 8, 128], BF16)
    r_s = dscr("r_s", [NT, 128, 3, 128], F32)

    st = ExitStack()
    with st:
        S_ = Sched(nc)
        S_.stop_after = stop_after
        add = S_.add
        NA = 44 * 1024
        arena_t = st.enter_context(nc.sbuf_tensor("arena", [128, NA], F32))
        AR = Arena(arena_t, NA)
        NC_ = 3 * 1024
        const_t = st.enter_context(nc.sbuf_tensor("consts", [128, NC_], F32))
        CA = Arena(const_t, NC_)
        psum = st.enter_context(nc.psum_tensor("psum", [128, 8 * 512], F32))

        def bank(i, n=1):
            return psum[:, i * 512:(i + n) * 512]

        def bank_bf(i):
            return psum[:, i * 512:(i + 1) * 512].bitcast(BF16)

        PK = [("ps", i) for i in range(8)]

        ident_f, k_idf = CA.get([128, 128], F32)
        ident_b, k_idb = CA.get([128, 128], BF16)
        iota_f, k_iota = CA.get([128, 128], F32)
        rowi, k_rowi = CA.get([128, 128], F32)
        mge_f, k_mgef = CA.get([128, 128], F32)
        mge_b, k_mgeb = CA.get([128, 128], BF16)
        mle_b, k_mleb = CA.get([128, 128], BF16)
        negm, k_negm = CA.get([128, 128], F32)
        ones_f, k_onesf = CA.get([128, 128], F32)
        bar_t, _kb = CA.get([128, 8], F32)
        S_.bar_tile = bar_t
        add("pool", lambda e: e.memset(bar_t, 0.0), writes=[_kb])
        cs_t, k_cs = CA.get([128, NT, 8], F32)
        sn_t, k_sn = CA.get([128, NT, 8], F32)
        aneg, k_aneg = CA.get([128, 16], F32)
        sd_t, k_sd = CA.get([128, 16], F32)
        dtb_t, k_dtb = CA.get([128, 16], F32)
        cw_t, k_cw = CA.get([128, 16, 4], F32)
        cb_t, k_cb = CA.get([128, 16], F32)

        add("pool", lambda e: e.iota(iota_f, pattern=[[1, 128]], base=0, channel_multiplier=0,
                                     allow_small_or_imprecise_dtypes=True), writes=[k_iota])
        add("pool", lambda e: e.iota(rowi, pattern=[[1, 128]], base=0, channel_multiplier=-1,
                                     allow_small_or_imprecise_dtypes=True), writes=[k_rowi])
        add("dve", lambda e: e.tensor_single_scalar(ident_f, rowi, 0.0, ALU.is_equal), reads=[k_rowi], writes=[k_idf])
        add("dve", lambda e: e.tensor_copy(ident_b, ident_f), reads=[k_idf], writes=[k_idb])
        add("dve", lambda e: e.tensor_single_scalar(mge_f, rowi, 0.0, ALU.is_ge), reads=[k_rowi], writes=[k_mgef])
        add("dve", lambda e: e.tensor_copy(mge_b, mge_f), reads=[k_mgef], writes=[k_mgeb])
        add("dve", lambda e: e.tensor_single_scalar(mle_b, rowi, 0.0, ALU.is_le), reads=[k_rowi], writes=[k_mleb])
        add("dve", lambda e: e.tensor_scalar(negm, mge_f, -1.0, 30000.0, ALU.add, ALU.mult), reads=[k_mgef], writes=[k_negm])
        add("pool", lambda e: e.memset(ones_f, 1.0), writes=[k_onesf])
        add("sp", lambda e: e.dma_start(out=aneg, in_=alog), writes=[k_aneg], dma_key="c_aneg")
        add("act", lambda e: e.activation(out=aneg, in_=aneg, func=AF.Exp), reads=[k_aneg], writes=[k_aneg])
        add("dve", lambda e: e.tensor_single_scalar(aneg, aneg, -1.0, ALU.mult), reads=[k_aneg], writes=[k_aneg])
        add("sp", lambda e: e.dma_start(out=sd_t, in_=sdd), writes=[k_sd], dma_key="c_sd")
        add("sp", lambda e: e.dma_start(out=dtb_t, in_=dtb), writes=[k_dtb], dma_key="c_dtb")
        add("sp", lambda e: e.dma_start(out=cw_t, in_=cw), writes=[k_cw], dma_key="c_cw")
        add("sp", lambda e: e.dma_start(out=cb_t, in_=cb), writes=[k_cb], dma_key="c_cb")

        AR.reset()
        pos_i, k_posi = AR.get([128, NT], I32)
        pos_f, k_posf = AR.get([128, NT], F32)
        ang, k_ang = AR.get([128, NT, 8], F32)
        kk, k_kk = AR.get([128, NT, 8], F32)
        rr, k_rr = AR.get([128, NT, 8], F32)
        add("sp", lambda e: e.dma_start(out=pos_i, in_=pos_t), writes=[k_posi], dma_key="c_pos")
        add("dve", lambda e: e.tensor_copy(pos_f, pos_i), reads=[k_posi], writes=[k_posf])
        inv = np.exp(np.float32(-math.log(500000.0) * (2.0 / 16)) * np.arange(8, dtype=np.float32)).astype(np.float32)
        for j in range(8):
            add("dve", lambda e, j=j: e.tensor_single_scalar(ang[:, :, j], pos_f, float(inv[j]), ALU.mult),
                reads=[k_posf], writes=[k_ang])
        C1 = 6.28125
        rem = 2.0 * math.pi - C1
        C2 = float(np.float32(rem).view(np.uint32) & np.uint32(0xFFFFF000))
        C2 = float(np.array([np.float32(rem).view(np.uint32) & np.uint32(0xFFFFF000)], dtype=np.uint32).view(np.float32)[0])
        C3 = float(np.float32(rem - C2))
        add("dve", lambda e: e.tensor_scalar(kk, ang, 1.0 / (2.0 * math.pi), MAGIC, ALU.mult, ALU.add), reads=[k_ang], writes=[k_kk])
        add("dve", lambda e: e.tensor_single_scalar(kk, kk, -MAGIC, ALU.add), reads=[k_kk], writes=[k_kk])
        add("dve", lambda e: e.scalar_tensor_tensor(out=rr, in0=kk, scalar=-C1, in1=ang, op0=ALU.mult, op1=ALU.add), reads=[k_kk, k_ang], writes=[k_rr])
        add("dve", lambda e: e.scalar_tensor_tensor(out=rr, in0=kk, scalar=-C2, in1=rr, op0=ALU.mult, op1=ALU.add), reads=[k_kk, k_rr], writes=[k_rr])
        add("dve", lambda e: e.scalar_tensor_tensor(out=rr, in0=kk, scalar=-C3, in1=rr, op0=ALU.mult, op1=ALU.add), reads=[k_kk, k_rr], writes=[k_rr])
        add("dve", lambda e: e.tensor_scalar(rr, rr, 3.14159, -3.14159, ALU.min, ALU.max), reads=[k_rr], writes=[k_rr])
        add("act", lambda e: e.activation(out=sn_t, in_=rr, func=AF.Sin), reads=[k_rr], writes=[k_sn])
        add("dve", lambda e: e.tensor_single_scalar(kk, rr, -1.0, ALU.mult), reads=[k_rr], writes=[k_kk])
        add("dve", lambda e: e.tensor_tensor(kk, kk, rr, ALU.max), reads=[k_rr, k_kk], writes=[k_kk])
        add("dve", lambda e: e.tensor_scalar(kk, kk, -1.0, math.pi / 2.0, ALU.mult, ALU.add), reads=[k_kk], writes=[k_kk])
        add("act", lambda e: e.activation(out=cs_t, in_=kk, func=AF.Sin), reads=[k_kk], writes=[k_cs])
        S_.barrier("c0")

        AR.reset()
        wtmp = [AR.get([128, NPROJ], BF16) for _ in range(2)]
        for dc in range(8):
            t, kt = wtmp[dc % 2]
            add("pool", lambda e, t=t, dc=dc: e.dma_start(out=t, in_=w_in_r[:, dc, :]), writes=[kt], dma_key=("wt", dc % 2))
            add("sp", lambda e, t=t, dc=dc: e.dma_start(out=w_in_b[:, dc, :], in_=t), reads=[kt], dma_key="w_in_b")
        S_.barrier("w0")
        S_.barrier("w1")

        def rms_rstd(src, src_keys, n, scratch, k_scr, ssq, k_ssq, rstd, k_rstd, nh=1, hd=None):
            hd = hd or n
            if nh == 1:
                add("dve", lambda e: e.memset(ssq[:, 0:1], 0.0), writes=[k_ssq])
                add("act", lambda e: e.activation(out=scratch[:, 0:n], in_=src, func=AF.Square, accum_out=ssq[:, 0:1]),
                    reads=src_keys, writes=[k_scr, k_ssq])
            else:
                add("act", lambda e: e.activation(out=scratch[:, 0:n], in_=src, func=AF.Square),
                    reads=src_keys, writes=[k_scr])
                add("dve", lambda e: e.tensor_reduce(out=ssq[:, 0:nh], in_=scratch[:, 0:n].rearrange("p (h d) -> p h d", h=nh),
                                                     axis=AX.X, op=ALU.add), reads=[k_scr], writes=[k_ssq])
            add("dve", lambda e: e.tensor_scalar(ssq[:, 0:nh], ssq[:, 0:nh], 1.0 / hd, EPS, ALU.mult, ALU.add),
                reads=[k_ssq], writes=[k_ssq])
            add("act", lambda e: e.activation(out=ssq[:, 0:nh], in_=ssq[:, 0:nh], func=AF.Ln), reads=[k_ssq], writes=[k_ssq])
            add("act", lambda e: e.activation(out=rstd[:, 0:nh], in_=ssq[:, 0:nh], func=AF.Exp, scale=-0.5),
                reads=[k_ssq], writes=[k_rstd])

        AR.reset()
        nmw_t, k_nmw = AR.get([128, D], F32)
        qnw_t, k_qnw = AR.get([128, 64], F32)
        knw_t, k_knw = AR.get([128, 64], F32)
        mqnw_t, k_mqnw = AR.get([128, 192], F32)
        add("sp", lambda e: e.dma_start(out=nmw_t, in_=nmw), writes=[k_nmw], dma_key="a_c0")
        add("sp", lambda e: e.dma_start(out=qnw_t, in_=qnw), writes=[k_qnw], dma_key="a_c1")
        add("sp", lambda e: e.dma_start(out=knw_t, in_=knw), writes=[k_knw], dma_key="a_c2")
        add("sp", lambda e: e.dma_start(out=mqnw_t, in_=mqnw), writes=[k_mqnw], dma_key="a_c3")
        xt_r = [AR.get([128, D], F32) for _ in range(2)]
        sq_t, k_sq = AR.get([128, D], F32)
        ssq_t, k_ssq = AR.get([128, 16], F32)
        rstd_t, k_rstd = AR.get([128, 16], F32)
        ub_t, k_ub = AR.get([128, D], BF16)
        uT_r = [AR.get([128, 8, 512], BF16) for _ in range(2)]
        wseg_r = [AR.get([128, 8, 512], BF16) for _ in range(3)]
        ob_r = [AR.get([128, 512], BF16) for _ in range(4)]
        of_r = [AR.get([128, 512], F32) for _ in range(3)]
        qn_t, k_qn = AR.get([128, 512], F32)
        r1_t, k_r1 = AR.get([128, 8, 8], F32)
        r2_t, k_r2 = AR.get([128, 8, 8], F32)
        dt1_t, k_dt1 = AR.get([128, 16], F32)
        dtv_r = [AR.get([128, 16], F32) for _ in range(2)]

        segs = []
        for c0 in (0, 512):
            segs.append((OFF_Z + c0, 512, "z", c0))
        for c0 in range(0, 2048, 512):
            segs.append((OFF_XBC + c0, 512, "xbc", c0))
        segs.append((OFF_DT, 16, "dt", 0))
        for nm, off in (("q", OFF_DQ), ("k", OFF_DK), ("v", OFF_DV)):
            segs.append((off, 512, nm, 0))
            segs.append((off + 512, 256, nm, 512))
        segs.append((OFF_MQ, 384, "mq", 0))
        segs.append((OFF_MQ + 384, 384, "mq", 384))
        for c0 in range(0, 3072, 512):
            segs.append((OFF_G + c0, 512, "g", c0))

        cnt = dict(ob=0, of=0, ws=0, ps=0, dtv=0)

        def nxt(name, ring):
            i = cnt[name]
            cnt[name] += 1
            return ring[i % len(ring)] + (i % len(ring),)

        for blk in range(NB):
            uT, k_uT = uT_r[blk % 2]
            for ti in range(4):
                tg = blk * 4 + ti
                xt, k_xt = xt_r[tg % 2]
                add("sp", lambda e, xt=xt, tg=tg: e.dma_start(out=xt, in_=x[tg * 128:(tg + 1) * 128, :]),
                    writes=[k_xt], dma_key=("a_x", tg % 2))
                rms_rstd(xt, [k_xt], D, sq_t, k_sq, ssq_t, k_ssq, rstd_t, k_rstd)
                add("dve", lambda e, xt=xt: e.scalar_tensor_tensor(out=ub_t, in0=xt, scalar=rstd_t[:, 0:1], in1=nmw_t,
                                                                   op0=ALU.mult, op1=ALU.mult),
                    reads=[k_xt, k_rstd, k_nmw], writes=[k_ub])
                pb = 6 + (tg % 2)
                for dc in range(8):
                    add("pe", lambda e, dc=dc, pb=pb: e.transpose(bank_bf(pb)[:, dc * 128:(dc + 1) * 128],
                                                                  ub_t[:, dc * 128:(dc + 1) * 128], ident_b),
                        reads=[k_ub, k_idb], writes=[PK[pb]])
                add("act", lambda e, uT=uT, ti=ti, pb=pb: e.copy(
                    uT[:, :, ti * 128:(ti + 1) * 128], bank_bf(pb).rearrange("p (a b) -> p a b", a=8)),
                    reads=[PK[pb]], writes=[k_uT])
            for (c0, width, mode, rel) in segs:
                ws, k_ws, wi = nxt("ws", wseg_r)
                add("sp", lambda e, ws=ws, c0=c0, width=width: e.dma_start(out=ws[:, :, 0:width], in_=w_in_b[:, :, c0:c0 + width]),
                    writes=[k_ws], dma_key=("a_ws", wi))
                if mode in ("xbc", "g"):
                    for c4 in range(4):
                        pbk = cnt["ps"] % 6
                        cnt["ps"] += 1
                        for dc in range(8):
                            add("pe", lambda e, ws=ws, dc=dc, c4=c4, pbk=pbk, uT=uT: e.matmul(
                                bank(pbk), ws[:, dc, c4 * 128:(c4 + 1) * 128], uT[:, dc, :], start=(dc == 0), stop=(dc == 7)),
                                reads=[k_ws, k_uT], writes=[PK[pbk]])
                        frow = rel + c4 * 128
                        if mode == "xbc":
                            of, k_of, oi = nxt("of", of_r)
                            add("act", lambda e, of=of, pbk=pbk: e.copy(of, bank(pbk)), reads=[PK[pbk]], writes=[k_of])
                            add("pool", lambda e, of=of, frow=frow, blk=blk: e.dma_start(
                                out=xbc_s[frow:frow + 128, blk * 512:(blk + 1) * 512], in_=of), reads=[k_of], dma_key=("a_of", oi))
                        else:
                            ob, k_ob, oi = nxt("ob", ob_r)
                            add("act", lambda e, ob=ob, pbk=pbk: e.activation(out=ob, in_=bank(pbk), func=AF.Sigmoid),
                                reads=[PK[pbk]], writes=[k_ob])
                            add("pool", lambda e, ob=ob, frow=frow, blk=blk: e.dma_start(
                                out=g_s[frow:frow + 128, blk * 512:(blk + 1) * 512], in_=ob), reads=[k_ob], dma_key=("a_ob", oi))
                    continue
                for ti in range(4):
                    tg = blk * 4 + ti
                    t0 = tg * 128
                    pbk = cnt["ps"] % 6
                    cnt["ps"] += 1
                    ps = bank(pbk)[:, 0:width]
                    for dc in range(8):
                        add("pe", lambda e, ws=ws, dc=dc, ti=ti, ps=ps, uT=uT, width=width: e.matmul(
                            ps, uT[:, dc, ti * 128:(ti + 1) * 128], ws[:, dc, 0:width], start=(dc == 0), stop=(dc == 7)),
                            reads=[k_ws, k_uT], writes=[PK[pbk]])
                    if mode == "z":
                        ob, k_ob, oi = nxt("ob", ob_r)
                        add("act", lambda e, ob=ob, ps=ps: e.activation(out=ob, in_=ps, func=AF.Silu), reads=[PK[pbk]], writes=[k_ob])
                        add("pool", lambda e, ob=ob, t0=t0, rel=rel: e.dma_start(out=z_s[t0:t0 + 128, rel:rel + 512], in_=ob),
                            reads=[k_ob], dma_key=("a_ob", oi))
                    elif mode == "v":
                        ob, k_ob, oi = nxt("ob", ob_r)
                        add("act", lambda e, ob=ob, ps=ps, width=width: e.copy(ob[:, 0:width], ps), reads=[PK[pbk]], writes=[k_ob])
                        add("pool", lambda e, ob=ob, t0=t0, rel=rel, width=width: e.dma_start(
                            out=v_s[t0:t0 + 128, rel:rel + width], in_=ob[:, 0:width]), reads=[k_ob], dma_key=("a_ob", oi))
                    elif mode == "dt":
                        dtv, k_dtv, di = nxt("dtv", dtv_r)
                        add("dve", lambda e, ps=ps: e.tensor_tensor(dt1_t, ps, dtb_t, ALU.add), reads=[PK[pbk], k_dtb], writes=[k_dt1])
                        add("act", lambda e: e.activation(out=dt1_t, in_=dt1_t, func=AF.Exp), reads=[k_dt1], writes=[k_dt1])
                        add("dve", lambda e: e.tensor_single_scalar(dt1_t, dt1_t, 1.0, ALU.add), reads=[k_dt1], writes=[k_dt1])
                        add("act", lambda e, dtv=dtv: e.activation(out=dtv, in_=dt1_t, func=AF.Ln), reads=[k_dt1], writes=[k_dtv])
                        add("pool", lambda e, dtv=dtv, t0=t0: e.dma_start(out=dt_s[t0:t0 + 128, :], in_=dtv), reads=[k_dtv], dma_key=("a_dtv", di))
                    elif mode in ("q", "k"):
                        nh = width // 64
                        nwt, k_nw = (qnw_t, k_qnw) if mode == "q" else (knw_t, k_knw)
                        dst = q_s if mode == "q" else k_s
                        rms_rstd(ps, [PK[pbk]], width, sq_t, k_sq, ssq_t, k_ssq, rstd_t, k_rstd, nh=nh, hd=64)
                        qv = qn_t[:, 0:width].rearrange("p (h d) -> p h d", h=nh)
                        add("dve", lambda e, ps=ps, qv=qv, nh=nh: e.tensor_tensor(
                            qv, ps.rearrange("p (h d) -> p h d", h=nh), bc(rstd_t[:, 0:nh], [128, nh, 64], 2), ALU.mult),
                            reads=[PK[pbk], k_rstd], writes=[k_qn])
                        add("dve", lambda e, qv=qv, nh=nh, nwt=nwt: e.tensor_tensor(qv, qv, bc(nwt, [128, nh, 64], 1), ALU.mult),
                            reads=[k_qn, k_nw], writes=[k_qn])
                        ob, k_ob, oi = nxt("ob", ob_r)
                        obv = ob[:, 0:width].rearrange("p (h d) -> p h d", h=nh)
                        add("act", lambda e, ob=ob, width=width: e.copy(ob[:, 0:width], qn_t[:, 0:width]), reads=[k_qn], writes=[k_ob])
                        cosb = bc(cs_t[:, tg, :], [128, nh, 8], 1)
                        sinb = bc(sn_t[:, tg, :], [128, nh, 8], 1)
                        a1 = r1_t[:, 0:nh, :]
                        a2 = r2_t[:, 0:nh, :]
                        add("dve", lambda e, qv=qv, a1=a1, cosb=cosb: e.tensor_tensor(a1, qv[:, :, 0:8], cosb, ALU.mult), reads=[k_qn, k_cs], writes=[k_r1])
                        add("dve", lambda e, qv=qv, a2=a2, sinb=sinb: e.tensor_tensor(a2, qv[:, :, 8:16], sinb, ALU.mult), reads=[k_qn, k_sn], writes=[k_r2])
                        add("dve", lambda e, obv=obv, a1=a1, a2=a2: e.tensor_tensor(obv[:, :, 0:8], a1, a2, ALU.subtract),
                            reads=[k_r1, k_r2, k_ob], writes=[k_ob])
                        add("dve", lambda e, qv=qv, a1=a1, cosb=cosb: e.tensor_tensor(a1, qv[:, :, 8:16], cosb, ALU.mult), reads=[k_qn, k_cs, k_ob], writes=[k_r1])
                        add("dve", lambda e, qv=qv, a2=a2, sinb=sinb: e.tensor_tensor(a2, qv[:, :, 0:8], sinb, ALU.mult), reads=[k_qn, k_sn, k_ob], writes=[k_r2])
                        add("dve", lambda e, obv=obv, a1=a1, a2=a2: e.tensor_tensor(obv[:, :, 8:16], a1, a2, ALU.add),
                            reads=[k_r1, k_r2, k_ob], writes=[k_ob])
                        add("pool", lambda e, ob=ob, t0=t0, rel=rel, width=width, dst=dst: e.dma_start(
                            out=dst[t0:t0 + 128, rel:rel + width], in_=ob[:, 0:width]), reads=[k_ob], dma_key=("a_ob", oi))
                    elif mode == "mq":
                        rms_rstd(ps, [PK[pbk]], 384, sq_t, k_sq, ssq_t, k_ssq, rstd_t, k_rstd, nh=2, hd=192)
                        qv = qn_t[:, 0:384].rearrange("p (h d) -> p h d", h=2)
                        add("dve", lambda e, ps=ps, qv=qv: e.tensor_tensor(
                            qv, ps.rearrange("p (h d) -> p h d", h=2), bc(rstd_t[:, 0:2], [128, 2, 192], 2), ALU.mult),
                            reads=[PK[pbk], k_rstd], writes=[k_qn])
                        ob, k_ob, oi = nxt("ob", ob_r)
                        add("dve", lambda e, ob=ob, qv=qv: e.tensor_tensor(
                            ob[:, 0:384].rearrange("p (h d) -> p h d", h=2), qv, bc(mqnw_t, [128, 2, 192], 1), ALU.mult),
                            reads=[k_qn, k_mqnw], writes=[k_ob])
                        add("pool", lambda e, ob=ob, t0=t0, rel=rel: e.dma_start(out=mq_s[t0:t0 + 128, rel:rel + 384], in_=ob[:, 0:384]),
                            reads=[k_ob], dma_key=("a_ob", oi))
        S_.barrier("a")

        AR.reset()
        stT, k_stT = AR.get([128, 16, 64], F32)
        stB, k_stB = AR.get([128, 16, 64], BF16)
        snw_t, k_snw = AR.get([128, D], F32)
        add("sp", lambda e: e.dma_start(out=snw_t, in_=snw), writes=[k_snw], dma_key="b_c0")
        add("dve", lambda e: e.memset(stT, 0.0), writes=[k_stT])
        add("pool", lambda e: e.memset(stB, 0.0), writes=[k_stB])
        raw_r = [AR.get([128, 16, 131], F32) for _ in range(2)]
        dtl_r = [AR.get([128, 16], F32) for _ in range(2)]
        zl_r = [AR.get([128, D], BF16) for _ in range(2)]
        cv_t, k_cv = AR.get([128, 16, 128], F32)
        xbT, k_xbT = AR.get([128, 16, 128], BF16)
        xs_t, k_xs = AR.get([128, 16, 64], BF16)
        Bt_t, k_Bt = AR.get([128, 4, 128], BF16)
        da_t, k_da = AR.get([128, 16], F32)
        acol, k_acol = AR.get([128, 16], F32)
        X_t, k_X = AR.get([128, 16, 128], F32)
        arow, k_arow = AR.get([128, 16, 128], F32)
        E_t, k_E = AR.get([128, 16, 128], F32)
        eA_t, k_eA = AR.get([128, 16, 128], F32)
        W_t, k_W = AR.get([128, 16, 128], BF16)
        CTp, k_CTp = AR.get([128, 16, 128], BF16)
        xdt, k_xdt = AR.get([128, 16, 64], BF16)
        xdd, k_xdd = AR.get([128, 16, 64], BF16)
        dec, k_dec = AR.get([128, 16], F32)
        dtd, k_dtd = AR.get([128, 16], F32)
        y_t, k_y = AR.get([128, D], F32)
        ysq, k_ysq = AR.get([128, D], F32)
        gss, k_gss = AR.get([128, 16], F32)
        grs, k_grs = AR.get([128, 16], F32)
        yb_r = [AR.get([128, D], BF16) for _ in range(2)]
        xbc_v = xbc_s.rearrange("(c p) t -> p c t", p=128)
        for c in range(NT):
            t0 = c * 128
            raw, k_raw = raw_r[c % 2]
            dtl, k_dtl = dtl_r[c % 2]
            zl, k_zl = zl_r[c % 2]
            if c == 0:
                add("pool", lambda e, raw=raw: e.memset(raw[:, :, 0:3], 0.0), writes=[k_raw])
                add("sp", lambda e, raw=raw: e.dma_start(out=raw[:, :, 3:131], in_=xbc_v[:, :, 0:128]), writes=[k_raw], dma_key=("b_raw", c % 2))
            else:
                add("sp", lambda e, raw=raw, t0=t0: e.dma_start(out=raw, in_=xbc_v[:, :, t0 - 3:t0 + 128]), writes=[k_raw], dma_key=("b_raw", c % 2))
            add("sp", lambda e, dtl=dtl, t0=t0: e.dma_start(out=dtl, in_=dt_s[t0:t0 + 128, :]), writes=[k_dtl], dma_key=("b_dt", c % 2))
            add("sp", lambda e, zl=zl, t0=t0: e.dma_start(out=zl, in_=z_s[t0:t0 + 128, :]), writes=[k_zl], dma_key=("b_z", c % 2))
            for cc in range(16):
                add("dve", lambda e, raw=raw, cc=cc: e.tensor_scalar(cv_t[:, cc, :], raw[:, cc, 3:131], cw_t[:, cc, 3:4], cb_t[:, cc:cc + 1],
                                                                     ALU.mult, ALU.add), reads=[k_raw, k_cw, k_cb], writes=[("cv", cc)])
            for kq in (2, 1, 0):
                for cc in range(16):
                    add("dve", lambda e, raw=raw, cc=cc, kq=kq: e.scalar_tensor_tensor(
                        out=cv_t[:, cc, :], in0=raw[:, cc, kq:kq + 128], scalar=cw_t[:, cc, kq:kq + 1], in1=cv_t[:, cc, :],
                        op0=ALU.mult, op1=ALU.add), reads=[k_raw, k_cw, ("cv", cc)], writes=[("cv", cc)])
            add("act", lambda e: e.activation(out=xbT, in_=cv_t, func=AF.Silu), reads=[("cv", cc) for cc in range(16)], writes=[k_xbT])
            for cc in range(8):
                add("pe", lambda e, cc=cc: e.transpose(bank_bf(0)[:, cc * 128:(cc + 1) * 128], xbT[:, cc, :], ident_b),
                    reads=[k_xbT, k_idb], writes=[PK[0]])
            for g in range(4):
                add("pe", lambda e, g=g: e.transpose(bank_bf(1)[:, g * 128:(g + 1) * 128], xbT[:, 8 + g, :], ident_b),
                    reads=[k_xbT, k_idb], writes=[PK[1]])
            add("act", lambda e: e.copy(xs_t.rearrange("p a b -> p (a b)"), bank_bf(0)), reads=[PK[0]], writes=[k_xs])
            add("act", lambda e: e.copy(Bt_t.rearrange("p a b -> p (a b)"), bank_bf(1)[:, 0:512]), reads=[PK[1]], writes=[k_Bt])
            add("dve", lambda e, dtl=dtl: e.tensor_tensor(da_t, dtl, aneg, ALU.mult), reads=[k_dtl, k_aneg], writes=[k_da])
            add("pe", lambda e: e.matmul(bank(2)[:, 0:16], mge_f, da_t, start=True, stop=True), reads=[k_mgef, k_da], writes=[PK[2]])
            add("act", lambda e: e.copy(acol, bank(2)[:, 0:16]), reads=[PK[2]], writes=[k_acol])
            add("dve", lambda e: e.tensor_tensor(X_t, bc(da_t, [128, 16, 128], 2), bc(mge_f, [128, 16, 128], 1), ALU.mult),
                reads=[k_da, k_mgef], writes=[k_X])
            for q4 in range(4):
                pbk = 3 + (q4 % 2)
                add("pe", lambda e, q4=q4, pbk=pbk: e.matmul(bank(pbk), ones_f, X_t[:, q4 * 4:(q4 + 1) * 4, :].rearrange("p a b -> p (a b)"),
                                                             start=True, stop=True), reads=[k_onesf, k_X], writes=[PK[pbk]])
                add("act", lambda e, q4=q4, pbk=pbk: e.copy(arow[:, q4 * 4:(q4 + 1) * 4, :].rearrange("p a b -> p (a b)"), bank(pbk)),
                    reads=[PK[pbk]], writes=[k_arow])
            add("dve", lambda e: e.tensor_tensor(E_t, arow, bc(negm, [128, 16, 128], 1), ALU.add), reads=[k_arow, k_negm], writes=[k_E])
            add("dve", lambda e: e.tensor_tensor(E_t, E_t, bc(acol, [128, 16, 128], 2), ALU.subtract), reads=[k_E, k_acol], writes=[k_E])
            add("act", lambda e: e.activation(out=E_t, in_=E_t, func=AF.Exp), reads=[k_E], writes=[k_E])
            add("act", lambda e: e.activation(out=eA_t, in_=arow, func=AF.Exp), reads=[k_arow], writes=[k_eA])
            for g in range(4):
                add("pe", lambda e, g=g: e.matmul(bank(5)[:, g * 128:(g + 1) * 128], xbT[:, 8 + g, :], xbT[:, 12 + g, :], start=True, stop=True),
                    reads=[k_xbT], writes=[PK[5]])
            for g in range(4):
                add("dve", lambda e, g=g: e.tensor_tensor(W_t[:, 4 * g:4 * g + 4, :], E_t[:, 4 * g:4 * g + 4, :],
                                                          bc(bank(5)[:, g * 128:(g + 1) * 128], [128, 4, 128], 1), ALU.mult),
                    reads=[k_E, PK[5]], writes=[k_W])
                add("pool", lambda e, g=g: e.tensor_tensor(CTp[:, 4 * g:4 * g + 4, :], eA_t[:, 4 * g:4 * g + 4, :],
                                                           bc(xbT[:, 12 + g, :], [128, 4, 128], 1), ALU.mult),
                    reads=[k_eA, k_xbT], writes=[k_CTp])
            add("dve", lambda e, dtl=dtl: e.tensor_tensor(xdt, xs_t, bc(dtl, [128, 16, 64], 2), ALU.mult), reads=[k_xs, k_dtl], writes=[k_xdt])
            for hd in range(16):
                pbk = 6 + hd // 8
                o = bank(pbk)[:, (hd % 8) * 64:(hd % 8 + 1) * 64]
                add("pe", lambda e, hd=hd, o=o: e.matmul(o, W_t[:, hd, :], xdt[:, hd, :], start=True, stop=False),
                    reads=[k_W, k_xdt], writes=[PK[pbk]])
                add("pe", lambda e, hd=hd, o=o: e.matmul(o, CTp[:, hd, :], stB[:, hd, :], start=False, stop=True),
                    reads=[k_CTp, k_stB], writes=[PK[pbk]])
            add("dve", lambda e: e.tensor_tensor(dec, arow[:, :, 127], acol, ALU.subtract), reads=[k_arow, k_acol], writes=[k_dec])
            add("act", lambda e: e.activation(out=dec, in_=dec, func=AF.Exp), reads=[k_dec], writes=[k_dec])
            add("dve", lambda e, dtl=dtl: e.tensor_tensor(dtd, dtl, dec, ALU.mult), reads=[k_dtl, k_dec], writes=[k_dtd])
            add("dve", lambda e: e.tensor_tensor(xdd, xs_t, bc(dtd, [128, 16, 64], 2), ALU.mult), reads=[k_xs, k_dtd], writes=[k_xdd])
            for hd in range(16):
                pbk = 3 + hd // 8
                o = bank(pbk)[:, (hd % 8) * 64:(hd % 8 + 1) * 64]
                add("pe", lambda e, hd=hd, o=o: e.matmul(o, Bt_t[:, hd // 4, :], xdd[:, hd, :], start=True, stop=True),
                    reads=[k_Bt, k_xdd], writes=[PK[pbk]])
            add("dve", lambda e: e.tensor_tensor(y_t.rearrange("p (a b) -> p a b", a=16), xs_t, bc(sd_t, [128, 16, 64], 2), ALU.mult),
                reads=[k_xs, k_sd], writes=[k_y])
            add("dve", lambda e: e.tensor_tensor(y_t[:, 0:512], y_t[:, 0:512], bank(6), ALU.add), reads=[k_y, PK[6]], writes=[k_y])
            add("dve", lambda e: e.tensor_tensor(y_t[:, 512:1024], y_t[:, 512:1024], bank(7), ALU.add), reads=[k_y, PK[7]], writes=[k_y])
            add("dve", lambda e, zl=zl: e.tensor_tensor(y_t, y_t, zl, ALU.mult), reads=[k_y, k_zl], writes=[k_y])
            rms_rstd(y_t, [k_y], D, ysq, k_ysq, gss, k_gss, grs, k_grs, nh=4, hd=256)
            add("dve", lambda e: e.tensor_tensor(y_t.rearrange("p (a b) -> p a b", a=4), y_t.rearrange("p (a b) -> p a b", a=4),
                                                 bc(grs[:, 0:4], [128, 4, 256], 2), ALU.mult), reads=[k_y, k_grs], writes=[k_y])
            yb, k_yb = yb_r[c % 2]
            add("dve", lambda e, yb=yb: e.tensor_tensor(yb, y_t, snw_t, ALU.mult), reads=[k_y, k_snw], writes=[k_yb])
            add("pool", lambda e, yb=yb, t0=t0: e.dma_start(out=yssd_s[t0:t0 + 128, :], in_=yb), reads=[k_yb], dma_key=("b_yb", c % 2))
            add("dve", lambda e: e.tensor_tensor(stT, stT, bc(eA_t[:, :, 127], [128, 16, 64], 2), ALU.mult), reads=[k_stT, k_eA], writes=[k_stT])
            add("dve", lambda e: e.tensor_tensor(stT[:, 0:8, :].rearrange("p a b -> p (a b)"), stT[:, 0:8, :].rearrange("p a b -> p (a b)"),
                                                 bank(3), ALU.add), reads=[k_stT, PK[3]], writes=[k_stT])
            add("dve", lambda e: e.tensor_tensor(stT[:, 8:16, :].rearrange("p a b -> p (a b)"), stT[:, 8:16, :].rearrange("p a b -> p (a b)"),
                                                 bank(4), ALU.add), reads=[k_stT, PK[4]], writes=[k_stT])
            add("act", lambda e: e.copy(stB, stT), reads=[k_stT], writes=[k_stB])
        S_.barrier("b")

        AR.reset()
        Qb_r = [AR.get([128, 256], BF16) for _ in range(2)]
        Kb_r = [AR.get([128, 256], BF16) for _ in range(2)]
        Vb_r = [AR.get([128, 256], BF16) for _ in range(2)]
        QT_r = [AR.get([128, 2, 128], BF16) for _ in range(2)]
        KT_r = [AR.get([128, 2, 128], BF16) for _ in range(2)]
        Va_r = [AR.get([128, 4, 65], BF16) for _ in range(2)]
        P_r = [AR.get([128, 128], BF16) for _ in range(4)]
        od_r = [AR.get([128, 260], F32) for _ in range(2)]
        for i in range(2):
            add("pool", lambda e, i=i: e.memset(Va_r[i][0][:, :, 64:65], 1.0), writes=[("va1", i)])
        etmp = [AR.get([128, 8, D], BF16) for _ in range(4)]
        casts = [(src, dst, nm, jg) for (src, dst, nm) in ((dnT_r, dnT_b, "dnT_b"), (up_r, up_b, "up_b")) for jg in range(16)]
        cast_i = [0]

        def emit_cast():
            ei = cast_i[0]
            if ei >= len(casts):
                return
            cast_i[0] += 1
            src, dst, nm, jg = casts[ei]
            t, kt = etmp[ei % 4]
            add("pool", lambda e, t=t, src=src, jg=jg: e.dma_start(
                out=t, in_=src[jg * 8:(jg + 1) * 8].rearrange("j p f -> p j f")), writes=[kt], dma_key=("et", ei % 4))
            add("act", lambda e, t=t, dst=dst, jg=jg: e.dma_start(
                out=dst[jg * 8:(jg + 1) * 8].rearrange("j p f -> p j f"), in_=t), reads=[kt], dma_key=("ets", ei % 4))
        bi = 0
        pi = 0
        for gi, dil in enumerate(DILS):
            nb = S // dil // 128
            for r in range(dil):
                for n in range(nb):
                    rows = slice(r + n * 128 * dil, r + n * 128 * dil + 127 * dil + 1, dil)
                    cols = slice(gi * 256, (gi + 1) * 256)
                    Qb, k_Qb = Qb_r[bi % 2]
                    Kb, k_Kb = Kb_r[bi % 2]
                    Vb, k_Vb = Vb_r[bi % 2]
                    QT, k_QT = QT_r[bi % 2]
                    KT, k_KT = KT_r[bi % 2]
                    Va, k_Va = Va_r[bi % 2]
                    KTp, k_KTp = KT_r[(bi + 1) % 2]
                    Vap, k_Vap = Va_r[(bi + 1) % 2]
                    add("sp", lambda e, Qb=Qb, rows=rows, cols=cols: e.dma_start(out=Qb, in_=q_s[rows, cols]), writes=[k_Qb], dma_key=("c_q", bi % 2))
                    add("sp", lambda e, Kb=Kb, rows=rows, cols=cols: e.dma_start(out=Kb, in_=k_s[rows, cols]), writes=[k_Kb], dma_key=("c_k", bi % 2))
                    add("sp", lambda e, Vb=Vb, rows=rows, cols=cols: e.dma_start(out=Vb, in_=v_s[rows, cols]), writes=[k_Vb], dma_key=("c_v", bi % 2))
                    for hp in range(2):
                        add("pe", lambda e, hp=hp, Qb=Qb: e.transpose(bank_bf(0)[:, hp * 128:(hp + 1) * 128], Qb[:, hp * 128:(hp + 1) * 128], ident_b),
                            reads=[k_Qb, k_idb], writes=[PK[0]])
                        add("pe", lambda e, hp=hp, Kb=Kb: e.transpose(bank_bf(0)[:, 256 + hp * 128:256 + (hp + 1) * 128], Kb[:, hp * 128:(hp + 1) * 128], ident_b),
                            reads=[k_Kb, k_idb], writes=[PK[0]])
                    add("act", lambda e, QT=QT: e.copy(QT.rearrange("p a b -> p (a b)"), bank_bf(0)[:, 0:256]), reads=[PK[0]], writes=[k_QT])
                    add("act", lambda e, KT=KT: e.copy(KT.rearrange("p a b -> p (a b)"), bank_bf(0)[:, 256:512]), reads=[PK[0]], writes=[k_KT])
                    add("dve", lambda e, Va=Va, Vb=Vb: e.tensor_copy(Va[:, :, 0:64], Vb.rearrange("p (h d) -> p h d", h=4)),
                        reads=[k_Vb, ("va1", bi % 2)], writes=[k_Va])
                    kts = ([("prev", KTp, k_KTp, Vap, k_Vap)] if n > 0 else []) + [("cur", KT, k_KT, Va, k_Va)]
                    opb = 3 + (bi % 2)
                    for h in range(4):
                        hp, hh = h // 2, h % 2
                        for ki, (which, kt_, k_kt, va_, k_va) in enumerate(kts):
                            spb = 1 + (pi % 2)
                            sslot = bank(spb)[:, ((pi // 2) % 4) * 128:((pi // 2) % 4 + 1) * 128]
                            P, k_P = P_r[pi % 4]
                            msk, k_msk = (mge_b, k_mgeb) if which == "cur" else (mle_b, k_mleb)
                            add("pe", lambda e, sslot=sslot, kt_=kt_, QT=QT, hp=hp, hh=hh: e.matmul(
                                sslot, kt_[hh * 64:(hh + 1) * 64, hp, :], QT[hh * 64:(hh + 1) * 64, hp, :], start=True, stop=True),
                                reads=[k_kt, k_QT], writes=[("pss", spb, (pi // 2) % 4)])
                            add("act", lambda e, P=P, sslot=sslot: e.activation(out=P, in_=sslot, func=AF.Exp, scale=0.125),
                                reads=[("pss", spb, (pi // 2) % 4)], writes=[k_P])
                            add("dve", lambda e, P=P, msk=msk: e.tensor_tensor(P, P, msk, ALU.mult), reads=[k_P, k_msk], writes=[k_P])
                            add("pe", lambda e, P=P, va_=va_, h=h, opb=opb, ki=ki, nk=len(kts): e.matmul(
                                bank(opb)[:, h * 65:(h + 1) * 65], P, va_[:, h, :], start=(ki == 0), stop=(ki == nk - 1)),
                                reads=[k_P, k_va, ("va1", 0), ("va1", 1)], writes=[PK[opb]])
                            pi += 1
                    od, k_od = od_r[bi % 2]
                    add("act", lambda e, od=od, opb=opb: e.copy(od, bank(opb)[:, 0:260]), reads=[PK[opb]], writes=[k_od])
                    add("pool", lambda e, od=od, rows=rows, gi=gi: e.dma_start(out=od_s[gi, rows, :], in_=od), reads=[k_od], dma_key=("c_od", bi % 2))
                    bi += 1
                    emit_cast()
        while cast_i[0] < len(casts):
            emit_cast()
        S_.barrier("c")

        AR.reset()
        mnw_t, k_mnw = AR.get([128, D], F32)
        mknw_t, k_mknw = AR.get([128, 192], F32)
        wkv_t, k_wkv = AR.get([128, 8, 1536], BF16)
        add("sp", lambda e: e.dma_start(out=mnw_t, in_=mnw), writes=[k_mnw], dma_key="m_c0")
        add("sp", lambda e: e.dma_start(out=mknw_t, in_=mknw), writes=[k_mknw], dma_key="m_c1")
        add("pool", lambda e: e.dma_start(out=wkv_t, in_=wkv), writes=[k_wkv], dma_key="m_c2")
        mt_t, k_mt = AR.get([128, D], F32)
        msq, k_msq = AR.get([128, D], F32)
        mss, k_mss = AR.get([128, 16], F32)
        mrs, k_mrs = AR.get([128, 16], F32)
        mub, k_mub = AR.get([128, D], BF16)
        memT, k_memT = AR.get([128, 8, 256], BF16)
        mkv, k_mkv = AR.get([128, 2, 1536], F32)
        mkn, k_mkn = AR.get([128, 2, 768], BF16)
        KmA, k_KmA = AR.get([128, 4, 256], BF16)
        KmB, k_KmB = AR.get([128, 4, 256], BF16)
        VmA, k_VmA = AR.get([128, 2, 4, 193], BF16)
        for mt in range(2):
            add("sp", lambda e, mt=mt: e.dma_start(out=mt_t, in_=mem[mt * 128:(mt + 1) * 128, :]), writes=[k_mt], dma_key="m_mem")
            rms_rstd(mt_t, [k_mt], D, msq, k_msq, mss, k_mss, mrs, k_mrs)
            add("dve", lambda e: e.scalar_tensor_tensor(out=mub, in0=mt_t, scalar=mrs[:, 0:1], in1=mnw_t, op0=ALU.mult, op1=ALU.mult),
                reads=[k_mt, k_mrs, k_mnw], writes=[k_mub])
            for dc in range(8):
                add("pe", lambda e, dc=dc: e.transpose(bank_bf(0)[:, dc * 128:(dc + 1) * 128], mub[:, dc * 128:(dc + 1) * 128], ident_b),
                    reads=[k_mub, k_idb], writes=[PK[0]])
            add("act", lambda e, mt=mt: e.copy(memT[:, :, mt * 128:(mt + 1) * 128], bank_bf(0).rearrange("p (a b) -> p a b", a=8)),
                reads=[PK[0]], writes=[k_memT])
        for mt in range(2):
            for cs3 in range(3):
                pbk = 1 + (mt * 3 + cs3) % 2
                for dc in range(8):
                    add("pe", lambda e, mt=mt, cs3=cs3, dc=dc, pbk=pbk: e.matmul(
                        bank(pbk), memT[:, dc, mt * 128:(mt + 1) * 128], wkv_t[:, dc, cs3 * 512:(cs3 + 1) * 512], start=(dc == 0), stop=(dc == 7)),
                        reads=[k_memT, k_wkv], writes=[PK[pbk]])
                add("act", lambda e, mt=mt, cs3=cs3, pbk=pbk: e.copy(mkv[:, mt, cs3 * 512:(cs3 + 1) * 512], bank(pbk)), reads=[PK[pbk]], writes=[k_mkv])
        for mt in range(2):
            src = mkv[:, mt, 0:768]
            rms_rstd(src, [k_mkv], 768, msq, k_msq, mss, k_mss, mrs, k_mrs, nh=4, hd=192)
            add("dve", lambda e, src=src: e.tensor_tensor(msq[:, 0:768].rearrange("p (h d) -> p h d", h=4), src.rearrange("p (h d) -> p h d", h=4),
                                                          bc(mrs[:, 0:4], [128, 4, 192], 2), ALU.mult), reads=[k_mkv, k_mrs], writes=[k_msq])
            add("dve", lambda e, mt=mt: e.tensor_tensor(mkn[:, mt, :].rearrange("p (h d) -> p h d", h=4), msq[:, 0:768].rearrange("p (h d) -> p h d", h=4),
                                                        bc(mknw_t, [128, 4, 192], 1), ALU.mult), reads=[k_msq, k_mknw], writes=[k_mkn])
            for h in range(4):
                add("pe", lambda e, mt=mt, h=h: e.transpose(bank_bf(3)[:, h * 128:(h + 1) * 128], mkn[:, mt, h * 192:h * 192 + 128], ident_b),
                    reads=[k_mkn, k_idb], writes=[PK[3]])
                add("pe", lambda e, mt=mt, h=h: e.transpose(bank_bf(4)[0:64, h * 128:(h + 1) * 128], mkn[:, mt, h * 192 + 128:(h + 1) * 192], ident_b),
                    reads=[k_mkn, k_idb], writes=[PK[4]])
            add("act", lambda e, mt=mt: e.copy(KmA[:, :, mt * 128:(mt + 1) * 128], bank_bf(3)[:, 0:512].rearrange("p (a b) -> p a b", a=4)),
                reads=[PK[3]], writes=[k_KmA])
            add("act", lambda e, mt=mt: e.copy(KmB[0:64, :, mt * 128:(mt + 1) * 128], bank_bf(4)[0:64, 0:512].rearrange("p (a b) -> p a b", a=4)),
                reads=[PK[4]], writes=[k_KmB])
            add("dve", lambda e, mt=mt: e.tensor_copy(VmA[:, mt, :, 0:192], mkv[:, mt, 768:1536].rearrange("p (h d) -> p h d", h=4)),
                reads=[k_mkv], writes=[k_VmA])
            add("pool", lambda e, mt=mt: e.memset(VmA[:, mt, :, 192:193], 1.0), writes=[("vm1", mt)])
        mq_r = [AR.get([128, 768], BF16) for _ in range(2)]
        mqA, k_mqA = AR.get([128, 4, 128], BF16)
        mqB, k_mqB = AR.get([128, 4, 128], BF16)
        Pm_r = [AR.get([128, 128], BF16) for _ in range(4)]
        rdn, k_rdn = AR.get([128, 4], F32)
        ym_r = [AR.get([128, 768], BF16) for _ in range(2)]
        pi = 0
        for tg in range(NT):
            t0 = tg * 128
            mqt, k_mqt = mq_r[tg % 2]
            add("sp", lambda e, mqt=mqt, t0=t0: e.dma_start(out=mqt, in_=mq_s[t0:t0 + 128, :]), writes=[k_mqt], dma_key=("m_mq", tg % 2))
            for h in range(4):
                add("pe", lambda e, mqt=mqt, h=h: e.transpose(bank_bf(3)[:, h * 128:(h + 1) * 128], mqt[:, h * 192:h * 192 + 128], ident_b),
                    reads=[k_mqt, k_idb], writes=[PK[3]])
                add("pe", lambda e, mqt=mqt, h=h: e.transpose(bank_bf(4)[0:64, h * 128:(h + 1) * 128], mqt[:, h * 192 + 128:(h + 1) * 192], ident_b),
                    reads=[k_mqt, k_idb], writes=[PK[4]])
            add("act", lambda e: e.copy(mqA.rearrange("p a b -> p (a b)"), bank_bf(3)[:, 0:512]), reads=[PK[3]], writes=[k_mqA])
            add("act", lambda e: e.copy(mqB[0:64].rearrange("p a b -> p (a b)"), bank_bf(4)[0:64, 0:512]), reads=[PK[4]], writes=[k_mqB])
            ob0 = 6
            for h in range(4):
                for mt in range(2):
                    spb = 1 + (pi % 2)
                    sslot = bank(spb)[:, ((pi // 2) % 4) * 128:((pi // 2) % 4 + 1) * 128]
                    ksl = ("pss", spb, (pi // 2) % 4)
                    P, k_P = Pm_r[pi % 4]
                    add("pe", lambda e, sslot=sslot, h=h, mt=mt: e.matmul(sslot, KmA[:, h, mt * 128:(mt + 1) * 128], mqA[:, h, :], start=True, stop=False),
                        reads=[k_KmA, k_mqA], writes=[ksl])
                    add("pe", lambda e, sslot=sslot, h=h, mt=mt: e.matmul(sslot, KmB[0:64, h, mt * 128:(mt + 1) * 128], mqB[0:64, h, :], start=False, stop=True),
                        reads=[k_KmB, k_mqB], writes=[ksl])
                    add("act", lambda e, P=P, sslot=sslot: e.activation(out=P, in_=sslot, func=AF.Exp, scale=192.0 ** -0.5), reads=[ksl], writes=[k_P])
                    pbk = ob0 + h // 2
                    add("pe", lambda e, P=P, h=h, mt=mt, pbk=pbk: e.matmul(bank(pbk)[:, (h % 2) * 256:(h % 2) * 256 + 193], P, VmA[:, mt, h, :],
                                                                           start=(mt == 0), stop=(mt == 1)),
                        reads=[k_P, k_VmA, ("vm1", 0), ("vm1", 1)], writes=[PK[pbk]])
                    pi += 1
            ym, k_ym = ym_r[tg % 2]
            pv = bank(6, 2).rearrange("p (h c) -> p h c", h=4)
            add("dve", lambda e, pv=pv: e.tensor_copy(rdn, pv[:, :, 192]), reads=[PK[6], PK[7]], writes=[k_rdn])
            add("dve", lambda e: e.reciprocal(rdn, rdn), reads=[k_rdn], writes=[k_rdn])
            add("dve", lambda e, pv=pv, ym=ym: e.tensor_tensor(ym.rearrange("p (h d) -> p h d", h=4), pv[:, :, 0:192], bc(rdn, [128, 4, 192], 2), ALU.mult),
                reads=[PK[6], PK[7], k_rdn], writes=[k_ym])
            add("pool", lambda e, ym=ym, t0=t0: e.dma_start(out=ymem_s[t0:t0 + 128, :], in_=ym), reads=[k_ym], dma_key=("m_ym", tg % 2))
        S_.barrier("m")

        AR.reset()
        wsb_t, k_wsb = AR.get([128, 8, D], BF16)
        wdb_t, k_wdb = AR.get([128, 2, D], BF16)
        wmb_t, k_wmb = AR.get([128, 8, D], BF16)
        wo_t, k_wo = AR.get([128, 8, D], BF16)
        nfw_t, k_nfw = AR.get([128, D], F32)
        add("pool", lambda e: e.dma_start(out=wsb_t, in_=wsb), writes=[k_wsb], dma_key="g_c0")
        add("pool", lambda e: e.dma_start(out=wdb_t, in_=wdb), writes=[k_wdb], dma_key="g_c1")
        add("pool", lambda e: e.dma_start(out=wmb_t, in_=wmb), writes=[k_wmb], dma_key="g_c2")
        add("pool", lambda e: e.dma_start(out=wo_t, in_=wo), writes=[k_wo], dma_key="g_c3")
        add("sp", lambda e: e.dma_start(out=nfw_t, in_=nfw), writes=[k_nfw], dma_key="g_c4")
        ys_r = [AR.get([128, D], BF16) for _ in range(2)]
        odl_r = [AR.get([128, 3, 260], F32) for _ in range(2)]
        yml_r = [AR.get([128, 768], BF16) for _ in range(2)]
        gl_r = [AR.get([128, 24, 128], BF16) for _ in range(2)]
        xl_r = [AR.get([128, D], F32) for _ in range(2)]
        oacc, k_oacc = AR.get([128, 260], F32)
        rdd, k_rdd = AR.get([128, 4], F32)
        ydl, k_ydl = AR.get([128, 256], BF16)
        yT, k_yT = AR.get([128, 18, 128], BF16)
        gm, k_gm = AR.get([128, 3, 128], F32)
        mT, k_mT = AR.get([128, 8, 128], BF16)
        hf_r = [AR.get([128, D], F32) for _ in range(2)]
        hsq, k_hsq = AR.get([128, D], F32)
        hss, k_hss = AR.get([128, 16], F32)
        hrs, k_hrs = AR.get([128, 16], F32)
        hnb, k_hnb = AR.get([128, D], BF16)
        hT_r = [AR.get([128, 8, 128], BF16) for _ in range(2)]
        g_v = g_s.rearrange("(c p) t -> p c t", p=128)
        for tg in range(NT):
            t0 = tg * 128
            ys, k_ys = ys_r[tg % 2]
            odl, k_odl = odl_r[tg % 2]
            yml, k_yml = yml_r[tg % 2]
            gl, k_gl = gl_r[tg % 2]
            xl, k_xl = xl_r[tg % 2]
            add("sp", lambda e, ys=ys, t0=t0: e.dma_start(out=ys, in_=yssd_s[t0:t0 + 128, :]), writes=[k_ys], dma_key=("g_ys", tg % 2))
            add("sp", lambda e, odl=odl, t0=t0: e.dma_start(out=odl, in_=od_s[:, t0:t0 + 128, :].rearrange("g t c -> t g c")), writes=[k_odl], dma_key=("g_od", tg % 2))
            add("sp", lambda e, yml=yml, t0=t0: e.dma_start(out=yml, in_=ymem_s[t0:t0 + 128, :]), writes=[k_yml], dma_key=("g_ym", tg % 2))
            add("sp", lambda e, gl=gl, t0=t0: e.dma_start(out=gl, in_=g_v[:, :, t0:t0 + 128]), writes=[k_gl], dma_key=("g_gl", tg % 2))
            add("sp", lambda e, xl=xl, t0=t0: e.dma_start(out=xl, in_=x[t0:t0 + 128, :]), writes=[k_xl], dma_key=("g_xl", tg % 2))
            add("dve", lambda e, odl=odl: e.tensor_tensor(oacc, odl[:, 0, :], odl[:, 1, :], ALU.add), reads=[k_odl], writes=[k_oacc])
            add("dve", lambda e, odl=odl: e.tensor_tensor(oacc, oacc, odl[:, 2, :], ALU.add), reads=[k_odl, k_oacc], writes=[k_oacc])
            ov = oacc.rearrange("p (h c) -> p h c", h=4)
            add("dve", lambda e, ov=ov: e.tensor_copy(rdd, ov[:, :, 64]), reads=[k_oacc], writes=[k_rdd])
            add("dve", lambda e: e.reciprocal(rdd, rdd), reads=[k_rdd], writes=[k_rdd])
            add("dve", lambda e, ov=ov: e.tensor_tensor(ydl.rearrange("p (h d) -> p h d", h=4), ov[:, :, 0:64], bc(rdd, [128, 4, 64], 2), ALU.mult),
                reads=[k_oacc, k_rdd], writes=[k_ydl])
            for kc in range(8):
                add("pe", lambda e, kc=kc, ys=ys: e.transpose(bank_bf(0)[:, kc * 128:(kc + 1) * 128], ys[:, kc * 128:(kc + 1) * 128], ident_b),
                    reads=[k_ys, k_idb], writes=[PK[0]])
            add("act", lambda e: e.copy(yT[:, 0:8, :].rearrange("p a b -> p (a b)"), bank_bf(0)), reads=[PK[0]], writes=[k_yT])
            for kc in range(2):
                add("pe", lambda e, kc=kc: e.transpose(bank_bf(1)[:, kc * 128:(kc + 1) * 128], ydl[:, kc * 128:(kc + 1) * 128], ident_b),
                    reads=[k_ydl, k_idb], writes=[PK[1]])
            for h in range(4):
                add("pe", lambda e, h=h, yml=yml: e.transpose(bank_bf(1)[:, (2 + h) * 128:(3 + h) * 128], yml[:, h * 192:h * 192 + 128], ident_b),
                    reads=[k_yml, k_idb], writes=[PK[1]])
                add("pe", lambda e, h=h, yml=yml: e.transpose(bank_bf(2)[0:64, h * 128:(h + 1) * 128], yml[:, h * 192 + 128:(h + 1) * 192], ident_b),
                    reads=[k_yml, k_idb], writes=[PK[2]])
            add("act", lambda e: e.copy(yT[:, 8:10, :].rearrange("p a b -> p (a b)"), bank_bf(1)[:, 0:256]), reads=[PK[1]], writes=[k_yT])
            for h in range(4):
                add("act", lambda e, h=h: e.copy(yT[:, 10 + 2 * h, :], bank_bf(1)[:, (2 + h) * 128:(3 + h) * 128]), reads=[PK[1]], writes=[k_yT])
                add("act", lambda e, h=h: e.copy(yT[0:64, 11 + 2 * h, :], bank_bf(2)[0:64, h * 128:(h + 1) * 128]), reads=[PK[2]], writes=[k_yT])
            for dmc in range(8):
                pbk = 3 + dmc % 2
                dsl = slice(dmc * 128, (dmc + 1) * 128)
                for kc in range(8):
                    add("pe", lambda e, kc=kc, dsl=dsl, pbk=pbk: e.matmul(bank(pbk)[:, 0:128], wsb_t[:, kc, dsl], yT[:, kc, :], start=(kc == 0), stop=(kc == 7)),
                        reads=[k_wsb, k_yT], writes=[PK[pbk]])
                for kc in range(2):
                    add("pe", lambda e, kc=kc, dsl=dsl, pbk=pbk: e.matmul(bank(pbk)[:, 128:256], wdb_t[:, kc, dsl], yT[:, 8 + kc, :], start=(kc == 0), stop=(kc == 1)),
                        reads=[k_wdb, k_yT], writes=[PK[pbk]])
                for kc in range(8):
                    if kc % 2 == 0:
                        add("pe", lambda e, kc=kc, dsl=dsl, pbk=pbk: e.matmul(bank(pbk)[:, 256:384], wmb_t[:, kc, dsl], yT[:, 10 + kc, :], start=(kc == 0), stop=False),
                            reads=[k_wmb, k_yT], writes=[PK[pbk]])
                    else:
                        add("pe", lambda e, kc=kc, dsl=dsl, pbk=pbk: e.matmul(bank(pbk)[:, 256:384], wmb_t[0:64, kc, dsl], yT[0:64, 10 + kc, :], start=False, stop=(kc == 7)),
                            reads=[k_wmb, k_yT], writes=[PK[pbk]])
                add("dve", lambda e, dmc=dmc, pbk=pbk, gl=gl: e.tensor_tensor(gm, bank(pbk)[:, 0:384].rearrange("p (a b) -> p a b", a=3), gl[:, dmc:24:8, :], ALU.mult),
                    reads=[PK[pbk], k_gl], writes=[k_gm])
                add("dve", lambda e: e.tensor_tensor(gm[:, 0, :], gm[:, 0, :], gm[:, 1, :], ALU.add), reads=[k_gm], writes=[k_gm])
                add("dve", lambda e, dmc=dmc: e.tensor_tensor(mT[:, dmc, :], gm[:, 0, :], gm[:, 2, :], ALU.add), reads=[k_gm], writes=[k_mT])
            for half in range(2):
                for kc in range(8):
                    add("pe", lambda e, kc=kc, half=half: e.matmul(bank(5 + half), mT[:, kc, :], wo_t[:, kc, half * 512:(half + 1) * 512], start=(kc == 0), stop=(kc == 7)),
                        reads=[k_mT, k_wo], writes=[PK[5 + half]])
            hf, k_hf = hf_r[tg % 2]
            add("dve", lambda e, hf=hf, xl=xl: e.tensor_tensor(hf, xl, bank(5, 2), ALU.add), reads=[k_xl, PK[5], PK[6]], writes=[k_hf])
            add("pool", lambda e, hf=hf, t0=t0: e.dma_start(out=h_s[t0:t0 + 128, :], in_=hf), reads=[k_hf], dma_key=("g_hf", tg % 2))
            rms_rstd(hf, [k_hf], D, hsq, k_hsq, hss, k_hss, hrs, k_hrs)
            add("dve", lambda e, hf=hf: e.scalar_tensor_tensor(out=hnb, in0=hf, scalar=hrs[:, 0:1], in1=nfw_t, op0=ALU.mult, op1=ALU.mult),
                reads=[k_hf, k_hrs, k_nfw], writes=[k_hnb])
            for dc in range(8):
                add("pe", lambda e, dc=dc: e.transpose(bank_bf(7)[:, dc * 128:(dc + 1) * 128], hnb[:, dc * 128:(dc + 1) * 128], ident_b),
                    reads=[k_hnb, k_idb], writes=[PK[7]])
            hT, k_hT = hT_r[tg % 2]
            add("act", lambda e, hT=hT: e.copy(hT.rearrange("p a b -> p (a b)"), bank_bf(7)), reads=[PK[7]], writes=[k_hT])
            add("pool", lambda e, hT=hT, tg=tg: e.dma_start(out=hnT_s[tg], in_=hT), reads=[k_hT], dma_key=("g_hT", tg % 2))
        S_.barrier("g")

        AR.reset()
        wq_t, k_wq = AR.get([128, 8, 2048], BF16)
        sk_t, k_sk = AR.get([128, 16, 128], BF16)
        iota16, k_i16 = AR.get([128, 16], F32)
        add("pool", lambda e: e.dma_start(out=wq_t, in_=wq), writes=[k_wq], dma_key="r_c0")
        add("pool", lambda e: e.dma_start(out=sk_t, in_=skT), writes=[k_sk], dma_key="r_c1")
        add("dve", lambda e: e.tensor_copy(iota16, iota_f[:, 0:16]), reads=[k_iota], writes=[k_i16])
        hl_r = [AR.get([128, 8, 128], BF16) for _ in range(2)]
        qT, k_qT = AR.get([128, 16, 128], BF16)
        sc, k_sc = AR.get([128, 16, 128], F32)
        wk, k_wk = AR.get([128, 16, 128], F32)
        mx, k_mx = AR.get([128, 16, 16], F32)
        mi, k_mi = AR.get([128, 16, 16], U32)
        mif, k_mif = AR.get([128, 16, 16], F32)
        cand, k_cand = AR.get([128, 8, 256], F32)
        wk2, k_wk2 = AR.get([128, 8, 256], F32)
        top, k_top = AR.get([128, 8, 16], F32)
        pos, k_pos = AR.get([128, 8, 16], U32)
        posf, k_pf = AR.get([128, 8, 16], F32)
        pa, k_pa = AR.get([128, 8, 16], F32)
        pbb, k_pb = AR.get([128, 8, 16], F32)
        oh, k_oh = AR.get([128, 8, 16, 16], F32)
        gex, k_gex = AR.get([128, 8, 16], F32)
        gz, k_gz = AR.get([128, 8], F32)
        rt, k_rt = AR.get([128, 3, 128], F32)
        rT_r = [AR.get([128, 3, 128], F32) for _ in range(2)]
        for tg in range(NT):
            hl, k_hl = hl_r[tg % 2]
            add("sp", lambda e, hl=hl, tg=tg: e.dma_start(out=hl, in_=hnT_s[tg]), writes=[k_hl], dma_key=("r_hl", tg % 2))
            for f4 in range(4):
                pbk = f4 % 2
                for fi in range(4):
                    fc = f4 * 4 + fi
                    for dc in range(8):
                        add("pe", lambda e, fc=fc, fi=fi, dc=dc, pbk=pbk, hl=hl: e.matmul(
                            bank(pbk)[:, fi * 128:(fi + 1) * 128], wq_t[:, dc, fc * 128:(fc + 1) * 128], hl[:, dc, :], start=(dc == 0), stop=(dc == 7)),
                            reads=[k_wq, k_hl], writes=[PK[pbk]])
                add("act", lambda e, f4=f4, pbk=pbk: e.copy(qT[:, f4 * 4:(f4 + 1) * 4, :].rearrange("p a b -> p (a b)"), bank(pbk)),
                    reads=[PK[pbk]], writes=[("qT", f4)])
            for f4 in range(4):
                pbk = 2 + f4 % 2
                for fi in range(4):
                    fc = f4 * 4 + fi
                    add("pe", lambda e, fc=fc, fi=fi, pbk=pbk: e.matmul(bank(pbk)[:, fi * 128:(fi + 1) * 128], qT[:, fc, :], sk_t[:, fc, :], start=True, stop=True),
                        reads=[("qT", f4), k_sk], writes=[PK[pbk]])
                add("act", lambda e, f4=f4, pbk=pbk: e.copy(sc[:, f4 * 4:(f4 + 1) * 4, :].rearrange("p a b -> p (a b)"), bank(pbk)),
                    reads=[PK[pbk]], writes=[("sc", f4)])
            for fc in range(16):
                add("dve", lambda e, fc=fc: e.max(out=mx[:, fc, 0:8], in_=sc[:, fc, :]), reads=[("sc", fc // 4)], writes=[("mx", fc)])
            for fc in range(16):
                add("dve", lambda e, fc=fc: e.max_index(mi[:, fc, 0:8], mx[:, fc, 0:8], sc[:, fc, :]), reads=[("sc", fc // 4), ("mx", fc)], writes=[("mi", fc)])
            for fc in range(16):
                add("dve", lambda e, fc=fc: e.match_replace(out=wk[:, fc, :], in_to_replace=mx[:, fc, 0:8], in_values=sc[:, fc, :], imm_value=-1e30),
                    reads=[("sc", fc // 4), ("mx", fc)], writes=[("wk", fc)])
            for fc in range(16):
                add("dve", lambda e, fc=fc: e.max(out=mx[:, fc, 8:16], in_=wk[:, fc, :]), reads=[("wk", fc)], writes=[("mx2", fc)])
            for fc in range(16):
                add("dve", lambda e, fc=fc: e.max_index(mi[:, fc, 8:16], mx[:, fc, 8:16], wk[:, fc, :]), reads=[("wk", fc), ("mx2", fc)], writes=[("mi2", fc)])
            allmx = [("mx", f) for f in range(16)] + [("mx2", f) for f in range(16)]
            allmi = [("mi", f) for f in range(16)] + [("mi2", f) for f in range(16)]
            add("dve", lambda e: e.tensor_copy(mif, mi), reads=allmi, writes=[k_mif])
            mxv = mx.rearrange("p (h c) k -> p h c k", c=2)
            mfv = mif.rearrange("p (h c) k -> p h c k", c=2)
            add("dve", lambda e, mxv=mxv: e.tensor_tensor(cand.rearrange("p h (a b) -> p h a b", a=16), bc(mxv[:, :, 0, :], [128, 8, 16, 16], 3),
                                                          bc(mxv[:, :, 1, :], [128, 8, 16, 16], 2), ALU.add), reads=allmx, writes=[k_cand])
            for h in range(8):
                add("dve", lambda e, h=h: e.max(out=top[:, h, 0:8], in_=cand[:, h, :]), reads=[k_cand], writes=[("top", h)])
            for h in range(8):
                add("dve", lambda e, h=h: e.max_index(pos[:, h, 0:8], top[:, h, 0:8], cand[:, h, :]), reads=[k_cand, ("top", h)], writes=[("pos", h)])
            for h in range(8):
                add("dve", lambda e, h=h: e.match_replace(out=wk2[:, h, :], in_to_replace=top[:, h, 0:8], in_values=cand[:, h, :], imm_value=-1e30),
                    reads=[k_cand, ("top", h)], writes=[("wk2", h)])
            for h in range(8):
                add("dve", lambda e, h=h: e.max(out=top[:, h, 8:16], in_=wk2[:, h, :]), reads=[("wk2", h)], writes=[("top2", h)])
            for h in range(8):
                add("dve", lambda e, h=h: e.max_index(pos[:, h, 8:16], top[:, h, 8:16], wk2[:, h, :]), reads=[("wk2", h), ("top2", h)], writes=[("pos2", h)])
            alltop = [("top", h) for h in range(8)] + [("top2", h) for h in range(8)]
            allpos = [("pos", h) for h in range(8)] + [("pos2", h) for h in range(8)]
            add("dve", lambda e: e.tensor_tensor(gex, top, bc(top[:, :, 0], [128, 8, 16], 2), ALU.subtract), reads=alltop, writes=[k_gex])
            add("act", lambda e: e.activation(out=gex, in_=gex, func=AF.Exp), reads=[k_gex], writes=[k_gex])
            add("dve", lambda e: e.tensor_reduce(out=gz, in_=gex, axis=AX.X, op=ALU.add), reads=[k_gex], writes=[k_gz])
            add("dve", lambda e: e.reciprocal(gz, gz), reads=[k_gz], writes=[k_gz])
            add("dve", lambda e: e.tensor_tensor(rt[:, 2, :].rearrange("p (h k) -> p h k", h=8), gex, bc(gz, [128, 8, 16], 2), ALU.mult),
                reads=[k_gex, k_gz], writes=[("rt", 2)])
            add("dve", lambda e: e.tensor_copy(posf, pos), reads=allpos, writes=[k_pf])
            add("dve", lambda e: e.tensor_scalar(pa, posf, -7.5, 0.0625, ALU.add, ALU.mult), reads=[k_pf], writes=[k_pa])
            add("dve", lambda e: e.tensor_single_scalar(pa, pa, MAGIC, ALU.add), reads=[k_pa], writes=[k_pa])
            add("dve", lambda e: e.tensor_single_scalar(pa, pa, -MAGIC, ALU.add), reads=[k_pa], writes=[k_pa])
            add("dve", lambda e: e.scalar_tensor_tensor(out=pbb, in0=pa, scalar=-16.0, in1=posf, op0=ALU.mult, op1=ALU.add), reads=[k_pa, k_pf], writes=[k_pb])
            for which, src, k_src, cidx in ((0, pa, k_pa, 0), (1, pbb, k_pb, 1)):
                add("dve", lambda e, src=src: e.tensor_tensor(oh, bc(src, [128, 8, 16, 16], 3),
                                                              iota16.unsqueeze(1).unsqueeze(1).to_broadcast([128, 8, 16, 16]), ALU.is_equal),
                    reads=[k_src, k_i16], writes=[k_oh])
                add("dve", lambda e, cidx=cidx, mfv=mfv: e.tensor_tensor(oh, oh, bc(mfv[:, :, cidx, :], [128, 8, 16, 16], 2), ALU.mult),
                    reads=[k_oh, k_mif], writes=[k_oh])
                add("dve", lambda e, which=which: e.tensor_reduce(out=rt[:, which, :].rearrange("p (h k) -> p h k", h=8), in_=oh, axis=AX.X, op=ALU.add),
                    reads=[k_oh], writes=[("rt", which)])
            rT, k_rT = rT_r[tg % 2]
            for w3 in range(3):
                add("pe", lambda e, w3=w3: e.transpose(bank(4)[:, w3 * 128:(w3 + 1) * 128], rt[:, w3, :], ident_f),
                    reads=[("rt", w3), k_idf], writes=[PK[4]])
            add("act", lambda e, rT=rT: e.copy(rT.rearrange("p a b -> p (a b)"), bank(4)[:, 0:384]), reads=[PK[4]], writes=[k_rT])
            add("pool", lambda e, rT=rT, tg=tg: e.dma_start(out=r_s[tg], in_=rT), reads=[k_rT], dma_key=("r_rT", tg % 2))
        S_.barrier("r")

        AR.reset()
        NB2 = S // 256
        hT2_r = [AR.get([128, 8, 256], BF16) for _ in range(2)]
        rl_r = [AR.get([128, 2, 3, 128], F32) for _ in range(2)]
        A_r = [AR.get([128, 64, 128], BF16) for _ in range(1)]
        B_r = [AR.get([128, 64, 128], BF16) for _ in range(1)]
        M_t, k_M = AR.get([128, 256, 128], BF16)
        dW_r = [AR.get([128, 4, D], BF16) for _ in range(3)]
        uW_r = [AR.get([128, 4, D], BF16) for _ in range(3)]
        ge_r = [AR.get([128, 256], BF16) for _ in range(4)]
        co_r = [AR.get([128, 256], BF16) for _ in range(4)]
        hl2_r = [AR.get([128, D], F32) for _ in range(2)]
        LA = 2
        oi2 = 0
        wcount = [0]
        for b2 in range(NB2):
            hT2, k_hT2 = hT2_r[b2 % 2]
            rl, k_rl = rl_r[b2 % 2]
            for tt in range(2):
                tg = b2 * 2 + tt
                add("sp", lambda e, hT2=hT2, tt=tt, tg=tg: e.dma_start(out=hT2[:, :, tt * 128:(tt + 1) * 128], in_=hnT_s[tg]),
                    writes=[k_hT2], dma_key=("p_h", b2 % 2))
                add("sp", lambda e, rl=rl, tt=tt, tg=tg: e.dma_start(out=rl[:, tt, :, :], in_=r_s[tg]), writes=[k_rl], dma_key=("p_r", b2 % 2))
            for sb in range(4):
                tt, to = sb // 2, (sb % 2) * 64
                A_, k_A = A_r[0]
                B_, k_B = B_r[0]
                i1 = rl[:, tt, 0, to:to + 64]
                i2 = rl[:, tt, 1, to:to + 64]
                gg = rl[:, tt, 2, to:to + 64]
                add("dve", lambda e, B_=B_, i2=i2: e.tensor_tensor(B_, bc(iota_f, [128, 64, 128], 1), bc(i2, [128, 64, 128], 2), ALU.is_equal),
                    reads=[k_rl, k_iota], writes=[k_B])
                add("pool", lambda e, B_=B_, gg=gg: e.tensor_tensor(B_, B_, bc(gg, [128, 64, 128], 2), ALU.mult), reads=[k_rl, k_B], writes=[k_B])
                add("dve", lambda e, A_=A_, i1=i1: e.tensor_tensor(A_, bc(iota_f, [128, 64, 128], 1), bc(i1, [128, 64, 128], 2), ALU.is_equal),
                    reads=[k_rl, k_iota], writes=[k_A])
                for q in range(16):
                    pbk = 4 + (q % 4)
                    for t4 in range(4):
                        tk = q * 4 + t4
                        add("pe", lambda e, A_=A_, B_=B_, tk=tk, t4=t4, pbk=pbk: e.matmul(
                            bank(pbk)[:, t4 * 128:(t4 + 1) * 128], A_[:, tk, :], B_[:, tk, :], start=True, stop=True),
                            reads=[k_A, k_B], writes=[PK[pbk]])
                    tb = sb * 64 + q * 4
                    add("act", lambda e, tb=tb, pbk=pbk: e.copy(M_t[:, tb:tb + 4, :].rearrange("p a b -> p (a b)"), bank(pbk)),
                        reads=[PK[pbk]], writes=[("M", sb)])
            allM = [("M", sb) for sb in range(4)]
            tiles = {}

            def load_w(jq):
                wi = wcount[0]
                wcount[0] += 1
                dW, k_dW = dW_r[wi % 3]
                uW, k_uW = uW_r[wi % 3]
                add("sp", lambda e, dW=dW, jq=jq: e.dma_start(out=dW, in_=dnT_b[jq * 4:(jq + 1) * 4].rearrange("j p f -> p j f")),
                    writes=[k_dW], dma_key=("p_dw", wi % 3))
                add("sp", lambda e, uW=uW, jq=jq: e.dma_start(out=uW, in_=up_b[jq * 4:(jq + 1) * 4].rearrange("j p f -> p j f")),
                    writes=[k_uW], dma_key=("p_uw", wi % 3))
                tiles[jq] = (dW, k_dW, uW, k_uW)

            def act_mm(j):
                dW, k_dW, uW, k_uW = tiles[j // 4]
                jj = j % 4
                pbk = 4 + (j % 4)
                for dc in range(8):
                    add("pe", lambda e, dW=dW, jj=jj, dc=dc, pbk=pbk, hT2=hT2: e.matmul(
                        bank(pbk)[:, 0:256], dW[:, jj, dc * 128:(dc + 1) * 128], hT2[:, dc, :], start=(dc == 0), stop=(dc == 7)),
                        reads=[k_dW, k_hT2], writes=[PK[pbk]])

            load_w(0)
            load_w(1)
            for j0 in range(LA):
                act_mm(j0)
            for j in range(128):
                jn = j + LA
                if jn < 128:
                    if jn % 4 == 0 and (jn // 4 + 1) < 32:
                        load_w(jn // 4 + 1)
                    act_mm(jn)
                dW, k_dW, uW, k_uW = tiles[j // 4]
                jj = j % 4
                pbk = 4 + (j % 4)
                ge, k_ge = ge_r[j % 4]
                co, k_co = co_r[j % 4]
                add("act", lambda e, ge=ge, pbk=pbk: e.activation(out=ge, in_=bank(pbk)[:, 0:256], func=AF.Gelu), reads=[PK[pbk]], writes=[k_ge])
                add("dve", lambda e, ge=ge, co=co, j=j: e.tensor_tensor(co, ge, M_t[:, :, j], ALU.mult), reads=[k_ge] + allM, writes=[k_co])
                for tt in range(2):
                    for half in range(2):
                        add("pe", lambda e, co=co, uW=uW, jj=jj, tt=tt, half=half, j=j: e.matmul(
                            bank(tt * 2 + half), co[:, tt * 128:(tt + 1) * 128], uW[:, jj, half * 512:(half + 1) * 512],
                            start=(j == 0), stop=(j == 127)), reads=[k_co, k_uW], writes=[PK[tt * 2 + half]])
            for tt in range(2):
                tg = b2 * 2 + tt
                t0 = tg * 128
                hl2, k_hl2 = hl2_r[oi2 % 2]
                add("sp", lambda e, hl2=hl2, t0=t0: e.dma_start(out=hl2, in_=h_s[t0:t0 + 128, :]), writes=[k_hl2], dma_key=("p_hl", oi2 % 2))
                add("dve", lambda e, hl2=hl2, tt=tt: e.tensor_tensor(hl2, hl2, bank(tt * 2, 2), ALU.add),
                    reads=[k_hl2, PK[tt * 2], PK[tt * 2 + 1]], writes=[k_hl2])
                add("pool", lambda e, hl2=hl2, t0=t0: e.dma_start(out=out[t0:t0 + 128, :], in_=hl2), reads=[k_hl2], dma_key=("p_out", oi2 % 2))
                oi2 += 1
        S_.emit(st)
        build_program.stats = S_.stats
    return nc


def _rep(v, n=128):
    return np.ascontiguousarray(np.broadcast_to(np.asarray(v, dtype=np.float32).reshape(1, -1), (n, np.asarray(v).size)))


def _kmaj(w, nchunk):
    w = np.asarray(w, dtype=np.float32)
    return np.ascontiguousarray(w.reshape(nchunk, 128, w.shape[1]).transpose(1, 0, 2))


def shared_inputs(inp):
    L = 0
    sh = {}
    sh["nmw"] = _rep(inp["norm_mix_w"][L])
    sh["w_in_r"] = _kmaj(inp["w_in"][L], 8)
    cwv = np.asarray(inp["ssd_conv_w"][L], dtype=np.float32)
    sh["cw"] = np.ascontiguousarray(cwv.reshape(4, 16, 128).transpose(2, 1, 0))
    sh["cb"] = np.ascontiguousarray(np.asarray(inp["ssd_conv_b"][L], dtype=np.float32).reshape(16, 128).T)
    sh["dtb"] = _rep(inp["ssd_dt_bias"][L])
    sh["alog"] = _rep(inp["ssd_a_log"][L])
    sh["sdd"] = _rep(inp["ssd_d"][L])
    sh["snw"] = _rep(inp["ssd_norm_w"][L])
    sh["qnw"] = _rep(inp["dil_q_norm_w"][L])
    sh["knw"] = _rep(inp["dil_k_norm_w"][L])
    sh["mnw"] = _rep(inp["mem_norm_w"][L])
    sh["wkv"] = _kmaj(inp["w_mem_kv"][L], 8)
    sh["mqnw"] = _rep(inp["mem_q_norm_w"][L])
    sh["mknw"] = _rep(inp["mem_k_norm_w"][L])
    sh["wsb"] = _kmaj(inp["w_ssd_br"][L], 8)
    sh["wdb"] = _kmaj(inp["w_dil_br"][L], 2)
    wm = np.asarray(inp["w_mem_br"][L], dtype=np.float32)
    wmp = np.zeros((128, 8, D), dtype=np.float32)
    for h in range(4):
        wmp[:, 2 * h, :] = wm[h * 192:h * 192 + 128]
        wmp[0:64, 2 * h + 1, :] = wm[h * 192 + 128:(h + 1) * 192]
    sh["wmb"] = wmp
    sh["wo"] = _kmaj(inp["w_out"][L], 8)
    sh["nfw"] = _rep(inp["norm_ffn_w"][L])
    sh["wq"] = _kmaj(inp["peer_w_query"][L], 8)
    sk = np.asarray(inp["peer_sub_keys"][L], dtype=np.float32)
    sh["skT"] = np.ascontiguousarray(sk.reshape(16, 128, 128).transpose(2, 0, 1))
    dn = np.asarray(inp["peer_down"][L], dtype=np.float32).reshape(128, 128, 8, 128)
    sh["dnT_r"] = np.ascontiguousarray(dn.transpose(1, 3, 2, 0)).reshape(128, 128, D)
    up = np.asarray(inp["peer_up"][L], dtype=np.float32).reshape(128, 128, D)
    sh["up_r"] = np.ascontiguousarray(up.transpose(1, 0, 2))
    return sh


def core_inputs(inp, b, S):
    NT = S // 128
    posb = np.asarray(inp["positions"][b][:S], dtype=np.int32)
    return {
        "x": np.ascontiguousarray(np.asarray(inp["x"][b][:S], dtype=np.float32)),
        "mem": np.ascontiguousarray(np.asarray(inp["mem"][b], dtype=np.float32)),
        "pos_t": np.ascontiguousarray(posb.reshape(NT, 128).T),
    }


def kernel(**inputs):
    B, S = inputs["x"].shape[0], inputs["x"].shape[1]
    nc = build_program(S)
    sh = shared_inputs(inputs)
    in_maps = []
    for b in range(B):
        m = dict(sh)
        m.update(core_inputs(inputs, b, S))
        in_maps.append(m)
    res = run_bass_kernel_spmd(nc, in_maps, core_ids=list(range(B)))
    return np.stack([np.asarray(r["out"], dtype=np.float32) for r in res.results], axis=0)
```

```python
import math
from contextlib import ExitStack

import numpy as np
import concourse.bass as bass
import concourse.mybir as mybir
from concourse.bass_utils import run_bass_kernel_spmd

F32 = mybir.dt.float32
BF16 = mybir.dt.bfloat16
U32 = mybir.dt.uint32
I32 = mybir.dt.int32
AF = mybir.ActivationFunctionType
ALU = mybir.AluOpType
AX = mybir.AxisListType

D = 1024
EPS = 1e-6
NPROJ = 9232
OFF_Z, OFF_XBC, OFF_DT, OFF_DQ, OFF_DK, OFF_DV, OFF_MQ, OFF_G = 0, 1024, 3072, 3088, 3856, 4624, 5392, 6160
DILS = (1, 4, 16)
MAGIC = 12582912.0


class Sched:
    ENGS = ("pe", "act", "dve", "pool", "sp")

    def __init__(self, nc):
        self.nc = nc
        self.ops = []
        self.last_w = {}
        self.readers = {}
        self.dma_count = {}
        self.slots = {}
        self.dead = False
        self.stop_after = None

    def add(self, eng, fn, reads=(), writes=(), dma_key=None, wait_all_dma=False):
        if self.dead:
            return -1
        idx = len(self.ops)
        deps = {}
        is_dma = dma_key is not None
        if is_dma:
            dma_key = self.slots.setdefault((eng, dma_key), (eng, sum(1 for k in self.slots if k[0] == eng)))

        def dep(j, kind):
            o = self.ops[j]
            if o["dma"] is None and o["eng"] == eng and not is_dma:
                if eng == "pe":
                    return
            deps[j] = True

        for o in reads:
            if o in self.last_w:
                dep(self.last_w[o], "raw")
        for o in writes:
            if o in self.last_w:
                dep(self.last_w[o], "waw")
            for r in self.readers.get(o, {}).values():
                dep(r, "war")
        for o in writes:
            self.last_w[o] = idx
            self.readers[o] = {}
        for o in reads:
            rk = ("dma", idx) if is_dma else eng
            self.readers.setdefault(o, {})[rk] = idx
        dma_waits = {}
        comp_deps = []
        for j in deps:
            o = self.ops[j]
            if o["dma"] is not None:
                dma_waits[o["dma"]] = self.dma_count[o["dma"]]
            else:
                comp_deps.append(j)
        if wait_all_dma:
            for k, c in self.dma_count.items():
                dma_waits[k] = c
        if is_dma:
            self.dma_count[dma_key] = self.dma_count.get(dma_key, 0) + 1
        self.ops.append(dict(eng=eng, fn=fn, dma=dma_key, comp_deps=comp_deps,
                             dma_waits=dma_waits, signal=False, seq=None))
        return idx

    def barrier(self, tag):
        bt = self.bar_tile
        fns = {"pe": lambda eng: eng.nop(), "sp": lambda eng: eng.nop(),
               "act": lambda eng: eng.copy(bt[:, 0:1], bt[:, 4:5]),
               "dve": lambda eng: eng.tensor_copy(bt[:, 1:2], bt[:, 4:5]),
               "pool": lambda eng: eng.memset(bt[:, 2:3], 0.0)}
        for e in self.ENGS:
            self.add(e, fns[e], writes=[("barA", tag, e)], wait_all_dma=True)
        for e in self.ENGS:
            self.add(e, lambda eng: eng.nop(), reads=[("barA", tag, x) for x in self.ENGS],
                     writes=[("barB", tag, e)])
        if tag == self.stop_after:
            self.dead = True
        self.slots = {}
        self.last_w = {}
        self.readers = {}

    def emit(self, stack):
        nc = self.nc
        ops = self.ops
        for o in ops:
            for j in o["comp_deps"]:
                ops[j]["signal"] = True
        cnt = {e: 0 for e in self.ENGS}
        for o in ops:
            if o["signal"]:
                cnt[o["eng"]] += 1
                o["seq"] = cnt[o["eng"]]
        self.stats = dict(cnt=dict(cnt), n_ops=len(ops), max_dma=max([16 * v for v in self.dma_count.values()] + [0]),
                          n_sems=5 + len(self.dma_count))
        sems = {e: stack.enter_context(nc.semaphore("s_" + e)) for e in self.ENGS}
        dsems = {k: stack.enter_context(nc.semaphore("d_%d" % i))
                 for i, k in enumerate(self.dma_count)}
        block = stack.enter_context(nc.Block())
        per = {e: [] for e in self.ENGS}
        for o in ops:
            per[o["eng"]].append(o)

        def run(eng_name, eng):
            seen = {e: 0 for e in self.ENGS}
            dseen = {}
            for o in per[eng_name]:
                need = {}
                for j in o["comp_deps"]:
                    d = ops[j]
                    if d["seq"] > seen[d["eng"]]:
                        need[d["eng"]] = max(need.get(d["eng"], 0), d["seq"])
                for e, v in need.items():
                    eng.wait_ge(sems[e], v)
                    seen[e] = v
                for k, c in o["dma_waits"].items():
                    if dseen.get(k, 0) < c:
                        eng.wait_ge(dsems[k], 16 * c)
                        dseen[k] = c
                ins = o["fn"](eng)
                if o["dma"] is not None:
                    ins.then_inc(dsems[o["dma"]], 16)
                elif o["signal"]:
                    ins.then_inc(sems[eng_name], 1)
            for k in self.dma_count:
                if dseen.get(k, 0) < self.dma_count[k]:
                    eng.wait_ge(dsems[k], 16 * self.dma_count[k])

        @block.tensor
        def _(e):
            run("pe", e)

        @block.scalar
        def _(e):
            run("act", e)

        @block.vector
        def _(e):
            run("dve", e)

        @block.gpsimd
        def _(e):
            run("pool", e)

        @block.sync
        def _(e):
            run("sp", e)


class Arena:
    def __init__(self, ap, nwords):
        self.ap = ap
        self.n = nwords
        self.off = 0
        self.cnt = 0

    def reset(self):
        self.off = 0

    def get(self, shape, dt):
        esz = 4 if dt in (F32, U32, I32) else 2
        nel = 1
        for s in shape[1:]:
            nel *= s
        words = (nel * esz + 3) // 4
        words = (words + 7) // 8 * 8
        assert self.off + words <= self.n, ("arena overflow", self.off, words, self.n)
        v = self.ap[:, self.off:self.off + words]
        self.off += words
        if dt != F32:
            v = v.bitcast(dt)
        v = v[:, 0:nel]
        if len(shape) == 3:
            v = v.rearrange("p (a b) -> p a b", a=shape[1])
        elif len(shape) == 4:
            v = v.rearrange("p (a b c) -> p a b c", a=shape[1], b=shape[2])
        self.cnt += 1
        return v, ("ar", self.cnt)


def bc(ap, shape, axis):
    return ap.unsqueeze(axis).to_broadcast(list(shape))


def build_program(S, debug=False, stop_after=None):
    nc = bass.Bass("TRN2", target_bir_lowering=False)
    NT = S // 128
    NB = S // 512
    skind = "ExternalOutput" if debug else "Internal"

    def din(name, shape, dt=F32):
        return nc.dram_tensor(name, list(shape), dt, kind="ExternalInput").ap()

    def dscr(name, shape, dt):
        return nc.dram_tensor(name, list(shape), dt, kind=skind).ap()

    x = din("x", [S, D])
    mem = din("mem", [256, D])
    pos_t = din("pos_t", [128, NT], I32)
    nmw = din("nmw", [128, D])
    w_in_r = din("w_in_r", [128, 8, NPROJ])
    cw = din("cw", [128, 16, 4])
    cb = din("cb", [128, 16])
    dtb = din("dtb", [128, 16])
    alog = din("alog", [128, 16])
    sdd = din("sdd", [128, 16])
    snw = din("snw", [128, D])
    qnw = din("qnw", [128, 64])
    knw = din("knw", [128, 64])
    mnw = din("mnw", [128, D])
    wkv = din("wkv", [128, 8, 1536])
    mqnw = din("mqnw", [128, 192])
    mknw = din("mknw", [128, 192])
    wsb = din("wsb", [128, 8, D])
    wdb = din("wdb", [128, 2, D])
    wmb = din("wmb", [128, 8, D])
    wo = din("wo", [128, 8, D])
    nfw = din("nfw", [128, D])
    wq = din("wq", [128, 8, 2048])
    skT = din("skT", [128, 16, 128])
    dnT_r = din("dnT_r", [128, 128, D])
    up_r = din("up_r", [128, 128, D])
    out = nc.dram_tensor("out", [S, D], F32, kind="ExternalOutput").ap()

    w_in_b = dscr("w_in_b", [128, 8, NPROJ], BF16)
    dnT_b = dscr("dnT_b", [128, 128, D], BF16)
    up_b = dscr("up_b", [128, 128, D], BF16)
    z_s = dscr("z_s", [S, D], BF16)
    xbc_s = dscr("xbc_s", [2048, S], F32)
    dt_s = dscr("dt_s", [S, 16], F32)
    q_s = dscr("q_s", [S, 768], BF16)
    k_s = dscr("k_s", [S, 768], BF16)
    v_s = dscr("v_s", [S, 768], BF16)
    mq_s = dscr("mq_s", [S, 768], BF16)
    g_s = dscr("g_s", [3072, S], BF16)
    yssd_s = dscr("yssd_s", [S, D], BF16)
    od_s = dscr("od_s", [3, S, 260], F32)
    ymem_s = dscr("ymem_s", [S, 768], BF16)
    h_s = dscr("h_s", [S, D], F32)
    hnT_s = dscr("hnT_s", [NT, 128, 8, 128], BF16)
    r_s = dscr("r_s", [NT, 128, 3, 128], F32)

    st = ExitStack()
    with st:
        S_ = Sched(nc)
        S_.stop_after = stop_after
        add = S_.add
        NA = 44 * 1024
        arena_t = st.enter_context(nc.sbuf_tensor("arena", [128, NA], F32))
        AR = Arena(arena_t, NA)
        NC_ = 3 * 1024
        const_t = st.enter_context(nc.sbuf_tensor("consts", [128, NC_], F32))
        CA = Arena(const_t, NC_)
        psum = st.enter_context(nc.psum_tensor("psum", [128, 8 * 512], F32))

        def bank(i, n=1):
            return psum[:, i * 512:(i + n) * 512]

        def bank_bf(i):
            return psum[:, i * 512:(i + 1) * 512].bitcast(BF16)

        PK = [("ps", i) for i in range(8)]

        ident_f, k_idf = CA.get([128, 128], F32)
        ident_b, k_idb = CA.get([128, 128], BF16)
        iota_f, k_iota = CA.get([128, 128], F32)
        rowi, k_rowi = CA.get([128, 128], F32)
        mge_f, k_mgef = CA.get([128, 128], F32)
        mge_b, k_mgeb = CA.get([128, 128], BF16)
        mle_b, k_mleb = CA.get([128, 128], BF16)
        negm, k_negm = CA.get([128, 128], F32)
        ones_f, k_onesf = CA.get([128, 128], F32)
        bar_t, _kb = CA.get([128, 8], F32)
        S_.bar_tile = bar_t
        add("pool", lambda e: e.memset(bar_t, 0.0), writes=[_kb])
        cs_t, k_cs = CA.get([128, NT, 8], F32)
        sn_t, k_sn = CA.get([128, NT, 8], F32)
        aneg, k_aneg = CA.get([128, 16], F32)
        sd_t, k_sd = CA.get([128, 16], F32)
        dtb_t, k_dtb = CA.get([128, 16], F32)
        cw_t, k_cw = CA.get([128, 16, 4], F32)
        cb_t, k_cb = CA.get([128, 16], F32)

        add("pool", lambda e: e.iota(iota_f, pattern=[[1, 128]], base=0, channel_multiplier=0,
                                     allow_small_or_imprecise_dtypes=True), writes=[k_iota])
        add("pool", lambda e: e.iota(rowi, pattern=[[1, 128]], base=0, channel_multiplier=-1,
                                     allow_small_or_imprecise_dtypes=True), writes=[k_rowi])
        add("dve", lambda e: e.tensor_single_scalar(ident_f, rowi, 0.0, ALU.is_equal), reads=[k_rowi], writes=[k_idf])
        add("dve", lambda e: e.tensor_copy(ident_b, ident_f), reads=[k_idf], writes=[k_idb])
        add("dve", lambda e: e.tensor_single_scalar(mge_f, rowi, 0.0, ALU.is_ge), reads=[k_rowi], writes=[k_mgef])
        add("dve", lambda e: e.tensor_copy(mge_b, mge_f), reads=[k_mgef], writes=[k_mgeb])
        add("dve", lambda e: e.tensor_single_scalar(mle_b, rowi, 0.0, ALU.is_le), reads=[k_rowi], writes=[k_mleb])
        add("dve", lambda e: e.tensor_scalar(negm, mge_f, -1.0, 30000.0, ALU.add, ALU.mult), reads=[k_mgef], writes=[k_negm])
        add("pool", lambda e: e.memset(ones_f, 1.0), writes=[k_onesf])
        add("sp", lambda e: e.dma_start(out=aneg, in_=alog), writes=[k_aneg], dma_key="c_aneg")
        add("act", lambda e: e.activation(out=aneg, in_=aneg, func=AF.Exp), reads=[k_aneg], writes=[k_aneg])
        add("dve", lambda e: e.tensor_single_scalar(aneg, aneg, -1.0, ALU.mult), reads=[k_aneg], writes=[k_aneg])
        add("sp", lambda e: e.dma_start(out=sd_t, in_=sdd), writes=[k_sd], dma_key="c_sd")
        add("sp", lambda e: e.dma_start(out=dtb_t, in_=dtb), writes=[k_dtb], dma_key="c_dtb")
        add("sp", lambda e: e.dma_start(out=cw_t, in_=cw), writes=[k_cw], dma_key="c_cw")
        add("sp", lambda e: e.dma_start(out=cb_t, in_=cb), writes=[k_cb], dma_key="c_cb")

        AR.reset()
        pos_i, k_posi = AR.get([128, NT], I32)
        pos_f, k_posf = AR.get([128, NT], F32)
        ang, k_ang = AR.get([128, NT, 8], F32)
        kk, k_kk = AR.get([128, NT, 8], F32)
        rr, k_rr = AR.get([128, NT, 8], F32)
        add("sp", lambda e: e.dma_start(out=pos_i, in_=pos_t), writes=[k_posi], dma_key="c_pos")
        add("dve", lambda e: e.tensor_copy(pos_f, pos_i), reads=[k_posi], writes=[k_posf])
        inv = np.exp(np.float32(-math.log(500000.0) * (2.0 / 16)) * np.arange(8, dtype=np.float32)).astype(np.float32)
        for j in range(8):
            add("dve", lambda e, j=j: e.tensor_single_scalar(ang[:, :, j], pos_f, float(inv[j]), ALU.mult),
                reads=[k_posf], writes=[k_ang])
        C1 = 6.28125
        rem = 2.0 * math.pi - C1
        C2 = float(np.float32(rem).view(np.uint32) & np.uint32(0xFFFFF000))
        C2 = float(np.array([np.float32(rem).view(np.uint32) & np.uint32(0xFFFFF000)], dtype=np.uint32).view(np.float32)[0])
        C3 = float(np.float32(rem - C2))
        add("dve", lambda e: e.tensor_scalar(kk, ang, 1.0 / (2.0 * math.pi), MAGIC, ALU.mult, ALU.add), reads=[k_ang], writes=[k_kk])
        add("dve", lambda e: e.tensor_single_scalar(kk, kk, -MAGIC, ALU.add), reads=[k_kk], writes=[k_kk])
        add("dve", lambda e: e.scalar_tensor_tensor(out=rr, in0=kk, scalar=-C1, in1=ang, op0=ALU.mult, op1=ALU.add), reads=[k_kk, k_ang], writes=[k_rr])
        add("dve", lambda e: e.scalar_tensor_tensor(out=rr, in0=kk, scalar=-C2, in1=rr, op0=ALU.mult, op1=ALU.add), reads=[k_kk, k_rr], writes=[k_rr])
        add("dve", lambda e: e.scalar_tensor_tensor(out=rr, in0=kk, scalar=-C3, in1=rr, op0=ALU.mult, op1=ALU.add), reads=[k_kk, k_rr], writes=[k_rr])
        add("dve", lambda e: e.tensor_scalar(rr, rr, 3.14159, -3.14159, ALU.min, ALU.max), reads=[k_rr], writes=[k_rr])
        add("act", lambda e: e.activation(out=sn_t, in_=rr, func=AF.Sin), reads=[k_rr], writes=[k_sn])
        add("dve", lambda e: e.tensor_single_scalar(kk, rr, -1.0, ALU.mult), reads=[k_rr], writes=[k_kk])
        add("dve", lambda e: e.tensor_tensor(kk, kk, rr, ALU.max), reads=[k_rr, k_kk], writes=[k_kk])
        add("dve", lambda e: e.tensor_scalar(kk, kk, -1.0, math.pi / 2.0, ALU.mult, ALU.add), reads=[k_kk], writes=[k_kk])
        add("act", lambda e: e.activation(out=cs_t, in_=kk, func=AF.Sin), reads=[k_kk], writes=[k_cs])
        S_.barrier("c0")

        AR.reset()
        wtmp = [AR.get([128, NPROJ], BF16) for _ in range(2)]
        for dc in range(8):
            t, kt = wtmp[dc % 2]
            add("pool", lambda e, t=t, dc=dc: e.dma_start(out=t, in_=w_in_r[:, dc, :]), writes=[kt], dma_key=("wt", dc % 2))
            add("sp", lambda e, t=t, dc=dc: e.dma_start(out=w_in_b[:, dc, :], in_=t), reads=[kt], dma_key="w_in_b")
        S_.barrier("w0")
        S_.barrier("w1")

        def rms_rstd(src, src_keys, n, scratch, k_scr, ssq, k_ssq, rstd, k_rstd, nh=1, hd=None):
            hd = hd or n
            if nh == 1:
                add("dve", lambda e: e.memset(ssq[:, 0:1], 0.0), writes=[k_ssq])
                add("act", lambda e: e.activation(out=scratch[:, 0:n], in_=src, func=AF.Square, accum_out=ssq[:, 0:1]),
                    reads=src_keys, writes=[k_scr, k_ssq])
            else:
                add("act", lambda e: e.activation(out=scratch[:, 0:n], in_=src, func=AF.Square),
                    reads=src_keys, writes=[k_scr])
                add("dve", lambda e: e.tensor_reduce(out=ssq[:, 0:nh], in_=scratch[:, 0:n].rearrange("p (h d) -> p h d", h=nh),
                                                     axis=AX.X, op=ALU.add), reads=[k_scr], writes=[k_ssq])
            add("dve", lambda e: e.tensor_scalar(ssq[:, 0:nh], ssq[:, 0:nh], 1.0 / hd, EPS, ALU.mult, ALU.add),
                reads=[k_ssq], writes=[k_ssq])
            add("act", lambda e: e.activation(out=ssq[:, 0:nh], in_=ssq[:, 0:nh], func=AF.Ln), reads=[k_ssq], writes=[k_ssq])
            add("act", lambda e: e.activation(out=rstd[:, 0:nh], in_=ssq[:, 0:nh], func=AF.Exp, scale=-0.5),
                reads=[k_ssq], writes=[k_rstd])

        AR.reset()
        nmw_t, k_nmw = AR.get([128, D], F32)
        qnw_t, k_qnw = AR.get([128, 64], F32)
        knw_t, k_knw = AR.get([128, 64], F32)
        mqnw_t, k_mqnw = AR.get([128, 192], F32)
        add("sp", lambda e: e.dma_start(out=nmw_t, in_=nmw), writes=[k_nmw], dma_key="a_c0")
        add("sp", lambda e: e.dma_start(out=qnw_t, in_=qnw), writes=[k_qnw], dma_key="a_c1")
        add("sp", lambda e: e.dma_start(out=knw_t, in_=knw), writes=[k_knw], dma_key="a_c2")
        add("sp", lambda e: e.dma_start(out=mqnw_t, in_=mqnw), writes=[k_mqnw], dma_key="a_c3")
        xt_r = [AR.get([128, D], F32) for _ in range(2)]
        sq_t, k_sq = AR.get([128, D], F32)
        ssq_t, k_ssq = AR.get([128, 16], F32)
        rstd_t, k_rstd = AR.get([128, 16], F32)
        ub_t, k_ub = AR.get([128, D], BF16)
        uT_r = [AR.get([128, 8, 512], BF16) for _ in range(2)]
        wseg_r = [AR.get([128, 8, 512], BF16) for _ in range(3)]
        ob_r = [AR.get([128, 512], BF16) for _ in range(4)]
        of_r = [AR.get([128, 512], F32) for _ in range(3)]
        qn_t, k_qn = AR.get([128, 512], F32)
        r1_t, k_r1 = AR.get([128, 8, 8], F32)
        r2_t, k_r2 = AR.get([128, 8, 8], F32)
        dt1_t, k_dt1 = AR.get([128, 16], F32)
        dtv_r = [AR.get([128, 16], F32) for _ in range(2)]

        segs = []
        for c0 in (0, 512):
            segs.append((OFF_Z + c0, 512, "z", c0))
        for c0 in range(0, 2048, 512):
            segs.append((OFF_XBC + c0, 512, "xbc", c0))
        segs.append((OFF_DT, 16, "dt", 0))
        for nm, off in (("q", OFF_DQ), ("k", OFF_DK), ("v", OFF_DV)):
            segs.append((off, 512, nm, 0))
            segs.append((off + 512, 256, nm, 512))
        segs.append((OFF_MQ, 384, "mq", 0))
        segs.append((OFF_MQ + 384, 384, "mq", 384))
        for c0 in range(0, 3072, 512):
            segs.append((OFF_G + c0, 512, "g", c0))

        cnt = dict(ob=0, of=0, ws=0, ps=0, dtv=0)

        def nxt(name, ring):
            i = cnt[name]
            cnt[name] += 1
            return ring[i % len(ring)] + (i % len(ring),)

        for blk in range(NB):
            uT, k_uT = uT_r[blk % 2]
            for ti in range(4):
                tg = blk * 4 + ti
                xt, k_xt = xt_r[tg % 2]
                add("sp", lambda e, xt=xt, tg=tg: e.dma_start(out=xt, in_=x[tg * 128:(tg + 1) * 128, :]),
                    writes=[k_xt], dma_key=("a_x", tg % 2))
                rms_rstd(xt, [k_xt], D, sq_t, k_sq, ssq_t, k_ssq, rstd_t, k_rstd)
                add("dve", lambda e, xt=xt: e.scalar_tensor_tensor(out=ub_t, in0=xt, scalar=rstd_t[:, 0:1], in1=nmw_t,
                                                                   op0=ALU.mult, op1=ALU.mult),
                    reads=[k_xt, k_rstd, k_nmw], writes=[k_ub])
                pb = 6 + (tg % 2)
                for dc in range(8):
                    add("pe", lambda e, dc=dc, pb=pb: e.transpose(bank_bf(pb)[:, dc * 128:(dc + 1) * 128],
                                                                  ub_t[:, dc * 128:(dc + 1) * 128], ident_b),
                        reads=[k_ub, k_idb], writes=[PK[pb]])
                add("act", lambda e, uT=uT, ti=ti, pb=pb: e.copy(
                    uT[:, :, ti * 128:(ti + 1) * 128], bank_bf(pb).rearrange("p (a b) -> p a b", a=8)),
                    reads=[PK[pb]], writes=[k_uT])
            for (c0, width, mode, rel) in segs:
                ws, k_ws, wi = nxt("ws", wseg_r)
                add("sp", lambda e, ws=ws, c0=c0, width=width: e.dma_start(out=ws[:, :, 0:width], in_=w_in_b[:, :, c0:c0 + width]),
                    writes=[k_ws], dma_key=("a_ws", wi))
                if mode in ("xbc", "g"):
                    for c4 in range(4):
                        pbk = cnt["ps"] % 6
                        cnt["ps"] += 1
                        for dc in range(8):
                            add("pe", lambda e, ws=ws, dc=dc, c4=c4, pbk=pbk, uT=uT: e.matmul(
                                bank(pbk), ws[:, dc, c4 * 128:(c4 + 1) * 128], uT[:, dc, :], start=(dc == 0), stop=(dc == 7)),
                                reads=[k_ws, k_uT], writes=[PK[pbk]])
                        frow = rel + c4 * 128
                        if mode == "xbc":
                            of, k_of, oi = nxt("of", of_r)
                            add("act", lambda e, of=of, pbk=pbk: e.copy(of, bank(pbk)), reads=[PK[pbk]], writes=[k_of])
                            add("pool", lambda e, of=of, frow=frow, blk=blk: e.dma_start(
                                out=xbc_s[frow:frow + 128, blk * 512:(blk + 1) * 512], in_=of), reads=[k_of], dma_key=("a_of", oi))
                        else:
                            ob, k_ob, oi = nxt("ob", ob_r)
                            add("act", lambda e, ob=ob, pbk=pbk: e.activation(out=ob, in_=bank(pbk), func=AF.Sigmoid),
                                reads=[PK[pbk]], writes=[k_ob])
                            add("pool", lambda e, ob=ob, frow=frow, blk=blk: e.dma_start(
                                out=g_s[frow:frow + 128, blk * 512:(blk + 1) * 512], in_=ob), reads=[k_ob], dma_key=("a_ob", oi))
                    continue
                for ti in range(4):
                    tg = blk * 4 + ti
                    t0 = tg * 128
                    pbk = cnt["ps"] % 6
                    cnt["ps"] += 1
                    ps = bank(pbk)[:, 0:width]
                    for dc in range(8):
                        add("pe", lambda e, ws=ws, dc=dc, ti=ti, ps=ps, uT=uT, width=width: e.matmul(
                            ps, uT[:, dc, ti * 128:(ti + 1) * 128], ws[:, dc, 0:width], start=(dc == 0), stop=(dc == 7)),
                            reads=[k_ws, k_uT], writes=[PK[pbk]])
                    if mode == "z":
                        ob, k_ob, oi = nxt("ob", ob_r)
                        add("act", lambda e, ob=ob, ps=ps: e.activation(out=ob, in_=ps, func=AF.Silu), reads=[PK[pbk]], writes=[k_ob])
                        add("pool", lambda e, ob=ob, t0=t0, rel=rel: e.dma_start(out=z_s[t0:t0 + 128, rel:rel + 512], in_=ob),
                            reads=[k_ob], dma_key=("a_ob", oi))
                    elif mode == "v":
                        ob, k_ob, oi = nxt("ob", ob_r)
                        add("act", lambda e, ob=ob, ps=ps, width=width: e.copy(ob[:, 0:width], ps), reads=[PK[pbk]], writes=[k_ob])
                        add("pool", lambda e, ob=ob, t0=t0, rel=rel, width=width: e.dma_start(
                            out=v_s[t0:t0 + 128, rel:rel + width], in_=ob[:, 0:width]), reads=[k_ob], dma_key=("a_ob", oi))
                    elif mode == "dt":
                        dtv, k_dtv, di = nxt("dtv", dtv_r)
                        add("dve", lambda e, ps=ps: e.tensor_tensor(dt1_t, ps, dtb_t, ALU.add), reads=[PK[pbk], k_dtb], writes=[k_dt1])
                        add("act", lambda e: e.activation(out=dt1_t, in_=dt1_t, func=AF.Exp), reads=[k_dt1], writes=[k_dt1])
                        add("dve", lambda e: e.tensor_single_scalar(dt1_t, dt1_t, 1.0, ALU.add), reads=[k_dt1], writes=[k_dt1])
                        add("act", lambda e, dtv=dtv: e.activation(out=dtv, in_=dt1_t, func=AF.Ln), reads=[k_dt1], writes=[k_dtv])
                        add("pool", lambda e, dtv=dtv, t0=t0: e.dma_start(out=dt_s[t0:t0 + 128, :], in_=dtv), reads=[k_dtv], dma_key=("a_dtv", di))
                    elif mode in ("q", "k"):
                        nh = width // 64
                        nwt, k_nw = (qnw_t, k_qnw) if mode == "q" else (knw_t, k_knw)
                        dst = q_s if mode == "q" else k_s
                        rms_rstd(ps, [PK[pbk]], width, sq_t, k_sq, ssq_t, k_ssq, rstd_t, k_rstd, nh=nh, hd=64)
                        qv = qn_t[:, 0:width].rearrange("p (h d) -> p h d", h=nh)
                        add("dve", lambda e, ps=ps, qv=qv, nh=nh: e.tensor_tensor(
                            qv, ps.rearrange("p (h d) -> p h d", h=nh), bc(rstd_t[:, 0:nh], [128, nh, 64], 2), ALU.mult),
                            reads=[PK[pbk], k_rstd], writes=[k_qn])
                        add("dve", lambda e, qv=qv, nh=nh, nwt=nwt: e.tensor_tensor(qv, qv, bc(nwt, [128, nh, 64], 1), ALU.mult),
                            reads=[k_qn, k_nw], writes=[k_qn])
                        ob, k_ob, oi = nxt("ob", ob_r)
                        obv = ob[:, 0:width].rearrange("p (h d) -> p h d", h=nh)
                        add("act", lambda e, ob=ob, width=width: e.copy(ob[:, 0:width], qn_t[:, 0:width]), reads=[k_qn], writes=[k_ob])
                        cosb = bc(cs_t[:, tg, :], [128, nh, 8], 1)
                        sinb = bc(sn_t[:, tg, :], [128, nh, 8], 1)
                        a1 = r1_t[:, 0:nh, :]
                        a2 = r2_t[:, 0:nh, :]
                        add("dve", lambda e, qv=qv, a1=a1, cosb=cosb: e.tensor_tensor(a1, qv[:, :, 0:8], cosb, ALU.mult), reads=[k_qn, k_cs], writes=[k_r1])
                        add("dve", lambda e, qv=qv, a2=a2, sinb=sinb: e.tensor_tensor(a2, qv[:, :, 8:16], sinb, ALU.mult), reads=[k_qn, k_sn], writes=[k_r2])
                        add("dve", lambda e, obv=obv, a1=a1, a2=a2: e.tensor_tensor(obv[:, :, 0:8], a1, a2, ALU.subtract),
                            reads=[k_r1, k_r2, k_ob], writes=[k_ob])
                        add("dve", lambda e, qv=qv, a1=a1, cosb=cosb: e.tensor_tensor(a1, qv[:, :, 8:16], cosb, ALU.mult), reads=[k_qn, k_cs, k_ob], writes=[k_r1])
                        add("dve", lambda e, qv=qv, a2=a2, sinb=sinb: e.tensor_tensor(a2, qv[:, :, 0:8], sinb, ALU.mult), reads=[k_qn, k_sn, k_ob], writes=[k_r2])
                        add("dve", lambda e, obv=obv, a1=a1, a2=a2: e.tensor_tensor(obv[:, :, 8:16], a1, a2, ALU.add),
                            reads=[k_r1, k_r2, k_ob], writes=[k_ob])
                        add("pool", lambda e, ob=ob, t0=t0, rel=rel, width=width, dst=dst: e.dma_start(
                            out=dst[t0:t0 + 128, rel:rel + width], in_=ob[:, 0:width]), reads=[k_ob], dma_key=("a_ob", oi))
                    elif mode == "mq":
                        rms_rstd(ps, [PK[pbk]], 384, sq_t, k_sq, ssq_t, k_ssq, rstd_t, k_rstd, nh=2, hd=192)
                        qv = qn_t[:, 0:384].rearrange("p (h d) -> p h d", h=2)
                        add("dve", lambda e, ps=ps, qv=qv: e.tensor_tensor(
                            qv, ps.rearrange("p (h d) -> p h d", h=2), bc(rstd_t[:, 0:2], [128, 2, 192], 2), ALU.mult),
                            reads=[PK[pbk], k_rstd], writes=[k_qn])
                        ob, k_ob, oi = nxt("ob", ob_r)
                        add("dve", lambda e, ob=ob, qv=qv: e.tensor_tensor(
                            ob[:, 0:384].rearrange("p (h d) -> p h d", h=2), qv, bc(mqnw_t, [128, 2, 192], 1), ALU.mult),
                            reads=[k_qn, k_mqnw], writes=[k_ob])
                        add("pool", lambda e, ob=ob, t0=t0, rel=rel: e.dma_start(out=mq_s[t0:t0 + 128, rel:rel + 384], in_=ob[:, 0:384]),
                            reads=[k_ob], dma_key=("a_ob", oi))
        S_.barrier("a")

        AR.reset()
        stT, k_stT = AR.get([128, 16, 64], F32)
        stB, k_stB = AR.get([128, 16, 64], BF16)
        snw_t, k_snw = AR.get([128, D], F32)
        add("sp", lambda e: e.dma_start(out=snw_t, in_=snw), writes=[k_snw], dma_key="b_c0")
        add("dve", lambda e: e.memset(stT, 0.0), writes=[k_stT])
        add("pool", lambda e: e.memset(stB, 0.0), writes=[k_stB])
        raw_r = [AR.get([128, 16, 131], F32) for _ in range(2)]
        dtl_r = [AR.get([128, 16], F32) for _ in range(2)]
        zl_r = [AR.get([128, D], BF16) for _ in range(2)]
        cv_t, k_cv = AR.get([128, 16, 128], F32)
        xbT, k_xbT = AR.get([128, 16, 128], BF16)
        xs_t, k_xs = AR.get([128, 16, 64], BF16)
        Bt_t, k_Bt = AR.get([128, 4, 128], BF16)
        da_t, k_da = AR.get([128, 16], F32)
        acol, k_acol = AR.get([128, 16], F32)
        X_t, k_X = AR.get([128, 16, 128], F32)
        arow, k_arow = AR.get([128, 16, 128], F32)
        E_t, k_E = AR.get([128, 16, 128], F32)
        eA_t, k_eA = AR.get([128, 16, 128], F32)
        W_t, k_W = AR.get([128, 16, 128], BF16)
        CTp, k_CTp = AR.get([128, 16, 128], BF16)
        xdt, k_xdt = AR.get([128, 16, 64], BF16)
        xdd, k_xdd = AR.get([128, 16, 64], BF16)
        dec, k_dec = AR.get([128, 16], F32)
        dtd, k_dtd = AR.get([128, 16], F32)
        y_t, k_y = AR.get([128, D], F32)
        ysq, k_ysq = AR.get([128, D], F32)
        gss, k_gss = AR.get([128, 16], F32)
        grs, k_grs = AR.get([128, 16], F32)
        yb_r = [AR.get([128, D], BF16) for _ in range(2)]
        xbc_v = xbc_s.rearrange("(c p) t -> p c t", p=128)
        for c in range(NT):
            t0 = c * 128
            raw, k_raw = raw_r[c % 2]
            dtl, k_dtl = dtl_r[c % 2]
            zl, k_zl = zl_r[c % 2]
            if c == 0:
                add("pool", lambda e, raw=raw: e.memset(raw[:, :, 0:3], 0.0), writes=[k_raw])
                add("sp", lambda e, raw=raw: e.dma_start(out=raw[:, :, 3:131], in_=xbc_v[:, :, 0:128]), writes=[k_raw], dma_key=("b_raw", c % 2))
            else:
                add("sp", lambda e, raw=raw, t0=t0: e.dma_start(out=raw, in_=xbc_v[:, :, t0 - 3:t0 + 128]), writes=[k_raw], dma_key=("b_raw", c % 2))
            add("sp", lambda e, dtl=dtl, t0=t0: e.dma_start(out=dtl, in_=dt_s[t0:t0 + 128, :]), writes=[k_dtl], dma_key=("b_dt", c % 2))
            add("sp", lambda e, zl=zl, t0=t0: e.dma_start(out=zl, in_=z_s[t0:t0 + 128, :]), writes=[k_zl], dma_key=("b_z", c % 2))
            for cc in range(16):
                add("dve", lambda e, raw=raw, cc=cc: e.tensor_scalar(cv_t[:, cc, :], raw[:, cc, 3:131], cw_t[:, cc, 3:4], cb_t[:, cc:cc + 1],
                                                                     ALU.mult, ALU.add), reads=[k_raw, k_cw, k_cb], writes=[("cv", cc)])
            for kq in (2, 1, 0):
                for cc in range(16):
                    add("dve", lambda e, raw=raw, cc=cc, kq=kq: e.scalar_tensor_tensor(
                        out=cv_t[:, cc, :], in0=raw[:, cc, kq:kq + 128], scalar=cw_t[:, cc, kq:kq + 1], in1=cv_t[:, cc, :],
                        op0=ALU.mult, op1=ALU.add), reads=[k_raw, k_cw, ("cv", cc)], writes=[("cv", cc)])
            add("act", lambda e: e.activation(out=xbT, in_=cv_t, func=AF.Silu), reads=[("cv", cc) for cc in range(16)], writes=[k_xbT])
            for cc in range(8):
                add("pe", lambda e, cc=cc: e.transpose(bank_bf(0)[:, cc * 128:(cc + 1) * 128], xbT[:, cc, :], ident_b),
                    reads=[k_xbT, k_idb], writes=[PK[0]])
            for g in range(4):
                add("pe", lambda e, g=g: e.transpose(bank_bf(1)[:, g * 128:(g + 1) * 128], xbT[:, 8 + g, :], ident_b),
                    reads=[k_xbT, k_idb], writes=[PK[1]])
            add("act", lambda e: e.copy(xs_t.rearrange("p a b -> p (a b)"), bank_bf(0)), reads=[PK[0]], writes=[k_xs])
            add("act", lambda e: e.copy(Bt_t.rearrange("p a b -> p (a b)"), bank_bf(1)[:, 0:512]), reads=[PK[1]], writes=[k_Bt])
            add("dve", lambda e, dtl=dtl: e.tensor_tensor(da_t, dtl, aneg, ALU.mult), reads=[k_dtl, k_aneg], writes=[k_da])
            add("pe", lambda e: e.matmul(bank(2)[:, 0:16], mge_f, da_t, start=True, stop=True), reads=[k_mgef, k_da], writes=[PK[2]])
            add("act", lambda e: e.copy(acol, bank(2)[:, 0:16]), reads=[PK[2]], writes=[k_acol])
            add("dve", lambda e: e.tensor_tensor(X_t, bc(da_t, [128, 16, 128], 2), bc(mge_f, [128, 16, 128], 1), ALU.mult),
                reads=[k_da, k_mgef], writes=[k_X])
            for q4 in range(4):
                pbk = 3 + (q4 % 2)
                add("pe", lambda e, q4=q4, pbk=pbk: e.matmul(bank(pbk), ones_f, X_t[:, q4 * 4:(q4 + 1) * 4, :].rearrange("p a b -> p (a b)"),
                                                             start=True, stop=True), reads=[k_onesf, k_X], writes=[PK[pbk]])
                add("act", lambda e, q4=q4, pbk=pbk: e.copy(arow[:, q4 * 4:(q4 + 1) * 4, :].rearrange("p a b -> p (a b)"), bank(pbk)),
                    reads=[PK[pbk]], writes=[k_arow])
            add("dve", lambda e: e.tensor_tensor(E_t, arow, bc(negm, [128, 16, 128], 1), ALU.add), reads=[k_arow, k_negm], writes=[k_E])
            add("dve", lambda e: e.tensor_tensor(E_t, E_t, bc(acol, [128, 16, 128], 2), ALU.subtract), reads=[k_E, k_acol], writes=[k_E])
            add("act", lambda e: e.activation(out=E_t, in_=E_t, func=AF.Exp), reads=[k_E], writes=[k_E])
            add("act", lambda e: e.activation(out=eA_t, in_=arow, func=AF.Exp), reads=[k_arow], writes=[k_eA])
            for g in range(4):
                add("pe", lambda e, g=g: e.matmul(bank(5)[:, g * 128:(g + 1) * 128], xbT[:, 8 + g, :], xbT[:, 12 + g, :], start=True, stop=True),
                    reads=[k_xbT], writes=[PK[5]])
            for g in range(4):
                add("dve", lambda e, g=g: e.tensor_tensor(W_t[:, 4 * g:4 * g + 4, :], E_t[:, 4 * g:4 * g + 4, :],
                                                          bc(bank(5)[:, g * 128:(g + 1) * 128], [128, 4, 128], 1), ALU.mult),
                    reads=[k_E, PK[5]], writes=[k_W])
                add("pool", lambda e, g=g: e.tensor_tensor(CTp[:, 4 * g:4 * g + 4, :], eA_t[:, 4 * g:4 * g + 4, :],
                                                           bc(xbT[:, 12 + g, :], [128, 4, 128], 1), ALU.mult),
                    reads=[k_eA, k_xbT], writes=[k_CTp])
            add("dve", lambda e, dtl=dtl: e.tensor_tensor(xdt, xs_t, bc(dtl, [128, 16, 64], 2), ALU.mult), reads=[k_xs, k_dtl], writes=[k_xdt])
            for hd in range(16):
                pbk = 6 + hd // 8
                o = bank(pbk)[:, (hd % 8) * 64:(hd % 8 + 1) * 64]
                add("pe", lambda e, hd=hd, o=o: e.matmul(o, W_t[:, hd, :], xdt[:, hd, :], start=True, stop=False),
                    reads=[k_W, k_xdt], writes=[PK[pbk]])
                add("pe", lambda e, hd=hd, o=o: e.matmul(o, CTp[:, hd, :], stB[:, hd, :], start=False, stop=True),
                    reads=[k_CTp, k_stB], writes=[PK[pbk]])
            add("dve", lambda e: e.tensor_tensor(dec, arow[:, :, 127], acol, ALU.subtract), reads=[k_arow, k_acol], writes=[k_dec])
            add("act", lambda e: e.activation(out=dec, in_=dec, func=AF.Exp), reads=[k_dec], writes=[k_dec])
            add("dve", lambda e, dtl=dtl: e.tensor_tensor(dtd, dtl, dec, ALU.mult), reads=[k_dtl, k_dec], writes=[k_dtd])
            add("dve", lambda e: e.tensor_tensor(xdd, xs_t, bc(dtd, [128, 16, 64], 2), ALU.mult), reads=[k_xs, k_dtd], writes=[k_xdd])
            for hd in range(16):
                pbk = 3 + hd // 8
                o = bank(pbk)[:, (hd % 8) * 64:(hd % 8 + 1) * 64]
                add("pe", lambda e, hd=hd, o=o: e.matmul(o, Bt_t[:, hd // 4, :], xdd[:, hd, :], start=True, stop=True),
                    reads=[k_Bt, k_xdd], writes=[PK[pbk]])
            add("dve", lambda e: e.tensor_tensor(y_t.rearrange("p (a b) -> p a b", a=16), xs_t, bc(sd_t, [128, 16, 64], 2), ALU.mult),
                reads=[k_xs, k_sd], writes=[k_y])
            add("dve", lambda e: e.tensor_tensor(y_t[:, 0:512], y_t[:, 0:512], bank(6), ALU.add), reads=[k_y, PK[6]], writes=[k_y])
            add("dve", lambda e: e.tensor_tensor(y_t[:, 512:1024], y_t[:, 512:1024], bank(7), ALU.add), reads=[k_y, PK[7]], writes=[k_y])
            add("dve", lambda e, zl=zl: e.tensor_tensor(y_t, y_t, zl, ALU.mult), reads=[k_y, k_zl], writes=[k_y])
            rms_rstd(y_t, [k_y], D, ysq, k_ysq, gss, k_gss, grs, k_grs, nh=4, hd=256)
            add("dve", lambda e: e.tensor_tensor(y_t.rearrange("p (a b) -> p a b", a=4), y_t.rearrange("p (a b) -> p a b", a=4),
                                                 bc(grs[:, 0:4], [128, 4, 256], 2), ALU.mult), reads=[k_y, k_grs], writes=[k_y])
            yb, k_yb = yb_r[c % 2]
            add("dve", lambda e, yb=yb: e.tensor_tensor(yb, y_t, snw_t, ALU.mult), reads=[k_y, k_snw], writes=[k_yb])
            add("pool", lambda e, yb=yb, t0=t0: e.dma_start(out=yssd_s[t0:t0 + 128, :], in_=yb), reads=[k_yb], dma_key=("b_yb", c % 2))
            add("dve", lambda e: e.tensor_tensor(stT, stT, bc(eA_t[:, :, 127], [128, 16, 64], 2), ALU.mult), reads=[k_stT, k_eA], writes=[k_stT])
            add("dve", lambda e: e.tensor_tensor(stT[:, 0:8, :].rearrange("p a b -> p (a b)"), stT[:, 0:8, :].rearrange("p a b -> p (a b)"),
                                                 bank(3), ALU.add), reads=[k_stT, PK[3]], writes=[k_stT])
            add("dve", lambda e: e.tensor_tensor(stT[:, 8:16, :].rearrange("p a b -> p (a b)"), stT[:, 8:16, :].rearrange("p a b -> p (a b)"),
                                                 bank(4), ALU.add), reads=[k_stT, PK[4]], writes=[k_stT])
            add("act", lambda e: e.copy(stB, stT), reads=[k_stT], writes=[k_stB])
        S_.barrier("b")

        AR.reset()
        Qb_r = [AR.get([128, 256], BF16) for _ in range(2)]
        Kb_r = [AR.get([128, 256], BF16) for _ in range(2)]
        Vb_r = [AR.get([128, 256], BF16) for _ in range(2)]
        QT_r = [AR.get([128, 2, 128], BF16) for _ in range(2)]
        KT_r = [AR.get([128, 2, 128], BF16) for _ in range(2)]
        Va_r = [AR.get([128, 4, 65], BF16) for _ in range(2)]
        P_r = [AR.get([128, 128], BF16) for _ in range(4)]
        od_r = [AR.get([128, 260], F32) for _ in range(2)]
        for i in range(2):
            add("pool", lambda e, i=i: e.memset(Va_r[i][0][:, :, 64:65], 1.0), writes=[("va1", i)])
        etmp = [AR.get([128, 8, D], BF16) for _ in range(4)]
        casts = [(src, dst, nm, jg) for (src, dst, nm) in ((dnT_r, dnT_b, "dnT_b"), (up_r, up_b, "up_b")) for jg in range(16)]
        cast_i = [0]

        def emit_cast():
            ei = cast_i[0]
            if ei >= len(casts):
                return
            cast_i[0] += 1
            src, dst, nm, jg = casts[ei]
            t, kt = etmp[ei % 4]
            add("pool", lambda e, t=t, src=src, jg=jg: e.dma_start(
                out=t, in_=src[jg * 8:(jg + 1) * 8].rearrange("j p f -> p j f")), writes=[kt], dma_key=("et", ei % 4))
            add("act", lambda e, t=t, dst=dst, jg=jg: e.dma_start(
                out=dst[jg * 8:(jg + 1) * 8].rearrange("j p f -> p j f"), in_=t), reads=[kt], dma_key=("ets", ei % 4))
        bi = 0
        pi = 0
        for gi, dil in enumerate(DILS):
            nb = S // dil // 128
            for r in range(dil):
                for n in range(nb):
                    rows = slice(r + n * 128 * dil, r + n * 128 * dil + 127 * dil + 1, dil)
                    cols = slice(gi * 256, (gi + 1) * 256)
                    Qb, k_Qb = Qb_r[bi % 2]
                    Kb, k_Kb = Kb_r[bi % 2]
                    Vb, k_Vb = Vb_r[bi % 2]
                    QT, k_QT = QT_r[bi % 2]
                    KT, k_KT = KT_r[bi % 2]
                    Va, k_Va = Va_r[bi % 2]
                    KTp, k_KTp = KT_r[(bi + 1) % 2]
                    Vap, k_Vap = Va_r[(bi + 1) % 2]
                    add("sp", lambda e, Qb=Qb, rows=rows, cols=cols: e.dma_start(out=Qb, in_=q_s[rows, cols]), writes=[k_Qb], dma_key=("c_q", bi % 2))
                    add("sp", lambda e, Kb=Kb, rows=rows, cols=cols: e.dma_start(out=Kb, in_=k_s[rows, cols]), writes=[k_Kb], dma_key=("c_k", bi % 2))
                    add("sp", lambda e, Vb=Vb, rows=rows, cols=cols: e.dma_start(out=Vb, in_=v_s[rows, cols]), writes=[k_Vb], dma_key=("c_v", bi % 2))
                    for hp in range(2):
                        add("pe", lambda e, hp=hp, Qb=Qb: e.transpose(bank_bf(0)[:, hp * 128:(hp + 1) * 128], Qb[:, hp * 128:(hp + 1) * 128], ident_b),
                            reads=[k_Qb, k_idb], writes=[PK[0]])
                        add("pe", lambda e, hp=hp, Kb=Kb: e.transpose(bank_bf(0)[:, 256 + hp * 128:256 + (hp + 1) * 128], Kb[:, hp * 128:(hp + 1) * 128], ident_b),
                            reads=[k_Kb, k_idb], writes=[PK[0]])
                    add("act", lambda e, QT=QT: e.copy(QT.rearrange("p a b -> p (a b)"), bank_bf(0)[:, 0:256]), reads=[PK[0]], writes=[k_QT])
                    add("act", lambda e, KT=KT: e.copy(KT.rearrange("p a b -> p (a b)"), bank_bf(0)[:, 256:512]), reads=[PK[0]], writes=[k_KT])
                    add("dve", lambda e, Va=Va, Vb=Vb: e.tensor_copy(Va[:, :, 0:64], Vb.rearrange("p (h d) -> p h d", h=4)),
                        reads=[k_Vb, ("va1", bi % 2)], writes=[k_Va])
                    kts = ([("prev", KTp, k_KTp, Vap, k_Vap)] if n > 0 else []) + [("cur", KT, k_KT, Va, k_Va)]
                    opb = 3 + (bi % 2)
                    for h in range(4):
                        hp, hh = h // 2, h % 2
                        for ki, (which, kt_, k_kt, va_, k_va) in enumerate(kts):
                            spb = 1 + (pi % 2)
                            sslot = bank(spb)[:, ((pi // 2) % 4) * 128:((pi // 2) % 4 + 1) * 128]
                            P, k_P = P_r[pi % 4]
                            msk, k_msk = (mge_b, k_mgeb) if which == "cur" else (mle_b, k_mleb)
                            add("pe", lambda e, sslot=sslot, kt_=kt_, QT=QT, hp=hp, hh=hh: e.matmul(
                                sslot, kt_[hh * 64:(hh + 1) * 64, hp, :], QT[hh * 64:(hh + 1) * 64, hp, :], start=True, stop=True),
                                reads=[k_kt, k_QT], writes=[("pss", spb, (pi // 2) % 4)])
                            add("act", lambda e, P=P, sslot=sslot: e.activation(out=P, in_=sslot, func=AF.Exp, scale=0.125),
                                reads=[("pss", spb, (pi // 2) % 4)], writes=[k_P])
                            add("dve", lambda e, P=P, msk=msk: e.tensor_tensor(P, P, msk, ALU.mult), reads=[k_P, k_msk], writes=[k_P])
                            add("pe", lambda e, P=P, va_=va_, h=h, opb=opb, ki=ki, nk=len(kts): e.matmul(
                                bank(opb)[:, h * 65:(h + 1) * 65], P, va_[:, h, :], start=(ki == 0), stop=(ki == nk - 1)),
                                reads=[k_P, k_va, ("va1", 0), ("va1", 1)], writes=[PK[opb]])
                            pi += 1
                    od, k_od = od_r[bi % 2]
                    add("act", lambda e, od=od, opb=opb: e.copy(od, bank(opb)[:, 0:260]), reads=[PK[opb]], writes=[k_od])
                    add("pool", lambda e, od=od, rows=rows, gi=gi: e.dma_start(out=od_s[gi, rows, :], in_=od), reads=[k_od], dma_key=("c_od", bi % 2))
                    bi += 1
                    emit_cast()
        while cast_i[0] < len(casts):
            emit_cast()
        S_.barrier("c")

        AR.reset()
        mnw_t, k_mnw = AR.get([128, D], F32)
        mknw_t, k_mknw = AR.get([128, 192], F32)
        wkv_t, k_wkv = AR.get([128, 8, 1536], BF16)
        add("sp", lambda e: e.dma_start(out=mnw_t, in_=mnw), writes=[k_mnw], dma_key="m_c0")
        add("sp", lambda e: e.dma_start(out=mknw_t, in_=mknw), writes=[k_mknw], dma_key="m_c1")
        add("pool", lambda e: e.dma_start(out=wkv_t, in_=wkv), writes=[k_wkv], dma_key="m_c2")
        mt_t, k_mt = AR.get([128, D], F32)
        msq, k_msq = AR.get([128, D], F32)
        mss, k_mss = AR.get([128, 16], F32)
        mrs, k_mrs = AR.get([128, 16], F32)
        mub, k_mub = AR.get([128, D], BF16)
        memT, k_memT = AR.get([128, 8, 256], BF16)
        mkv, k_mkv = AR.get([128, 2, 1536], F32)
        mkn, k_mkn = AR.get([128, 2, 768], BF16)
        KmA, k_KmA = AR.get([128, 4, 256], BF16)
        KmB, k_KmB = AR.get([128, 4, 256], BF16)
        VmA, k_VmA = AR.get([128, 2, 4, 193], BF16)
        for mt in range(2):
            add("sp", lambda e, mt=mt: e.dma_start(out=mt_t, in_=mem[mt * 128:(mt + 1) * 128, :]), writes=[k_mt], dma_key="m_mem")
            rms_rstd(mt_t, [k_mt], D, msq, k_msq, mss, k_mss, mrs, k_mrs)
            add("dve", lambda e: e.scalar_tensor_tensor(out=mub, in0=mt_t, scalar=mrs[:, 0:1], in1=mnw_t, op0=ALU.mult, op1=ALU.mult),
                reads=[k_mt, k_mrs, k_mnw], writes=[k_mub])
            for dc in range(8):
                add("pe", lambda e, dc=dc: e.transpose(bank_bf(0)[:, dc * 128:(dc + 1) * 128], mub[:, dc * 128:(dc + 1) * 128], ident_b),
                    reads=[k_mub, k_idb], writes=[PK[0]])
            add("act", lambda e, mt=mt: e.copy(memT[:, :, mt * 128:(mt + 1) * 128], bank_bf(0).rearrange("p (a b) -> p a b", a=8)),
                reads=[PK[0]], writes=[k_memT])
        for mt in range(2):
            for cs3 in range(3):
                pbk = 1 + (mt * 3 + cs3) % 2
                for dc in range(8):
                    add("pe", lambda e, mt=mt, cs3=cs3, dc=dc, pbk=pbk: e.matmul(
                        bank(pbk), memT[:, dc, mt * 128:(mt + 1) * 128], wkv_t[:, dc, cs3 * 512:(cs3 + 1) * 512], start=(dc == 0), stop=(dc == 7)),
                        reads=[k_memT, k_wkv], writes=[PK[pbk]])
                add("act", lambda e, mt=mt, cs3=cs3, pbk=pbk: e.copy(mkv[:, mt, cs3 * 512:(cs3 + 1) * 512], bank(pbk)), reads=[PK[pbk]], writes=[k_mkv])
        for mt in range(2):
            src = mkv[:, mt, 0:768]
            rms_rstd(src, [k_mkv], 768, msq, k_msq, mss, k_mss, mrs, k_mrs, nh=4, hd=192)
            add("dve", lambda e, src=src: e.tensor_tensor(msq[:, 0:768].rearrange("p (h d) -> p h d", h=4), src.rearrange("p (h d) -> p h d", h=4),
                                                          bc(mrs[:, 0:4], [128, 4, 192], 2), ALU.mult), reads=[k_mkv, k_mrs], writes=[k_msq])
            add("dve", lambda e, mt=mt: e.tensor_tensor(mkn[:, mt, :].rearrange("p (h d) -> p h d", h=4), msq[:, 0:768].rearrange("p (h d) -> p h d", h=4),
                                                        bc(mknw_t, [128, 4, 192], 1), ALU.mult), reads=[k_msq, k_mknw], writes=[k_mkn])
            for h in range(4):
                add("pe", lambda e, mt=mt, h=h: e.transpose(bank_bf(3)[:, h * 128:(h + 1) * 128], mkn[:, mt, h * 192:h * 192 + 128], ident_b),
                    reads=[k_mkn, k_idb], writes=[PK[3]])
                add("pe", lambda e, mt=mt, h=h: e.transpose(bank_bf(4)[0:64, h * 128:(h + 1) * 128], mkn[:, mt, h * 192 + 128:(h + 1) * 192], ident_b),
                    reads=[k_mkn, k_idb], writes=[PK[4]])
            add("act", lambda e, mt=mt: e.copy(KmA[:, :, mt * 128:(mt + 1) * 128], bank_bf(3)[:, 0:512].rearrange("p (a b) -> p a b", a=4)),
                reads=[PK[3]], writes=[k_KmA])
            add("act", lambda e, mt=mt: e.copy(KmB[0:64, :, mt * 128:(mt + 1) * 128], bank_bf(4)[0:64, 0:512].rearrange("p (a b) -> p a b", a=4)),
                reads=[PK[4]], writes=[k_KmB])
            add("dve", lambda e, mt=mt: e.tensor_copy(VmA[:, mt, :, 0:192], mkv[:, mt, 768:1536].rearrange("p (h d) -> p h d", h=4)),
                reads=[k_mkv], writes=[k_VmA])
            add("pool", lambda e, mt=mt: e.memset(VmA[:, mt, :, 192:193], 1.0), writes=[("vm1", mt)])
        mq_r = [AR.get([128, 768], BF16) for _ in range(2)]
        mqA, k_mqA = AR.get([128, 4, 128], BF16)
        mqB, k_mqB = AR.get([128, 4, 128], BF16)
        Pm_r = [AR.get([128, 128], BF16) for _ in range(4)]
        rdn, k_rdn = AR.get([128, 4], F32)
        ym_r = [AR.get([128, 768], BF16) for _ in range(2)]
        pi = 0
        for tg in range(NT):
            t0 = tg * 128
            mqt, k_mqt = mq_r[tg % 2]
            add("sp", lambda e, mqt=mqt, t0=t0: e.dma_start(out=mqt, in_=mq_s[t0:t0 + 128, :]), writes=[k_mqt], dma_key=("m_mq", tg % 2))
            for h in range(4):
                add("pe", lambda e, mqt=mqt, h=h: e.transpose(bank_bf(3)[:, h * 128:(h + 1) * 128], mqt[:, h * 192:h * 192 + 128], ident_b),
                    reads=[k_mqt, k_idb], writes=[PK[3]])
                add("pe", lambda e, mqt=mqt, h=h: e.transpose(bank_bf(4)[0:64, h * 128:(h + 1) * 128], mqt[:, h * 192 + 128:(h + 1) * 192], ident_b),
                    reads=[k_mqt, k_idb], writes=[PK[4]])
            add("act", lambda e: e.copy(mqA.rearrange("p a b -> p (a b)"), bank_bf(3)[:, 0:512]), reads=[PK[3]], writes=[k_mqA])
            add("act", lambda e: e.copy(mqB[0:64].rearrange("p a b -> p (a b)"), bank_bf(4)[0:64, 0:512]), reads=[PK[4]], writes=[k_mqB])
            ob0 = 6
            for h in range(4):
                for mt in range(2):
                    spb = 1 + (pi % 2)
                    sslot = bank(spb)[:, ((pi // 2) % 4) * 128:((pi // 2) % 4 + 1) * 128]
                    ksl = ("pss", spb, (pi // 2) % 4)
                    P, k_P = Pm_r[pi % 4]
                    add("pe", lambda e, sslot=sslot, h=h, mt=mt: e.matmul(sslot, KmA[:, h, mt * 128:(mt + 1) * 128], mqA[:, h, :], start=True, stop=False),
                        reads=[k_KmA, k_mqA], writes=[ksl])
                    add("pe", lambda e, sslot=sslot, h=h, mt=mt: e.matmul(sslot, KmB[0:64, h, mt * 128:(mt + 1) * 128], mqB[0:64, h, :], start=False, stop=True),
                        reads=[k_KmB, k_mqB], writes=[ksl])
                    add("act", lambda e, P=P, sslot=sslot: e.activation(out=P, in_=sslot, func=AF.Exp, scale=192.0 ** -0.5), reads=[ksl], writes=[k_P])
                    pbk = ob0 + h // 2
                    add("pe", lambda e, P=P, h=h, mt=mt, pbk=pbk: e.matmul(bank(pbk)[:, (h % 2) * 256:(h % 2) * 256 + 193], P, VmA[:, mt, h, :],
                                                                           start=(mt == 0), stop=(mt == 1)),
                        reads=[k_P, k_VmA, ("vm1", 0), ("vm1", 1)], writes=[PK[pbk]])
                    pi += 1
            ym, k_ym = ym_r[tg % 2]
            pv = bank(6, 2).rearrange("p (h c) -> p h c", h=4)
            add("dve", lambda e, pv=pv: e.tensor_copy(rdn, pv[:, :, 192]), reads=[PK[6], PK[7]], writes=[k_rdn])
            add("dve", lambda e: e.reciprocal(rdn, rdn), reads=[k_rdn], writes=[k_rdn])
            add("dve", lambda e, pv=pv, ym=ym: e.tensor_tensor(ym.rearrange("p (h d) -> p h d", h=4), pv[:, :, 0:192], bc(rdn, [128, 4, 192], 2), ALU.mult),
                reads=[PK[6], PK[7], k_rdn], writes=[k_ym])
            add("pool", lambda e, ym=ym, t0=t0: e.dma_start(out=ymem_s[t0:t0 + 128, :], in_=ym), reads=[k_ym], dma_key=("m_ym", tg % 2))
        S_.barrier("m")

        AR.reset()
        wsb_t, k_wsb = AR.get([128, 8, D], BF16)
        wdb_t, k_wdb = AR.get([128, 2, D], BF16)
        wmb_t, k_wmb = AR.get([128, 8, D], BF16)
        wo_t, k_wo = AR.get([128, 8, D], BF16)
        nfw_t, k_nfw = AR.get([128, D], F32)
        add("pool", lambda e: e.dma_start(out=wsb_t, in_=wsb), writes=[k_wsb], dma_key="g_c0")
        add("pool", lambda e: e.dma_start(out=wdb_t, in_=wdb), writes=[k_wdb], dma_key="g_c1")
        add("pool", lambda e: e.dma_start(out=wmb_t, in_=wmb), writes=[k_wmb], dma_key="g_c2")
        add("pool", lambda e: e.dma_start(out=wo_t, in_=wo), writes=[k_wo], dma_key="g_c3")
        add("sp", lambda e: e.dma_start(out=nfw_t, in_=nfw), writes=[k_nfw], dma_key="g_c4")
        ys_r = [AR.get([128, D], BF16) for _ in range(2)]
        odl_r = [AR.get([128, 3, 260], F32) for _ in range(2)]
        yml_r = [AR.get([128, 768], BF16) for _ in range(2)]
        gl_r = [AR.get([128, 24, 128], BF16) for _ in range(2)]
        xl_r = [AR.get([128, D], F32) for _ in range(2)]
        oacc, k_oacc = AR.get([128, 260], F32)
        rdd, k_rdd = AR.get([128, 4], F32)
        ydl, k_ydl = AR.get([128, 256], BF16)
        yT, k_yT = AR.get([128, 18, 128], BF16)
        gm, k_gm = AR.get([128, 3, 128], F32)
        mT, k_mT = AR.get([128, 8, 128], BF16)
        hf_r = [AR.get([128, D], F32) for _ in range(2)]
        hsq, k_hsq = AR.get([128, D], F32)
        hss, k_hss = AR.get([128, 16], F32)
        hrs, k_hrs = AR.get([128, 16], F32)
        hnb, k_hnb = AR.get([128, D], BF16)
        hT_r = [AR.get([128, 8, 128], BF16) for _ in range(2)]
        g_v = g_s.rearrange("(c p) t -> p c t", p=128)
        for tg in range(NT):
            t0 = tg * 128
            ys, k_ys = ys_r[tg % 2]
            odl, k_odl = odl_r[tg % 2]
            yml, k_yml = yml_r[tg % 2]
            gl, k_gl = gl_r[tg % 2]
            xl, k_xl = xl_r[tg % 2]
            add("sp", lambda e, ys=ys, t0=t0: e.dma_start(out=ys, in_=yssd_s[t0:t0 + 128, :]), writes=[k_ys], dma_key=("g_ys", tg % 2))
            add("sp", lambda e, odl=odl, t0=t0: e.dma_start(out=odl, in_=od_s[:, t0:t0 + 128, :].rearrange("g t c -> t g c")), writes=[k_odl], dma_key=("g_od", tg % 2))
            add("sp", lambda e, yml=yml, t0=t0: e.dma_start(out=yml, in_=ymem_s[t0:t0 + 128, :]), writes=[k_yml], dma_key=("g_ym", tg % 2))
            add("sp", lambda e, gl=gl, t0=t0: e.dma_start(out=gl, in_=g_v[:, :, t0:t0 + 128]), writes=[k_gl], dma_key=("g_gl", tg % 2))
            add("sp", lambda e, xl=xl, t0=t0: e.dma_start(out=xl, in_=x[t0:t0 + 128, :]), writes=[k_xl], dma_key=("g_xl", tg % 2))
            add("dve", lambda e, odl=odl: e.tensor_tensor(oacc, odl[:, 0, :], odl[:, 1, :], ALU.add), reads=[k_odl], writes=[k_oacc])
            add("dve", lambda e, odl=odl: e.tensor_tensor(oacc, oacc, odl[:, 2, :], ALU.add), reads=[k_odl, k_oacc], writes=[k_oacc])
            ov = oacc.rearrange("p (h c) -> p h c", h=4)
            add("dve", lambda e, ov=ov: e.tensor_copy(rdd, ov[:, :, 64]), reads=[k_oacc], writes=[k_rdd])
            add("dve", lambda e: e.reciprocal(rdd, rdd), reads=[k_rdd], writes=[k_rdd])
            add("dve", lambda e, ov=ov: e.tensor_tensor(ydl.rearrange("p (h d) -> p h d", h=4), ov[:, :, 0:64], bc(rdd, [128, 4, 64], 2), ALU.mult),
                reads=[k_oacc, k_rdd], writes=[k_ydl])
            for kc in range(8):
                add("pe", lambda e, kc=kc, ys=ys: e.transpose(bank_bf(0)[:, kc * 128:(kc + 1) * 128], ys[:, kc * 128:(kc + 1) * 128], ident_b),
                    reads=[k_ys, k_idb], writes=[PK[0]])
            add("act", lambda e: e.copy(yT[:, 0:8, :].rearrange("p a b -> p (a b)"), bank_bf(0)), reads=[PK[0]], writes=[k_yT])
            for kc in range(2):
                add("pe", lambda e, kc=kc: e.transpose(bank_bf(1)[:, kc * 128:(kc + 1) * 128], ydl[:, kc * 128:(kc + 1) * 128], ident_b),
                    reads=[k_ydl, k_idb], writes=[PK[1]])
            for h in range(4):
                add("pe", lambda e, h=h, yml=yml: e.transpose(bank_bf(1)[:, (2 + h) * 128:(3 + h) * 128], yml[:, h * 192:h * 192 + 128], ident_b),
                    reads=[k_yml, k_idb], writes=[PK[1]])
                add("pe", lambda e, h=h, yml=yml: e.transpose(bank_bf(2)[0:64, h * 128:(h + 1) * 128], yml[:, h * 192 + 128:(h + 1) * 192], ident_b),
                    reads=[k_yml, k_idb], writes=[PK[2]])
            add("act", lambda e: e.copy(yT[:, 8:10, :].rearrange("p a b -> p (a b)"), bank_bf(1)[:, 0:256]), reads=[PK[1]], writes=[k_yT])
            for h in range(4):
                add("act", lambda e, h=h: e.copy(yT[:, 10 + 2 * h, :], bank_bf(1)[:, (2 + h) * 128:(3 + h) * 128]), reads=[PK[1]], writes=[k_yT])
                add("act", lambda e, h=h: e.copy(yT[0:64, 11 + 2 * h, :], bank_bf(2)[0:64, h * 128:(h + 1) * 128]), reads=[PK[2]], writes=[k_yT])
            for dmc in range(8):
                pbk = 3 + dmc % 2
                dsl = slice(dmc * 128, (dmc + 1) * 128)
                for kc in range(8):
                    add("pe", lambda e, kc=kc, dsl=dsl, pbk=pbk: e.matmul(bank(pbk)[:, 0:128], wsb_t[:, kc, dsl], yT[:, kc, :], start=(kc == 0), stop=(kc == 7)),
                        reads=[k_wsb, k_yT], writes=[PK[pbk]])
                for kc in range(2):
                    add("pe", lambda e, kc=kc, dsl=dsl, pbk=pbk: e.matmul(bank(pbk)[:, 128:256], wdb_t[:, kc, dsl], yT[:, 8 + kc, :], start=(kc == 0), stop=(kc == 1)),
                        reads=[k_wdb, k_yT], writes=[PK[pbk]])
                for kc in range(8):
                    if kc % 2 == 0:
                        add("pe", lambda e, kc=kc, dsl=dsl, pbk=pbk: e.matmul(bank(pbk)[:, 256:384], wmb_t[:, kc, dsl], yT[:, 10 + kc, :], start=(kc == 0), stop=False),
                            reads=[k_wmb, k_yT], writes=[PK[pbk]])
                    else:
                        add("pe", lambda e, kc=kc, dsl=dsl, pbk=pbk: e.matmul(bank(pbk)[:, 256:384], wmb_t[0:64, kc, dsl], yT[0:64, 10 + kc, :], start=False, stop=(kc == 7)),
                            reads=[k_wmb, k_yT], writes=[PK[pbk]])
                add("dve", lambda e, dmc=dmc, pbk=pbk, gl=gl: e.tensor_tensor(gm, bank(pbk)[:, 0:384].rearrange("p (a b) -> p a b", a=3), gl[:, dmc:24:8, :], ALU.mult),
                    reads=[PK[pbk], k_gl], writes=[k_gm])
                add("dve", lambda e: e.tensor_tensor(gm[:, 0, :], gm[:, 0, :], gm[:, 1, :], ALU.add), reads=[k_gm], writes=[k_gm])
                add("dve", lambda e, dmc=dmc: e.tensor_tensor(mT[:, dmc, :], gm[:, 0, :], gm[:, 2, :], ALU.add), reads=[k_gm], writes=[k_mT])
            for half in range(2):
                for kc in range(8):
                    add("pe", lambda e, kc=kc, half=half: e.matmul(bank(5 + half), mT[:, kc, :], wo_t[:, kc, half * 512:(half + 1) * 512], start=(kc == 0), stop=(kc == 7)),
                        reads=[k_mT, k_wo], writes=[PK[5 + half]])
            hf, k_hf = hf_r[tg % 2]
            add("dve", lambda e, hf=hf, xl=xl: e.tensor_tensor(hf, xl, bank(5, 2), ALU.add), reads=[k_xl, PK[5], PK[6]], writes=[k_hf])
            add("pool", lambda e, hf=hf, t0=t0: e.dma_start(out=h_s[t0:t0 + 128, :], in_=hf), reads=[k_hf], dma_key=("g_hf", tg % 2))
            rms_rstd(hf, [k_hf], D, hsq, k_hsq, hss, k_hss, hrs, k_hrs)
            add("dve", lambda e, hf=hf: e.scalar_tensor_tensor(out=hnb, in0=hf, scalar=hrs[:, 0:1], in1=nfw_t, op0=ALU.mult, op1=ALU.mult),
                reads=[k_hf, k_hrs, k_nfw], writes=[k_hnb])
            for dc in range(8):
                add("pe", lambda e, dc=dc: e.transpose(bank_bf(7)[:, dc * 128:(dc + 1) * 128], hnb[:, dc * 128:(dc + 1) * 128], ident_b),
                    reads=[k_hnb, k_idb], writes=[PK[7]])
            hT, k_hT = hT_r[tg % 2]
            add("act", lambda e, hT=hT: e.copy(hT.rearrange("p a b -> p (a b)"), bank_bf(7)), reads=[PK[7]], writes=[k_hT])
            add("pool", lambda e, hT=hT, tg=tg: e.dma_start(out=hnT_s[tg], in_=hT), reads=[k_hT], dma_key=("g_hT", tg % 2))
        S_.barrier("g")

        AR.reset()
        wq_t, k_wq = AR.get([128, 8, 2048], BF16)
        sk_t, k_sk = AR.get([128, 16, 128], BF16)
        iota16, k_i16 = AR.get([128, 16], F32)
        add("pool", lambda e: e.dma_start(out=wq_t, in_=wq), writes=[k_wq], dma_key="r_c0")
        add("pool", lambda e: e.dma_start(out=sk_t, in_=skT), writes=[k_sk], dma_key="r_c1")
        add("dve", lambda e: e.tensor_copy(iota16, iota_f[:, 0:16]), reads=[k_iota], writes=[k_i16])
        hl_r = [AR.get([128, 8, 128], BF16) for _ in range(2)]
        qT, k_qT = AR.get([128, 16, 128], BF16)
        sc, k_sc = AR.get([128, 16, 128], F32)
        wk, k_wk = AR.get([128, 16, 128], F32)
        mx, k_mx = AR.get([128, 16, 16], F32)
        mi, k_mi = AR.get([128, 16, 16], U32)
        mif, k_mif = AR.get([128, 16, 16], F32)
        cand, k_cand = AR.get([128, 8, 256], F32)
        wk2, k_wk2 = AR.get([128, 8, 256], F32)
        top, k_top = AR.get([128, 8, 16], F32)
        pos, k_pos = AR.get([128, 8, 16], U32)
        posf, k_pf = AR.get([128, 8, 16], F32)
        pa, k_pa = AR.get([128, 8, 16], F32)
        pbb, k_pb = AR.get([128, 8, 16], F32)
        oh, k_oh = AR.get([128, 8, 16, 16], F32)
        gex, k_gex = AR.get([128, 8, 16], F32)
        gz, k_gz = AR.get([128, 8], F32)
        rt, k_rt = AR.get([128, 3, 128], F32)
        rT_r = [AR.get([128, 3, 128], F32) for _ in range(2)]
        for tg in range(NT):
            hl, k_hl = hl_r[tg % 2]
            add("sp", lambda e, hl=hl, tg=tg: e.dma_start(out=hl, in_=hnT_s[tg]), writes=[k_hl], dma_key=("r_hl", tg % 2))
            for f4 in range(4):
                pbk = f4 % 2
                for fi in range(4):
                    fc = f4 * 4 + fi
                    for dc in range(8):
                        add("pe", lambda e, fc=fc, fi=fi, dc=dc, pbk=pbk, hl=hl: e.matmul(
                            bank(pbk)[:, fi * 128:(fi + 1) * 128], wq_t[:, dc, fc * 128:(fc + 1) * 128], hl[:, dc, :], start=(dc == 0), stop=(dc == 7)),
                            reads=[k_wq, k_hl], writes=[PK[pbk]])
                add("act", lambda e, f4=f4, pbk=pbk: e.copy(qT[:, f4 * 4:(f4 + 1) * 4, :].rearrange("p a b -> p (a b)"), bank(pbk)),
                    reads=[PK[pbk]], writes=[("qT", f4)])
            for f4 in range(4):
                pbk = 2 + f4 % 2
                for fi in range(4):
                    fc = f4 * 4 + fi
                    add("pe", lambda e, fc=fc, fi=fi, pbk=pbk: e.matmul(bank(pbk)[:, fi * 128:(fi + 1) * 128], qT[:, fc, :], sk_t[:, fc, :], start=True, stop=True),
                        reads=[("qT", f4), k_sk], writes=[PK[pbk]])
                add("act", lambda e, f4=f4, pbk=pbk: e.copy(sc[:, f4 * 4:(f4 + 1) * 4, :].rearrange("p a b -> p (a b)"), bank(pbk)),
                    reads=[PK[pbk]], writes=[("sc", f4)])
            for fc in range(16):
                add("dve", lambda e, fc=fc: e.max(out=mx[:, fc, 0:8], in_=sc[:, fc, :]), reads=[("sc", fc // 4)], writes=[("mx", fc)])
            for fc in range(16):
                add("dve", lambda e, fc=fc: e.max_index(mi[:, fc, 0:8], mx[:, fc, 0:8], sc[:, fc, :]), reads=[("sc", fc // 4), ("mx", fc)], writes=[("mi", fc)])
            for fc in range(16):
                add("dve", lambda e, fc=fc: e.match_replace(out=wk[:, fc, :], in_to_replace=mx[:, fc, 0:8], in_values=sc[:, fc, :], imm_value=-1e30),
                    reads=[("sc", fc // 4), ("mx", fc)], writes=[("wk", fc)])
            for fc in range(16):
                add("dve", lambda e, fc=fc: e.max(out=mx[:, fc, 8:16], in_=wk[:, fc, :]), reads=[("wk", fc)], writes=[("mx2", fc)])
            for fc in range(16):
                add("dve", lambda e, fc=fc: e.max_index(mi[:, fc, 8:16], mx[:, fc, 8:16], wk[:, fc, :]), reads=[("wk", fc), ("mx2", fc)], writes=[("mi2", fc)])
            allmx = [("mx", f) for f in range(16)] + [("mx2", f) for f in range(16)]
            allmi = [("mi", f) for f in range(16)] + [("mi2", f) for f in range(16)]
            add("dve", lambda e: e.tensor_copy(mif, mi), reads=allmi, writes=[k_mif])
            mxv = mx.rearrange("p (h c) k -> p h c k", c=2)
            mfv = mif.rearrange("p (h c) k -> p h c k", c=2)
            add("dve", lambda e, mxv=mxv: e.tensor_tensor(cand.rearrange("p h (a b) -> p h a b", a=16), bc(mxv[:, :, 0, :], [128, 8, 16, 16], 3),
                                                          bc(mxv[:, :, 1, :], [128, 8, 16, 16], 2), ALU.add), reads=allmx, writes=[k_cand])
            for h in range(8):
                add("dve", lambda e, h=h: e.max(out=top[:, h, 0:8], in_=cand[:, h, :]), reads=[k_cand], writes=[("top", h)])
            for h in range(8):
                add("dve", lambda e, h=h: e.max_index(pos[:, h, 0:8], top[:, h, 0:8], cand[:, h, :]), reads=[k_cand, ("top", h)], writes=[("pos", h)])
            for h in range(8):
                add("dve", lambda e, h=h: e.match_replace(out=wk2[:, h, :], in_to_replace=top[:, h, 0:8], in_values=cand[:, h, :], imm_value=-1e30),
                    reads=[k_cand, ("top", h)], writes=[("wk2", h)])
            for h in range(8):
                add("dve", lambda e, h=h: e.max(out=top[:, h, 8:16], in_=wk2[:, h, :]), reads=[("wk2", h)], writes=[("top2", h)])
            for h in range(8):
                add("dve", lambda e, h=h: e.max_index(pos[:, h, 8:16], top[:, h, 8:16], wk2[:, h, :]), reads=[("wk2", h), ("top2", h)], writes=[("pos2", h)])
            alltop = [("top", h) for h in range(8)] + [("top2", h) for h in range(8)]
            allpos = [("pos", h) for h in range(8)] + [("pos2", h) for h in range(8)]
            add("dve", lambda e: e.tensor_tensor(gex, top, bc(top[:, :, 0], [128, 8, 16], 2), ALU.subtract), reads=alltop, writes=[k_gex])
            add("act", lambda e: e.activation(out=gex, in_=gex, func=AF.Exp), reads=[k_gex], writes=[k_gex])
            add("dve", lambda e: e.tensor_reduce(out=gz, in_=gex, axis=AX.X, op=ALU.add), reads=[k_gex], writes=[k_gz])
            add("dve", lambda e: e.reciprocal(gz, gz), reads=[k_gz], writes=[k_gz])
            add("dve", lambda e: e.tensor_tensor(rt[:, 2, :].rearrange("p (h k) -> p h k", h=8), gex, bc(gz, [128, 8, 16], 2), ALU.mult),
                reads=[k_gex, k_gz], writes=[("rt", 2)])
            add("dve", lambda e: e.tensor_copy(posf, pos), reads=allpos, writes=[k_pf])
            add("dve", lambda e: e.tensor_scalar(pa, posf, -7.5, 0.0625, ALU.add, ALU.mult), reads=[k_pf], writes=[k_pa])
            add("dve", lambda e: e.tensor_single_scalar(pa, pa, MAGIC, ALU.add), reads=[k_pa], writes=[k_pa])
            add("dve", lambda e: e.tensor_single_scalar(pa, pa, -MAGIC, ALU.add), reads=[k_pa], writes=[k_pa])
            add("dve", lambda e: e.scalar_tensor_tensor(out=pbb, in0=pa, scalar=-16.0, in1=posf, op0=ALU.mult, op1=ALU.add), reads=[k_pa, k_pf], writes=[k_pb])
            for which, src, k_src, cidx in ((0, pa, k_pa, 0), (1, pbb, k_pb, 1)):
                add("dve", lambda e, src=src: e.tensor_tensor(oh, bc(src, [128, 8, 16, 16], 3),
                                                              iota16.unsqueeze(1).unsqueeze(1).to_broadcast([128, 8, 16, 16]), ALU.is_equal),
                    reads=[k_src, k_i16], writes=[k_oh])
                add("dve", lambda e, cidx=cidx, mfv=mfv: e.tensor_tensor(oh, oh, bc(mfv[:, :, cidx, :], [128, 8, 16, 16], 2), ALU.mult),
                    reads=[k_oh, k_mif], writes=[k_oh])
                add("dve", lambda e, which=which: e.tensor_reduce(out=rt[:, which, :].rearrange("p (h k) -> p h k", h=8), in_=oh, axis=AX.X, op=ALU.add),
                    reads=[k_oh], writes=[("rt", which)])
            rT, k_rT = rT_r[tg % 2]
            for w3 in range(3):
                add("pe", lambda e, w3=w3: e.transpose(bank(4)[:, w3 * 128:(w3 + 1) * 128], rt[:, w3, :], ident_f),
                    reads=[("rt", w3), k_idf], writes=[PK[4]])
            add("act", lambda e, rT=rT: e.copy(rT.rearrange("p a b -> p (a b)"), bank(4)[:, 0:384]), reads=[PK[4]], writes=[k_rT])
            add("pool", lambda e, rT=rT, tg=tg: e.dma_start(out=r_s[tg], in_=rT), reads=[k_rT], dma_key=("r_rT", tg % 2))
        S_.barrier("r")

        AR.reset()
        NB2 = S // 256
        hT2_r = [AR.get([128, 8, 256], BF16) for _ in range(2)]
        rl_r = [AR.get([128, 2, 3, 128], F32) for _ in range(2)]
        A_r = [AR.get([128, 32, 128], BF16) for _ in range(2)]
        B_r = [AR.get([128, 32, 128], BF16) for _ in range(2)]
        M_t, k_M = AR.get([128, 256, 128], BF16)
        dW_r = [AR.get([128, 4, D], BF16) for _ in range(3)]
        uW_r = [AR.get([128, 4, D], BF16) for _ in range(3)]
        ge_r = [AR.get([128, 256], BF16) for _ in range(4)]
        co_r = [AR.get([128, 256], BF16) for _ in range(4)]
        hl2_r = [AR.get([128, D], F32) for _ in range(2)]
        LA = 2
        oi2 = 0
        sbi = 0
        wcount = [0]
        for b2 in range(NB2):
            hT2, k_hT2 = hT2_r[b2 % 2]
            rl, k_rl = rl_r[b2 % 2]
            for tt in range(2):
                tg = b2 * 2 + tt
                add("sp", lambda e, hT2=hT2, tt=tt, tg=tg: e.dma_start(out=hT2[:, :, tt * 128:(tt + 1) * 128], in_=hnT_s[tg]),
                    writes=[k_hT2], dma_key=("p_h", b2 % 2))
                add("sp", lambda e, rl=rl, tt=tt, tg=tg: e.dma_start(out=rl[:, tt, :, :], in_=r_s[tg]), writes=[k_rl], dma_key=("p_r", b2 % 2))
            for sb in range(8):
                tt, to = sb // 4, (sb % 4) * 32
                A_, k_A = A_r[sbi % 2]
                B_, k_B = B_r[sbi % 2]
                sbi += 1
                i1 = rl[:, tt, 0, to:to + 32]
                i2 = rl[:, tt, 1, to:to + 32]
                gg = rl[:, tt, 2, to:to + 32]
                add("dve", lambda e, A_=A_, i1=i1: e.tensor_tensor(A_, bc(iota_f, [128, 32, 128], 1), bc(i1, [128, 32, 128], 2), ALU.is_equal),
                    reads=[k_rl, k_iota], writes=[k_A])
                add("pool", lambda e, A_=A_, gg=gg: e.tensor_tensor(A_, A_, bc(gg, [128, 32, 128], 2), ALU.mult), reads=[k_rl, k_A], writes=[k_A])
                add("dve", lambda e, B_=B_, i2=i2: e.tensor_tensor(B_, bc(iota_f, [128, 32, 128], 1), bc(i2, [128, 32, 128], 2), ALU.is_equal),
                    reads=[k_rl, k_iota], writes=[k_B])
                for q in range(8):
                    pbk = 4 + (q % 4)
                    for t4 in range(4):
                        tk = q * 4 + t4
                        add("pe", lambda e, A_=A_, B_=B_, tk=tk, t4=t4, pbk=pbk: e.matmul(
                            bank(pbk)[:, t4 * 128:(t4 + 1) * 128], A_[:, tk, :], B_[:, tk, :], start=True, stop=True),
                            reads=[k_A, k_B], writes=[PK[pbk]])
                    tb = sb * 32 + q * 4
                    add("act", lambda e, tb=tb, pbk=pbk: e.copy(M_t[:, tb:tb + 4, :].rearrange("p a b -> p (a b)"), bank(pbk)),
                        reads=[PK[pbk]], writes=[("M", sb)])
            allM = [("M", sb) for sb in range(8)]
            tiles = {}

            def load_w(jq):
                wi = wcount[0]
                wcount[0] += 1
                dW, k_dW = dW_r[wi % 3]
                uW, k_uW = uW_r[wi % 3]
                add("sp", lambda e, dW=dW, jq=jq: e.dma_start(out=dW, in_=dnT_b[jq * 4:(jq + 1) * 4].rearrange("j p f -> p j f")),
                    writes=[k_dW], dma_key=("p_dw", wi % 3))
                add("sp", lambda e, uW=uW, jq=jq: e.dma_start(out=uW, in_=up_b[jq * 4:(jq + 1) * 4].rearrange("j p f -> p j f")),
                    writes=[k_uW], dma_key=("p_uw", wi % 3))
                tiles[jq] = (dW, k_dW, uW, k_uW)

            def act_mm(j):
                dW, k_dW, uW, k_uW = tiles[j // 4]
                jj = j % 4
                pbk = 4 + (j % 4)
                for dc in range(8):
                    add("pe", lambda e, dW=dW, jj=jj, dc=dc, pbk=pbk, hT2=hT2: e.matmul(
                        bank(pbk)[:, 0:256], dW[:, jj, dc * 128:(dc + 1) * 128], hT2[:, dc, :], start=(dc == 0), stop=(dc == 7)),
                        reads=[k_dW, k_hT2], writes=[PK[pbk]])

            load_w(0)
            load_w(1)
            for j0 in range(LA):
                act_mm(j0)
            for j in range(128):
                jn = j + LA
                if jn < 128:
                    if jn % 4 == 0 and (jn // 4 + 1) < 32:
                        load_w(jn // 4 + 1)
                    act_mm(jn)
                dW, k_dW, uW, k_uW = tiles[j // 4]
                jj = j % 4
                pbk = 4 + (j % 4)
                ge, k_ge = ge_r[j % 4]
                co, k_co = co_r[j % 4]
                add("act", lambda e, ge=ge, pbk=pbk: e.activation(out=ge, in_=bank(pbk)[:, 0:256], func=AF.Gelu), reads=[PK[pbk]], writes=[k_ge])
                add("dve", lambda e, ge=ge, co=co, j=j: e.tensor_tensor(co, ge, M_t[:, :, j], ALU.mult), reads=[k_ge] + allM, writes=[k_co])
                for tt in range(2):
                    for half in range(2):
                        add("pe", lambda e, co=co, uW=uW, jj=jj, tt=tt, half=half, j=j: e.matmul(
                            bank(tt * 2 + half), co[:, tt * 128:(tt + 1) * 128], uW[:, jj, half * 512:(half + 1) * 512],
                            start=(j == 0), stop=(j == 127)), reads=[k_co, k_uW], writes=[PK[tt * 2 + half]])
            for tt in range(2):
                tg = b2 * 2 + tt
                t0 = tg * 128
                hl2, k_hl2 = hl2_r[oi2 % 2]
                add("sp", lambda e, hl2=hl2, t0=t0: e.dma_start(out=hl2, in_=h_s[t0:t0 + 128, :]), writes=[k_hl2], dma_key=("p_hl", oi2 % 2))
                add("dve", lambda e, hl2=hl2, tt=tt: e.tensor_tensor(hl2, hl2, bank(tt * 2, 2), ALU.add),
                    reads=[k_hl2, PK[tt * 2], PK[tt * 2 + 1]], writes=[k_hl2])
                add("pool", lambda e, hl2=hl2, t0=t0: e.dma_start(out=out[t0:t0 + 128, :], in_=hl2), reads=[k_hl2], dma_key=("p_out", oi2 % 2))
                oi2 += 1
        S_.emit(st)
        build_program.stats = S_.stats
    return nc


def _rep(v, n=128):
    return np.ascontiguousarray(np.broadcast_to(np.asarray(v, dtype=np.float32).reshape(1, -1), (n, np.asarray(v).size)))


def _kmaj(w, nchunk):
    w = np.asarray(w, dtype=np.float32)
    return np.ascontiguousarray(w.reshape(nchunk, 128, w.shape[1]).transpose(1, 0, 2))


def shared_inputs(inp):
    L = 0
    sh = {}
    sh["nmw"] = _rep(inp["norm_mix_w"][L])
    sh["w_in_r"] = _kmaj(inp["w_in"][L], 8)
    cwv = np.asarray(inp["ssd_conv_w"][L], dtype=np.float32)
    sh["cw"] = np.ascontiguousarray(cwv.reshape(4, 16, 128).transpose(2, 1, 0))
    sh["cb"] = np.ascontiguousarray(np.asarray(inp["ssd_conv_b"][L], dtype=np.float32).reshape(16, 128).T)
    sh["dtb"] = _rep(inp["ssd_dt_bias"][L])
    sh["alog"] = _rep(inp["ssd_a_log"][L])
    sh["sdd"] = _rep(inp["ssd_d"][L])
    sh["snw"] = _rep(inp["ssd_norm_w"][L])
    sh["qnw"] = _rep(inp["dil_q_norm_w"][L])
    sh["knw"] = _rep(inp["dil_k_norm_w"][L])
    sh["mnw"] = _rep(inp["mem_norm_w"][L])
    sh["wkv"] = _kmaj(inp["w_mem_kv"][L], 8)
    sh["mqnw"] = _rep(inp["mem_q_norm_w"][L])
    sh["mknw"] = _rep(inp["mem_k_norm_w"][L])
    sh["wsb"] = _kmaj(inp["w_ssd_br"][L], 8)
    sh["wdb"] = _kmaj(inp["w_dil_br"][L], 2)
    wm = np.asarray(inp["w_mem_br"][L], dtype=np.float32)
    wmp = np.zeros((128, 8, D), dtype=np.float32)
    for h in range(4):
        wmp[:, 2 * h, :] = wm[h * 192:h * 192 + 128]
        wmp[0:64, 2 * h + 1, :] = wm[h * 192 + 128:(h + 1) * 192]
    sh["wmb"] = wmp
    sh["wo"] = _kmaj(inp["w_out"][L], 8)
    sh["nfw"] = _rep(inp["norm_ffn_w"][L])
    sh["wq"] = _kmaj(inp["peer_w_query"][L], 8)
    sk = np.asarray(inp["peer_sub_keys"][L], dtype=np.float32)
    sh["skT"] = np.ascontiguousarray(sk.reshape(16, 128, 128).transpose(2, 0, 1))
    dn = np.asarray(inp["peer_down"][L], dtype=np.float32).reshape(128, 128, 8, 128)
    sh["dnT_r"] = np.ascontiguousarray(dn.transpose(1, 3, 2, 0)).reshape(128, 128, D)
    up = np.asarray(inp["peer_up"][L], dtype=np.float32).reshape(128, 128, D)
    sh["up_r"] = np.ascontiguousarray(up.transpose(1, 0, 2))
    return sh


def core_inputs(inp, b, S):
    NT = S // 128
    posb = np.asarray(inp["positions"][b][:S], dtype=np.int32)
    return {
        "x": np.ascontiguousarray(np.asarray(inp["x"][b][:S], dtype=np.float32)),
        "mem": np.ascontiguousarray(np.asarray(inp["mem"][b], dtype=np.float32)),
        "pos_t": np.ascontiguousarray(posb.reshape(NT, 128).T),
    }


def kernel(**inputs):
    B, S = inputs["x"].shape[0], inputs["x"].shape[1]
    nc = build_program(S)
    sh = shared_inputs(inputs)
    in_maps = []
    for b in range(B):
        m = dict(sh)
        m.update(core_inputs(inputs, b, S))
        in_maps.append(m)
    res = run_bass_kernel_spmd(nc, in_maps, core_ids=list(range(B)))
    return np.stack([np.asarray(r["out"], dtype=np.float32) for r in res.results], axis=0)
```
